# Optimizing a Trainium2 kernel written in Bass

```python
import jax, jax.numpy as jnp
from jax import lax
import numpy as np

D_MODEL = 1024
BATCH = 16
SEQ = 2048
DEPTH = 1

CHUNK = 64
RWKV_WIDTH = 512
HEAD_SIZE = 64
N_HEADS = RWKV_WIDTH // HEAD_SIZE
DECAY_LORA = 64
AAA_LORA = 64
GATE_LORA = 128
GN_EPS = HEAD_SIZE * 1e-5
POOL_WIDTH = 512
POOL_WINDOWS = (2, 4, 8, 16)
N_POOL_GROUPS = len(POOL_WINDOWS)
POOL_GROUP = POOL_WIDTH // N_POOL_GROUPS
N_BRANCH = 2
PROJ_WIDTH = 3 * RWKV_WIDTH + POOL_WIDTH + N_BRANCH * D_MODEL
D_FF = 2816
RMS_EPS = 1e-6

kernel_name = "macaron_gated_rwkv7_multiscale_pool_block"


def rms_norm(x, g):
    xf = x.astype(jnp.float32)
    y = xf * lax.rsqrt(jnp.mean(xf * xf, axis=-1, keepdims=True) + RMS_EPS)
    return (y * g.astype(jnp.float32)).astype(x.dtype)


def token_shift(x):
    return jnp.pad(x, ((0, 0), (1, 0), (0, 0)))[:, :-1]


def swiglu(x, w_gate, w_up, w_down):
    return (jax.nn.silu(x @ w_gate) * (x @ w_up)) @ w_down


def wkv7_recurrence(r, decay, k, v, a_vec, b_vec):
    b, s, h, n = r.shape

    def to_chunks(t):
        return t.astype(jnp.float32).transpose(1, 0, 2, 3).reshape(s // CHUNK, CHUNK, b, h, n)

    def frame_step(state, inp):
        r_t, w_t, k_t, v_t, a_t, b_t = inp
        sa = jnp.einsum('bhij,bhj->bhi', state, a_t)
        state = (state * w_t[:, :, None, :]
                 + sa[..., None] * b_t[:, :, None, :]
                 + v_t[..., None] * k_t[:, :, None, :])
        y_t = jnp.einsum('bhij,bhj->bhi', state, r_t)
        return state, y_t

    def chunk_step(state, chunk_inp):
        return lax.scan(frame_step, state, chunk_inp)

    state0 = jnp.zeros((b, h, n, n), jnp.float32)
    inputs = (to_chunks(r), to_chunks(decay), to_chunks(k), to_chunks(v),
              to_chunks(a_vec), to_chunks(b_vec))
    _, y = lax.scan(chunk_step, state0, inputs)
    return y.reshape(s, b, h, n).transpose(1, 0, 2, 3)


def rwkv7_branch(h, p_r, p_k, p_v, mu_rkv, mu_wag, w0, decay_a, decay_b, a0,
                 aaa_a, aaa_b, gate_a, gate_b, k_k, k_a, r_k, ln_x_w, ln_x_b):
    bsz, s, _ = h.shape
    f32 = jnp.float32
    r = p_r + (token_shift(p_r) - p_r) * mu_rkv[0]
    k = p_k + (token_shift(p_k) - p_k) * mu_rkv[1]
    v = p_v + (token_shift(p_v) - p_v) * mu_rkv[2]
    hx = token_shift(h) - h
    xw = h + hx * mu_wag[0]
    xa = h + hx * mu_wag[1]
    xg = h + hx * mu_wag[2]
    w_log = -jax.nn.softplus(-(w0 + jnp.tanh(xw @ decay_a) @ decay_b).astype(f32)) - 0.5
    decay = jnp.exp(-jnp.exp(w_log))
    a = jax.nn.sigmoid((a0 + (xa @ aaa_a) @ aaa_b).astype(f32))
    g = jax.nn.sigmoid(xg @ gate_a) @ gate_b

    heads = lambda t: t.reshape(bsz, s, N_HEADS, HEAD_SIZE)
    pheads = lambda t: t.astype(f32).reshape(N_HEADS, HEAD_SIZE)
    r_h, k_h, v_h = heads(r.astype(f32)), heads(k.astype(f32)), heads(v.astype(f32))
    a_h, w_h = heads(a), heads(decay)
    kk = k_h * pheads(k_k)
    kk = kk / jnp.maximum(jnp.sqrt(jnp.sum(kk * kk, axis=-1, keepdims=True)), 1e-12)
    k_h = k_h * (1.0 + (a_h - 1.0) * pheads(k_a))

    y = wkv7_recurrence(r_h, w_h, k_h, v_h, -kk, kk * a_h)
    mu = jnp.mean(y, axis=-1, keepdims=True)
    var = jnp.mean(jnp.square(y - mu), axis=-1, keepdims=True)
    y = (y - mu) * lax.rsqrt(var + GN_EPS)
    y = y * pheads(ln_x_w) + pheads(ln_x_b)
    bonus = jnp.sum(r_h * k_h * r_k.astype(f32), axis=-1, keepdims=True) * v_h
    y = (y + bonus).reshape(bsz, s, RWKV_WIDTH)
    return (y * g.astype(f32)).astype(h.dtype)


def multiscale_pool_branch(p, pool_w, pool_scale):
    bsz, s, _ = p.shape
    pf = p.astype(jnp.float32).reshape(bsz, s, N_POOL_GROUPS, POOL_GROUP)
    cs = jnp.pad(jnp.cumsum(pf, axis=1), ((0, 0), (1, 0), (0, 0), (0, 0)))
    windows = jnp.array(POOL_WINDOWS, dtype=jnp.int32)
    hi = jnp.arange(1, s + 1, dtype=jnp.int32)[:, None]
    lo = jnp.maximum(hi - windows[None, :], 0)
    cs_lo = cs[:, lo, jnp.arange(N_POOL_GROUPS)[None, :], :]
    count = (hi - lo).astype(jnp.float32)[None, :, :, None]
    mixed = (cs[:, 1:] - cs_lo) / count - pf
    out = jnp.einsum('bsgc,gcd->bsgd', mixed, pool_w.astype(jnp.float32))
    out = out.reshape(bsz, s, POOL_WIDTH) * pool_scale.astype(jnp.float32)
    return out.astype(p.dtype)


def _normal(key, shape, fan_in, scale=1.0):
    return scale * jax.random.normal(key, shape, jnp.float32) * (fan_in ** -0.5)


def setup_inputs(seed: int = 0) -> dict:
    key = jax.random.key(seed)
    ks = jax.random.split(key, 40)
    L, D, RW, PW = DEPTH, D_MODEL, RWKV_WIDTH, POOL_WIDTH
    nrm = lambda k, shape: jax.random.normal(k, shape, jnp.float32)
    return {
        "x": nrm(ks[0], (BATCH, SEQ, D)),
        "norm_gains": 1.0 + 0.02 * nrm(ks[1], (L, 6, D)),
        "ffn1_gate": _normal(ks[2], (L, D, D_FF), D),
        "ffn1_up": _normal(ks[3], (L, D, D_FF), D),
        "ffn1_down": _normal(ks[4], (L, D_FF, D), D_FF),
        "w_in": _normal(ks[5], (L, D, PROJ_WIDTH), D),
        "gate_bias": 0.1 * nrm(ks[6], (L, N_BRANCH, D)),
        "mu_rkv": jax.random.uniform(ks[7], (L, 3, RW), jnp.float32),
        "mu_wag": jax.random.uniform(ks[8], (L, 3, D), jnp.float32),
        "w0": jax.random.uniform(ks[9], (L, RW), jnp.float32, minval=-6.5, maxval=-1.5),
        "decay_a": _normal(ks[10], (L, D, DECAY_LORA), D),
        "decay_b": _normal(ks[11], (L, DECAY_LORA, RW), DECAY_LORA, 0.1),
        "a0": 0.1 * nrm(ks[12], (L, RW)),
        "aaa_a": _normal(ks[13], (L, D, AAA_LORA), D),
        "aaa_b": _normal(ks[14], (L, AAA_LORA, RW), AAA_LORA),
        "gate_a": _normal(ks[15], (L, D, GATE_LORA), D),
        "gate_b": _normal(ks[16], (L, GATE_LORA, RW), GATE_LORA),
        "k_k": 0.85 + 0.02 * nrm(ks[17], (L, RW)),
        "k_a": 1.0 + 0.02 * nrm(ks[18], (L, RW)),
        "r_k": 0.1 * nrm(ks[19], (L, N_HEADS, HEAD_SIZE)),
        "ln_x_w": 1.0 + 0.02 * nrm(ks[20], (L, RW)),
        "ln_x_b": 0.02 * nrm(ks[21], (L, RW)),
        "pool_w": _normal(ks[22], (L, N_POOL_GROUPS, POOL_GROUP, POOL_GROUP), POOL_GROUP),
        "pool_scale": 1.0 + 0.02 * nrm(ks[23], (L, PW)),
        "w_branch_rwkv": _normal(ks[24], (L, RW, D), RW),
        "w_branch_pool": _normal(ks[25], (L, PW, D), PW),
        "w_out": _normal(ks[26], (L, D, D), D),
        "ffn2_gate": _normal(ks[27], (L, D, D_FF), D),
        "ffn2_up": _normal(ks[28], (L, D, D_FF), D),
        "ffn2_down": _normal(ks[29], (L, D_FF, D), D_FF),
    }


def reference(x, norm_gains, ffn1_gate, ffn1_up, ffn1_down, w_in, gate_bias, mu_rkv,
              mu_wag, w0, decay_a, decay_b, a0, aaa_a, aaa_b, gate_a, gate_b, k_k, k_a,
              r_k, ln_x_w, ln_x_b, pool_w, pool_scale, w_branch_rwkv, w_branch_pool,
              w_out, ffn2_gate, ffn2_up, ffn2_down):
    bsz, s, d = x.shape
    split_at = [RWKV_WIDTH, 2 * RWKV_WIDTH, 3 * RWKV_WIDTH, 3 * RWKV_WIDTH + POOL_WIDTH]
    for l in range(DEPTH):
        g = norm_gains[l]
        f = swiglu(rms_norm(x, g[0]), ffn1_gate[l], ffn1_up[l], ffn1_down[l])
        x = x + 0.5 * rms_norm(f, g[1])

        h = rms_norm(x, g[2])
        proj = h @ w_in[l]
        p_r, p_k, p_v, p_pool, gate_logits = jnp.split(proj, split_at, axis=-1)
        gates = jax.nn.sigmoid(gate_logits.reshape(bsz, s, N_BRANCH, d) + gate_bias[l])

        y_rwkv = rwkv7_branch(h, p_r, p_k, p_v, mu_rkv[l], mu_wag[l], w0[l], decay_a[l],
                              decay_b[l], a0[l], aaa_a[l], aaa_b[l], gate_a[l], gate_b[l],
                              k_k[l], k_a[l], r_k[l], ln_x_w[l], ln_x_b[l])
        y_pool = multiscale_pool_branch(p_pool, pool_w[l], pool_scale[l])

        merged = (gates[:, :, 0] * (y_rwkv @ w_branch_rwkv[l])
                  + gates[:, :, 1] * (y_pool @ w_branch_pool[l]))
        x = x + rms_norm(merged @ w_out[l], g[3])

        f = swiglu(rms_norm(x, g[4]), ffn2_gate[l], ffn2_up[l], ffn2_down[l])
        x = x + 0.5 * rms_norm(f, g[5])
    return x
```

```python
import numpy as np
from contextlib import ExitStack
import concourse.bass as bass
import concourse.mybir as mybir
from concourse.bass_utils import run_bass_kernel_spmd

F32 = mybir.dt.float32
BF16 = mybir.dt.bfloat16
AF = mybir.ActivationFunctionType
ALU = mybir.AluOpType
ESZ = {F32: 4, BF16: 2}

ENGS = ("pe", "act", "dve", "pool", "sp")
GRAN = 256
NCORES = 8
NH = 2
TT = 512
D = 1024
DFF = 2816
NFU = 22
CH = 64
C0 = float(np.exp(-0.5))
RMS_EPS = 1e-6
GN_EPS = 64e-5


class Prog:
    def __init__(self, nc, n_dma_chan):
        self.nc = nc
        self.ops = {e: [] for e in ENGS}
        self.cnt = {e: 0 for e in ENGS}
        self.pending = {e: False for e in ENGS}
        self.last_w = {}
        self.readers = {}
        self.water = {e: {} for e in ENGS}
        self.dcnt = [0] * n_dma_chan
        self.n_dma_chan = n_dma_chan
        self.tracked = set()
        self.nops = 0

    def keys(self, ap):
        name = ap.tensor.name
        if name not in self.tracked:
            return ()
        esz = ESZ[ap.dtype]
        pat = ap.ap
        ps = pat[0][0]
        off = ap.offset % ps if ps > 0 else ap.offset
        span = 1
        for st, n in pat[1:]:
            span += (n - 1) * abs(st)
        lo = off * esz
        hi = (off + span) * esz
        return [(name, g) for g in range(lo // GRAN, (hi - 1) // GRAN + 1)]

    def _collect(self, eng, rkeys, wkeys):
        deps = {}

        def add(d, raw):
            k, v, pe = d
            if pe == eng and not raw:
                return
            if deps.get(k, 0) < v:
                deps[k] = v

        lw = self.last_w
        for key in rkeys:
            d = lw.get(key)
            if d is not None:
                add(d, True)
        for key in wkeys:
            d = lw.get(key)
            if d is not None:
                add(d, False)
            for d in self.readers.get(key, ()):
                add(d, False)
        out = []
        wm = self.water[eng]
        for k, v in deps.items():
            if wm.get(k, 0) < v:
                wm[k] = v
                out.append((k, v))
        return out

    def _record(self, dep, rkeys, wkeys):
        for key in rkeys:
            lst = self.readers.setdefault(key, [])
            for i, d in enumerate(lst):
                if d[0] == dep[0]:
                    lst[i] = dep
                    break
            else:
                lst.append(dep)
        for key in wkeys:
            self.last_w[key] = dep
            self.readers[key] = []

    def _rw(self, reads, writes):
        rk = []
        for a in reads:
            rk.extend(self.keys(a))
        wk = []
        for a in writes:
            wk.extend(self.keys(a))
        return rk, wk

    def op(self, eng, fn, reads, writes, signal=True):
        rk, wk = self._rw(reads, writes)
        waits = self._collect(eng, rk, wk)
        if signal:
            self.cnt[eng] += 1
            dep = (eng, self.cnt[eng], eng)
            self.pending[eng] = False
        else:
            dep = (eng, self.cnt[eng] + 1, eng)
            self.pending[eng] = True
        self._record(dep, rk, wk)
        self.ops[eng].append((waits, fn, "e" if signal else None))
        self.nops += 1

    def dma(self, eng, chan, out, in_, **kw):
        rk, wk = self._rw([in_], [out])
        waits = self._collect(eng, rk, wk)
        self.dcnt[chan] += 16
        dep = (("dma", chan), self.dcnt[chan], "dma")
        self._record(dep, rk, wk)
        self.ops[eng].append((waits, lambda e: e.dma_start(out=out, in_=in_, **kw), ("dma", chan)))
        self.nops += 1

    def bump(self, chan):
        k = ("dma", chan)
        full = (k, self.dcnt[chan], "dma")
        for key, dep in self.last_w.items():
            if dep[0] == k:
                self.last_w[key] = full

    def finish(self, eng="sp"):
        waits = []
        for e in ENGS:
            if e != eng and self.cnt[e] > 0:
                waits.append((e, self.cnt[e]))
        for c in range(self.n_dma_chan):
            if self.dcnt[c] > 0:
                waits.append((("dma", c), self.dcnt[c]))
        self.ops[eng].append((waits, None, None))

    def emit(self, block, sems, dsems):
        for e in ENGS:
            assert not self.pending[e], f"unsignalled tail on {e}"

        def semof(k):
            return dsems[k[1]] if isinstance(k, tuple) else sems[k]

        def run(name, engine):
            sem = sems[name]
            for waits, fn, inc in self.ops[name]:
                for k, v in waits:
                    engine.wait_ge(semof(k), v)
                if fn is None:
                    continue
                ins = fn(engine)
                if inc is None:
                    continue
                if inc == "e":
                    ins.then_inc(sem, 1)
                else:
                    ins.then_inc(dsems[inc[1]], 16)

        block.tensor(lambda e: run("pe", e))
        block.scalar(lambda e: run("act", e))
        block.vector(lambda e: run("dve", e))
        block.gpsimd(lambda e: run("pool", e))
        block.sync(lambda e: run("sp", e))

    def mm(self, out, lhsT, rhs, start=True, stop=True, signal=True):
        self.op("pe", lambda e: e.matmul(out, lhsT=lhsT, rhs=rhs, start=start, stop=stop),
                [lhsT, rhs], [out], signal)

    def tr(self, out, in_, ident, signal=True):
        self.op("pe", lambda e: e.transpose(out, in_, ident), [in_, ident], [out], signal)

    def act(self, out, in_, func, bias=None, scale=None, eng="act"):
        reads = [in_]
        kw = {}
        if bias is not None:
            kw["bias"] = bias
            if not isinstance(bias, (int, float)):
                reads.append(bias)
        if scale is not None:
            kw["scale"] = scale
            if not isinstance(scale, (int, float)):
                reads.append(scale)
        self.op(eng, lambda e: e.activation(out=out, in_=in_, func=func, **kw), reads, [out])

    def tt(self, out, in0, in1, op, eng="dve"):
        self.op(eng, lambda e: e.tensor_tensor(out=out, in0=in0, in1=in1, op=op), [in0, in1], [out])

    def ts(self, out, in0, s1, op0, s2=None, op1=None, eng="dve"):
        reads = [in0]
        for s in (s1, s2):
            if s is not None and not isinstance(s, (int, float)):
                reads.append(s)
        if op1 is None:
            fn = lambda e: e.tensor_scalar(out=out, in0=in0, scalar1=s1, scalar2=None, op0=op0)
        else:
            fn = lambda e: e.tensor_scalar(out=out, in0=in0, scalar1=s1, scalar2=s2, op0=op0, op1=op1)
        self.op(eng, fn, reads, [out])

    def stt(self, out, in0, scalar, in1, op0, op1):
        reads = [in0, in1]
        if not isinstance(scalar, (int, float)):
            reads.append(scalar)
        self.op("dve", lambda e: e.scalar_tensor_tensor(out=out, in0=in0, scalar=scalar, in1=in1, op0=op0, op1=op1),
                reads, [out])

    def copy(self, out, in_, eng="dve"):
        if eng == "act":
            self.act(out, in_, AF.Copy)
        else:
            self.op(eng, lambda e: e.tensor_copy(out=out, in_=in_), [in_], [out])

    def scan(self, out, d0, d1, init, op0, op1):
        self.op("dve", lambda e: e.tensor_tensor_scan(out=out, data0=d0, data1=d1, initial=init, op0=op0, op1=op1),
                [d0, d1], [out])

    def recip(self, out, in_):
        self.op("dve", lambda e: e.reciprocal(out=out, in_=in_), [in_], [out])

    def memset(self, ap, val, eng="dve"):
        self.op(eng, lambda e: e.memset(ap, val), [], [ap])


V_G = 0
V_GB = 48
V_MU = 64
V_W0 = 76
V_A0 = 80
V_KK = 84
V_KA = 88
V_RK = 92
V_LW = 96
V_LB = 100
V_PS = 104
V_OMU = 108
V_HG1 = 120
V_HG5 = 128
NV = 136
NV_IN = 108


def host_consts():
    c = {}
    c["ident"] = np.eye(128, dtype=np.float32)
    bo = np.zeros((128, 128), np.float32)
    bo[:64, :64] = 1.0
    bo[64:, 64:] = 1.0
    c["blockones"] = bo
    c["ones"] = np.ones((128, 128), np.float32)
    s = np.arange(64)[:, None]
    t = np.arange(64)[None, :]
    strict = (s < t).astype(np.float32)
    incl = (s <= t).astype(np.float32)
    m4 = np.concatenate([strict, incl, strict, incl], 1)
    c["maskA"] = np.tile(m4, (1, 2)).copy()
    c["maskNT"] = np.tile(strict.T, (1, 8)).copy()
    c["identM"] = np.tile(np.eye(64, dtype=np.float32), (1, 16)).copy()
    rm = np.ones((128, TT), np.float32)
    rm[:, ::CH] = 0.0
    c["resetmask"] = rm
    ic = np.zeros((128, 4, 16), np.float32)
    for g, w in enumerate((2, 4, 8, 16)):
        ic[:, g, :] = 1.0 / np.minimum(np.arange(1, 17), w)
    c["invc0"] = ic.reshape(128, 64)
    return c


def host_weights(inp):
    L = 0
    w = {}

    def A(wg, wu):
        g = wg.reshape(8, 128, NFU, 128).transpose(2, 1, 0, 3)
        u = wu.reshape(8, 128, NFU, 128).transpose(2, 1, 0, 3)
        return np.ascontiguousarray(np.stack([g, u], 2).reshape(NFU, 128, 2048))

    def B(wd):
        return np.ascontiguousarray(wd.reshape(NFU, 128, 8, 128).transpose(2, 1, 0, 3).reshape(8, 128, DFF))

    w["wA1"] = A(inp["ffn1_gate"][L], inp["ffn1_up"][L])
    w["wB1"] = B(inp["ffn1_down"][L])
    w["wA2"] = A(inp["ffn2_gate"][L], inp["ffn2_up"][L])
    w["wB2"] = B(inp["ffn2_down"][L])
    w["win"] = np.ascontiguousarray(inp["w_in"][L].reshape(8, 128, 32, 128).transpose(2, 1, 0, 3).reshape(32, 128, 1024))
    cat = np.concatenate([inp["decay_a"][L], inp["aaa_a"][L], inp["gate_a"][L]], 1)
    w["la"] = np.ascontiguousarray(cat.reshape(8, 128, 256).transpose(1, 0, 2).reshape(128, 2048))
    mw = inp["mu_wag"][L]
    mucat = np.concatenate([np.broadcast_to(mw[0][:, None], (1024, 64)), np.broadcast_to(mw[1][:, None], (1024, 64)),
                            np.broadcast_to(mw[2][:, None], (1024, 128))], 1)
    w["mula"] = np.ascontiguousarray(mucat.reshape(8, 128, 256).transpose(1, 0, 2).reshape(128, 2048))
    lb1 = np.concatenate([inp["decay_b"][L], inp["aaa_b"][L]], 0)
    w["lb"] = np.ascontiguousarray(np.concatenate([lb1, inp["gate_b"][L]], 1))
    w["poolw"] = np.ascontiguousarray(inp["pool_w"][L].transpose(1, 0, 2).reshape(128, 512))
    br = inp["w_branch_rwkv"][L].reshape(4, 128, 8, 128).transpose(2, 1, 0, 3)
    bp = inp["w_branch_pool"][L].reshape(4, 128, 8, 128).transpose(2, 1, 0, 3)
    w["wbr"] = np.ascontiguousarray(np.concatenate([br, bp], 2).reshape(8, 128, 1024))
    w["wo"] = np.ascontiguousarray(inp["w_out"][L].reshape(8, 128, 8, 128).transpose(2, 1, 0, 3).reshape(8, 128, 1024))
    v = np.zeros((128, NV_IN), np.float32)

    def put(col, vec):
        n = vec.shape[0] // 128
        v[:, col:col + n] = vec.reshape(n, 128).T

    for i in range(6):
        put(V_G + i * 8, inp["norm_gains"][L][i])
    for b in range(2):
        put(V_GB + b * 8, inp["gate_bias"][L][b])
    for i in range(3):
        put(V_MU + i * 4, inp["mu_rkv"][L][i])
    put(V_W0, inp["w0"][L])
    put(V_A0, inp["a0"][L])
    put(V_KK, inp["k_k"][L])
    put(V_KA, inp["k_a"][L])
    put(V_RK, inp["r_k"][L].reshape(512))
    put(V_LW, inp["ln_x_w"][L])
    put(V_LB, inp["ln_x_b"][L])
    put(V_PS, inp["pool_scale"][L])
    w["vecs"] = v
    return w


DRAM_IN = {
    "wA1": [NFU, 128, 2048], "wB1": [8, 128, DFF], "wA2": [NFU, 128, 2048], "wB2": [8, 128, DFF],
    "win": [32, 128, 1024], "la": [128, 2048], "mula": [128, 2048], "lb": [128, 1024], "poolw": [128, 512],
    "wbr": [8, 128, 1024], "wo": [8, 128, 1024], "vecs": [128, NV_IN],
    "ident": [128, 128], "blockones": [128, 128], "ones": [128, 128], "maskA": [64, 512], "maskNT": [64, 512],
    "identM": [64, 1024], "resetmask": [128, TT], "invc0": [128, 64],
}


class StopBuild(Exception):
    pass


def build(S, dbg=None, stop_after=None):
    NT = S // TT

    def chk(name):
        if stop_after == name:
            raise StopBuild()

    nc = bass.Bass("TRN2", target_bir_lowering=False)
    dr = {}
    dr["xin"] = nc.dram_tensor("xin", [NH, S, D], F32, kind="ExternalInput").ap()
    for name, shp in DRAM_IN.items():
        dr[name] = nc.dram_tensor(name, shp, F32, kind="ExternalInput").ap()
    dr["out"] = nc.dram_tensor("out", [NH, S, D], F32, kind="ExternalOutput").ap()
    dbg_out = {}
    if dbg:
        for name, shp in dbg.items():
            dbg_out[name] = nc.dram_tensor("dbg_" + name, shp, F32, kind="ExternalOutput").ap()

    NCHAN = 24
    with ExitStack() as es:
        P = Prog(nc, NCHAN)

        def sb(name, shape, dt):
            t = es.enter_context(nc.sbuf_tensor("sb_" + name, shape, dt))
            P.tracked.add("sb_" + name)
            return t

        xT = sb("xT", [128, NH, 8, TT], F32)
        hT = sb("hT", [128, NH, 8, 514], BF16)
        SCR = sb("SCR", [128, 76 * 256], F32)
        NSLOT = 4
        wring = [sb(f"wr{i}", [128, 3072], BF16) for i in range(NSLOT)]
        la_cur = sb("la_cur", [128, 2048], BF16)
        la_prev = sb("la_prev", [128, 2048], BF16)
        lb = sb("lb", [128, 1024], BF16)
        poolw = sb("poolw", [128, 512], BF16)
        ident = sb("ident", [128, 128], F32)
        identb = sb("identb", [128, 128], BF16)
        onesb = sb("onesb", [128, 128], BF16)
        bonesb = sb("bonesb", [128, 128], BF16)
        maskA = sb("maskA", [64, 512], F32)
        maskNT = sb("maskNT", [64, 512], F32)
        identM = sb("identM", [64, 1024], BF16)
        resetm = sb("resetm", [128, TT], F32)
        invc0 = sb("invc0", [128, 64], F32)
        vecs = sb("vecs", [128, NV], F32)
        xio = [sb(f"xio{i}", [128, D], F32) for i in range(3)]
        sq = [sb(f"sq{i}", [128, TT], BF16) for i in range(2)]
        sil = [sb(f"sil{i}", [128, TT], BF16) for i in range(2)]
        rstd = sb("rstd", [128, NH, TT], F32)
        rtmp = sb("rtmp", [128, TT], F32)
        lora1 = sb("lora1", [128, NH, 2, TT], BF16)
        hprev = sb("hprev", [128, NH, 8], BF16)
        pcar = sb("pcar", [128, NH, 12], F32)
        poolcar = sb("poolcar", [128, NH, 4, 16], F32)
        Z32 = sb("Z32", [128, NH, 4, 64], F32)
        Zb = sb("Zb", [128, NH, 4, 64], BF16)

        def scr(off_b, nbytes, dt, pattern=None, parts=128, **kw):
            assert off_b % 4 == 0 and nbytes % 4 == 0 and off_b + nbytes <= 76 * 1024
            a = SCR[0:parts, off_b // 4:(off_b + nbytes) // 4]
            if dt != F32:
                a = a.bitcast(dt)
            if pattern:
                a = a.rearrange(pattern, **kw)
            return a

        K = 1024
        hid = scr(0, 44 * K, BF16, "p (h u t) -> p h u t", h=NH, u=NFU)
        f32b = scr(44 * K, 32 * K, F32, "p (h k t) -> p h k t", h=NH, k=8)
        ybuf = scr(0, 8 * K, BF16, "p (h c t) -> p h c t", h=NH, c=4)
        ypool = scr(8 * K, 8 * K, BF16, "p (h c t) -> p h c t", h=NH, c=4)
        eP = scr(16 * K, 2 * K, F32)
        G32 = scr(18 * K, 2 * K, F32)
        bonv = scr(20 * K, 2 * K, F32)
        bk = scr(22 * K, 2 * K, BF16, "p (c two t) -> p c two t", two=2, t=64)
        ar = scr(24 * K, 2 * K, BF16, "p (c two t) -> p c two t", two=2, t=64)
        vT = scr(26 * K, 2 * K, BF16, "p (c x) -> p c x", x=128, parts=64)
        bT = scr(28 * K, 2 * K, BF16, "p (c x) -> p c x", x=128, parts=64)
        kT = scr(30 * K, 2 * K, BF16, "p (c x) -> p c x", x=128, parts=64)
        AM = scr(32 * K, 8 * K, BF16, "p (hd c q t) -> p hd c q t", hd=2, q=4, t=64, parts=64)
        Pst = [scr(40 * K + i * 2304, 2056, F32) for i in range(3)]
        r32 = scr(47 * K, 2 * K, F32)
        k32 = scr(49 * K, 2 * K, F32)
        sw = scr(51 * K, 2 * K, F32)
        a32 = scr(53 * K, 2 * K, F32)
        Lp = scr(55 * K, 2 * K, F32)
        eN = scr(57 * K, 2 * K, F32)
        ePm = scr(59 * K, 2 * K, F32)
        kkn = scr(61 * K, 2 * K, F32)
        kmod = scr(63 * K, 2 * K, F32)
        t1 = scr(65 * K, 2 * K, F32)
        t2 = scr(67 * K, 2 * K, F32)
        vb = scr(69 * K, 1 * K, BF16)
        sqk = scr(70 * K, 1 * K, BF16)
        rbb = scr(71 * K, 1 * K, BF16)
        PM = [scr(40 * K + i * 4 * K, 4 * K, BF16, "p (e two t) -> p e two t", two=2, t=64, parts=64) for i in range(2)]
        PkT = [scr(48 * K + i * 2 * K, 2 * K, BF16, "p (e t) -> p e t", t=64, parts=64) for i in range(2)]
        PV32 = scr(52 * K, 4 * K, F32, "p (e t) -> p e t", t=64, parts=64)
        Pb = scr(56 * K, 256, BF16, "p (hd t) -> p hd t", hd=2, parts=64)
        Ub = scr(56 * K + 256, 256, BF16, "p (hd t) -> p hd t", hd=2, parts=64)
        Tt = scr(56 * K + 512, 256, F32)
        Y1 = scr(57 * K, 2 * K, F32)
        Y32 = scr(59 * K, 2 * K, F32)
        gt1 = scr(61 * K, 2 * K, F32)
        gt2 = scr(63 * K, 2 * K, F32)
        gt3 = scr(65 * K, 2 * K, F32)
        ybf = scr(67 * K, 1 * K, BF16)
        ysq = scr(68 * K, 1 * K, BF16)
        PB = scr(40 * K, 2112, F32)
        PS1 = scr(43 * K, 2112, F32)
        PS2 = scr(46 * K, 2112, F32)
        pmix = scr(49 * K, 1 * K, BF16)
        mrg = scr(16 * K, 16 * K, BF16, "p (h k t) -> p h k t", h=NH, k=8)
        ms0 = scr(40 * K, 2 * K, F32)
        ms1 = scr(42 * K, 2 * K, F32)
        mm0 = scr(44 * K, 2 * K, F32)
        mm1 = scr(46 * K, 2 * K, F32)
        la_st = scr(0, 8 * K, F32)
        mula_st = scr(8 * K, 8 * K, F32)
        la_t = scr(16 * K, 8 * K, F32)

        ps = []
        for i in range(8):
            t = es.enter_context(nc.psum_tensor(f"ps{i}", [128, 512], F32))
            P.tracked.add(f"ps{i}")
            ps.append(t)

        sems = {e: es.enter_context(nc.semaphore("s_" + e)) for e in ENGS}
        dsems = [es.enter_context(nc.semaphore(f"d{i}")) for i in range(NCHAN)]
        block = es.enter_context(nc.Block())

        MUL, ADD, SUB, MAX = ALU.mult, ALU.add, ALU.subtract, ALU.max
        CH_W = list(range(0, NSLOT))
        CH_X = [NSLOT + i for i in range(3)]
        CH_MISC = NSLOT + 3
        CH_DBG = NSLOT + 4
        rot = {"evac": 0, "xio": 0}

        def vcol(c, n=1):
            return vecs[:, c:c + n]

        def evac_eng():
            rot["evac"] += 1
            return "act" if rot["evac"] % 2 else "dve"

        P.dma("sp", CH_MISC, ident[:, :], dr["ident"][:, :])
        P.dma("sp", CH_MISC, maskA[:, :], dr["maskA"][:, :])
        P.dma("sp", CH_MISC, maskNT[:, :], dr["maskNT"][:, :])
        P.dma("sp", CH_MISC, resetm[:, :], dr["resetmask"][:, :])
        P.dma("sp", CH_MISC, invc0[:, :], dr["invc0"][:, :])
        P.dma("sp", CH_MISC, vecs[:, 0:NV_IN], dr["vecs"][:, :])
        P.dma("sp", CH_MISC, la_st, dr["la"][:, :])
        P.dma("sp", CH_MISC, mula_st, dr["mula"][:, :])
        P.dma("pool", CH_MISC + 2, identb[:, :], dr["ident"][:, :], max_dma_last_dim=4096)
        P.dma("pool", CH_MISC + 2, onesb[:, :], dr["ones"][:, :], max_dma_last_dim=4096)
        P.dma("pool", CH_MISC + 2, bonesb[:, :], dr["blockones"][:, :], max_dma_last_dim=4096)
        P.dma("pool", CH_MISC + 2, identM[:, :], dr["identM"][:, :], max_dma_last_dim=4096)
        P.dma("pool", CH_MISC + 2, lb[:, :], dr["lb"][:, :], max_dma_last_dim=4096)
        P.dma("pool", CH_MISC + 2, poolw[:, :], dr["poolw"][:, :], max_dma_last_dim=4096)
        P.bump(CH_MISC)
        P.bump(CH_MISC + 2)
        P.ts(vcol(V_OMU, 12), vcol(V_MU, 12), -1.0, MUL, 1.0, ADD)
        P.ts(vcol(V_HG1, 8), vcol(V_G + 8, 8), 0.5, MUL)
        P.ts(vcol(V_HG5, 8), vcol(V_G + 40, 8), 0.5, MUL)
        P.tt(la_t, la_st, mula_st, MUL)
        P.copy(la_prev[:, :], la_t)
        P.tt(la_cur[:, :], la_st, la_t, SUB)
        P.memset(hprev[:, :, :], 0.0)
        P.memset(pcar[:, :, :], 0.0)
        P.memset(poolcar[:, :, :, :], 0.0)
        P.memset(Z32[:, :, :, :], 0.0)
        P.memset(Zb[:, :, :, :], 0.0)

        units = []
        for j in range(NT):
            for u in range(NFU):
                units.append((dr["wA1"][u, :, :], 2048))
            for d_ in range(8):
                units.append((dr["wB1"][d_, :, :], DFF))
            for c4 in range(4):
                units.append(("R", c4))
            for g in range(4):
                units.append((dr["win"][12 + g, :, :], 1024))
            for d_ in range(8):
                units.append(("M", d_))
            for d_ in range(8):
                units.append((dr["wo"][d_, :, :], 1024))
            for u in range(NFU):
                units.append((dr["wA2"][u, :, :], 2048))
            for d_ in range(8):
                units.append((dr["wB2"][d_, :, :], DFF))
        ws = {"issued": 0, "next": 0}

        def ws_issue(i):
            slot = wring[i % NSLOT]
            ch = CH_W[i % NSLOT]
            u = units[i]
            if u[0] == "R":
                c4 = u[1]
                for q in range(3):
                    P.dma("pool", ch, slot[:, q * 1024:(q + 1) * 1024], dr["win"][q * 4 + c4, :, :], max_dma_last_dim=4096)
                P.bump(ch)
            elif u[0] == "M":
                d_ = u[1]
                P.dma("pool", ch, slot[:, 0:1024], dr["win"][16 + d_, :, :], max_dma_last_dim=4096)
                P.dma("pool", ch, slot[:, 1024:2048], dr["win"][24 + d_, :, :], max_dma_last_dim=4096)
                P.dma("pool", ch, slot[:, 2048:3072], dr["wbr"][d_, :, :], max_dma_last_dim=4096)
                P.bump(ch)
            else:
                src, n = u
                P.dma("pool", ch, slot[:, 0:n], src, max_dma_last_dim=4096)

        def ws_get():
            i = ws["next"]
            ws["next"] += 1
            while ws["issued"] <= min(len(units) - 1, i + NSLOT - 1):
                ws_issue(ws["issued"])
                ws["issued"] += 1
            return wring[i % NSLOT]

        def dbg_dump(name, src_ap):
            if name in dbg_out:
                P.dma("sp", CH_DBG, dbg_out[name], src_ap)

        def rms_stats(src, h, psn):
            for k in range(8):
                s = sq[k % 2]
                P.act(s[:, :], src[:, h, k, :], AF.Square)
                P.mm(psn[:, :], onesb[:, :], s[:, :], start=(k == 0), stop=(k == 7))

        def rstd_from(psn, h, n, eps):
            P.act(rtmp[:, :], psn[:, :], AF.Sqrt, scale=1.0 / n, bias=eps)
            P.recip(rstd[:, h, :], rtmp[:, :])

        def norm_to_hT(gcol):
            for h in range(NH):
                psn = ps[6 + h]
                rms_stats(xT, h, psn)
                rstd_from(psn, h, D, RMS_EPS)
                for k in range(8):
                    P.stt(hT[:, h, k, 2:514], xT[:, h, k, :], vcol(gcol + k), rstd[:, h, :], MUL, MUL)

        def residual_update(hgcol):
            for h in range(NH):
                rstd_from(ps[6 + h], h, D, RMS_EPS)
            for h in range(NH):
                for k in range(8):
                    P.tt(f32b[:, h, k, :], f32b[:, h, k, :], rstd[:, h, :], MUL)
                    P.stt(xT[:, h, k, :], f32b[:, h, k, :], vcol(hgcol + k), xT[:, h, k, :], MUL, ADD)

        def out_proj_phase(nk, rhs_of):
            for dch in range(8):
                w = ws_get()
                for h in range(NH):
                    pso = ps[4 + (dch * NH + h) % 2]
                    for k in range(nk):
                        P.mm(pso[:, :], w[:, k * 128:(k + 1) * 128], rhs_of(h, k), start=(k == 0), stop=(k == nk - 1))
                    P.act(f32b[:, h, dch, :], pso[:, :], AF.Copy)
                    s = sq[(dch * NH + h) % 2]
                    P.act(s[:, :], pso[:, :], AF.Square)
                    P.mm(ps[6 + h][:, :], onesb[:, :], s[:, :], start=(dch == 0), stop=(dch == 7))

        def ffn(gcol_in, hgcol_out):
            norm_to_hT(gcol_in)
            chk("ffn_norm")
            for u in range(NFU):
                if u == 1:
                    chk("ffnA0")
                w = ws_get()
                for h in range(NH):
                    i = (u * NH + h) % 2
                    psg, psu = ps[2 * i], ps[2 * i + 1]
                    for k in range(8):
                        P.mm(psg[:, :], w[:, k * 128:(k + 1) * 128], hT[:, h, k, 2:514], start=(k == 0), stop=(k == 7))
                    for k in range(8):
                        P.mm(psu[:, :], w[:, 1024 + k * 128:1024 + (k + 1) * 128], hT[:, h, k, 2:514], start=(k == 0), stop=(k == 7))
                    P.act(sil[i][:, :], psg[:, :], AF.Silu)
                    P.tt(hid[:, h, u, :], psu[:, :], sil[i][:, :], MUL)
            chk("ffnA")
            out_proj_phase(NFU, lambda h, k: hid[:, h, k, :])
            chk("ffnB")
            residual_update(hgcol_out)
            chk("ffn")

        def wkv_unit(j, h, c4, w):
            pA, pB = ps[0], ps[1]
            Zs32 = Z32[:, h, c4, :]
            Zsb = Zb[:, h, c4, :]
            dst = [r32, k32, None]
            for i in range(3):
                pp = ps[i % 2]
                for k in range(8):
                    P.mm(pp[:, :], w[:, i * 1024 + k * 128:i * 1024 + (k + 1) * 128], hT[:, h, k, 2:514], start=(k == 0), stop=(k == 7))
                st = Pst[i]
                P.copy(st[:, 0:1], pcar[:, h, c4 * 3 + i:c4 * 3 + i + 1], eng="dve")
                P.act(st[:, 1:513], pp[:, :], AF.Copy)
                P.copy(pcar[:, h, c4 * 3 + i:c4 * 3 + i + 1], st[:, 512:513], eng="dve")
                P.ts(t1, st[:, 0:512], vcol(V_MU + i * 4 + c4), MUL)
                o = dst[i] if dst[i] is not None else vb
                P.stt(o, st[:, 1:513], vcol(V_OMU + i * 4 + c4), t1, MUL, ADD)
            chk("wkv_proj")
            cs = slice(c4 * 128, (c4 + 1) * 128)
            P.mm(pA[:, :], lb[0:64, cs], lora1[0:64, h, 0, :])
            P.act(sw, pA[:, :], AF.Sigmoid, bias=vcol(V_W0 + c4))
            P.mm(pB[:, :], lb[64:128, cs], lora1[64:128, h, 0, :])
            P.act(a32, pB[:, :], AF.Sigmoid, bias=vcol(V_A0 + c4))
            P.mm(pA[:, :], lb[:, 512 + c4 * 128:512 + (c4 + 1) * 128], lora1[:, h, 1, :])
            P.act(G32, pA[:, :], AF.Copy)
            chk("wkv_lorab")
            P.scan(Lp, resetm[:, :], sw, 0.0, MUL, ADD)
            P.tt(t2, Lp, sw, SUB)
            P.act(eP, Lp, AF.Exp, scale=-C0)
            P.act(eN, Lp, AF.Exp, scale=C0)
            P.act(ePm, t2, AF.Exp, scale=-C0)
            P.act(sqk, k32, AF.Square, scale=vcol(V_KK + c4))
            P.mm(pB[:, :], bonesb[:, :], sqk)
            P.act(t1, pB[:, :], AF.Sqrt)
            P.ts(t1, t1, 1e-12, MAX)
            P.recip(t1, t1)
            P.stt(kkn, k32, vcol(V_KK + c4), t1, MUL, MUL)
            P.ts(t2, a32, -1.0, ADD, vcol(V_KA + c4), MUL)
            P.stt(kmod, t2, 1.0, k32, ADD, MUL)
            bk_b = bk[:, :, 0, :]
            bk_k = bk[:, :, 1, :]
            ar_a = ar[:, :, 0, :]
            ar_r = ar[:, :, 1, :]
            v3 = lambda a: a.rearrange("p (c t) -> p c t", t=64)
            P.tt(bk_k, v3(kmod), v3(eN), MUL)
            P.tt(t2, kkn, a32, MUL)
            P.tt(bk_b, v3(t2), v3(eN), MUL)
            P.stt(ar_a, v3(kkn), -1.0, v3(ePm), MUL, MUL)
            P.tt(ar_r, v3(r32), v3(eP), MUL)
            P.stt(rbb, r32, vcol(V_RK + c4), kmod, MUL, MUL)
            P.mm(pA[:, :], bonesb[:, :], rbb)
            P.tt(bonv, pA[:, :], vb, MUL)
            chk("wkv_prep")
            for (src_of, dstT, pst) in ((lambda c: vb[:, c * 64:(c + 1) * 64], vT, ps[2]),
                                        (lambda c: bk[:, c, 0, :], bT, ps[3]),
                                        (lambda c: bk[:, c, 1, :], kT, ps[2])):
                pv = pst[0:64, :].bitcast(BF16).rearrange("p (c x) -> p c x", x=128)
                for c in range(8):
                    P.tr(pv[:, c, :], src_of(c), identb[:, :], signal=(c == 7))
                P.copy(dstT[:, :, :], pv[:, :, :], eng=evac_eng())
            chk("wkv_tr")
            for cp in range(4):
                for hd in range(2):
                    pb = hd * 64
                    pa = ps[2 + hd + 2 * (cp % 2)][0:64, :].rearrange("p (cc x) -> p cc x", cc=2)
                    for cc in range(2):
                        c = cp * 2 + cc
                        rhs = ar[pb:pb + 64, c, :, :].rearrange("p two t -> p (two t)")
                        P.mm(pa[:, cc, 0:128], bk[pb:pb + 64, c, 0, :], rhs, signal=False)
                        P.mm(pa[:, cc, 128:256], bk[pb:pb + 64, c, 1, :], rhs, signal=(cc == 1))
                    P.tt(AM[:, hd, cp * 2:cp * 2 + 2, :, :].rearrange("p c q t -> p (c q t)"),
                         ps[2 + hd + 2 * (cp % 2)][0:64, :], maskA[:, :], MUL)
            pnt = [ps[6][0:64, :].rearrange("p (e t) -> p e t", t=64), ps[7][0:64, :].rearrange("p (e t) -> p e t", t=64)]
            for c in range(8):
                for hd in range(2):
                    pb = hd * 64
                    P.mm(pnt[hd][:, c, :], ar[pb:pb + 64, c, 0, :], bk[pb:pb + 64, c, 0, :], signal=(c == 7))
            for hd in range(2):
                P.tt(PkT[0][:, hd * 8:(hd + 1) * 8, :].rearrange("p e t -> p (e t)"), ps[6 + hd][0:64, :], maskNT[:, :], MUL)
            chk("wkv_A")
            Nview = AM[:, :, :, 0, :].rearrange("p hd c t -> p (hd c) t")
            P.copy(PM[0][:, :, 0, :], Nview, eng="act")
            P.tt(PM[0][:, :, 1, :], Nview, identM[:, :].rearrange("p (e t) -> p e t", t=64), ADD)
            ppv = [ps[2][0:64, :].rearrange("p (e t) -> p e t", t=64), ps[3][0:64, :].rearrange("p (e t) -> p e t", t=64)]
            for hd in range(2):
                for c in range(8):
                    P.mm(ppv[hd][:, c, :], AM[:, hd, c, 2, :], vT[:, c, hd * 64:(hd + 1) * 64], signal=(c == 7))
            P.copy(PV32[:, 0:8, :], ppv[0], eng="act")
            P.copy(PV32[:, 8:16, :], ppv[1], eng="dve")
            chk("wkv_pv")
            cur = 0
            for lvl in range(6):
                nxt = 1 - cur
                for sbi in range(2):
                    es_ = range(sbi * 8, sbi * 8 + 8)
                    bq = 2 + 3 * sbi
                    p1 = [ps[bq + 0][0:64, :].rearrange("p (e x) -> p e x", x=128),
                          ps[bq + 1][0:64, :].rearrange("p (e x) -> p e x", x=128)]
                    p2 = ps[bq + 2][0:64, :].rearrange("p (e t) -> p e t", t=64)
                    sl8 = slice(sbi * 8, sbi * 8 + 8)
                    if lvl == 0:
                        for i, e in enumerate(es_):
                            P.mm(p1[i // 4][:, i % 4, 0:64], PkT[cur][:, e, :], PM[cur][:, e, 0, :], signal=(i == 7))
                        for i, e in enumerate(es_):
                            P.mm(p2[:, i, :], PM[cur][:, e, 0, :], PkT[cur][:, e, :], signal=(i == 7))
                        for q in range(2):
                            P.copy(PM[nxt][:, sbi * 8 + q * 4:sbi * 8 + q * 4 + 4, 0, :], p1[q][:, :, 0:64], eng=evac_eng())
                        P.copy(PM[nxt][:, sl8, 1, :], PM[cur][:, sl8, 1, :], eng="dve")
                        P.copy(PkT[nxt][:, sl8, :], p2, eng="act")
                    elif lvl < 5:
                        for i, e in enumerate(es_):
                            P.mm(p1[i // 4][:, i % 4, :], PkT[cur][:, e, :], PM[cur][:, e, :, :].rearrange("p two t -> p (two t)"), signal=(i == 7))
                        for i, e in enumerate(es_):
                            P.mm(p2[:, i, :], PM[cur][:, e, 0, :], PkT[cur][:, e, :], signal=(i == 7))
                        for q in range(2):
                            sl = slice(sbi * 8 + q * 4, sbi * 8 + q * 4 + 4)
                            P.copy(PM[nxt][:, sl, 0, :], p1[q][:, :, 0:64], eng="act")
                            P.tt(PM[nxt][:, sl, 1, :], p1[q][:, :, 64:128], PM[cur][:, sl, 1, :], ADD)
                        P.copy(PkT[nxt][:, sl8, :], p2, eng="act")
                    else:
                        for i, e in enumerate(es_):
                            P.mm(p2[:, i, :], PkT[cur][:, e, :], PM[cur][:, e, 1, :], signal=(i == 7))
                        P.tt(PM[nxt][:, sl8, 1, :], p2, PM[cur][:, sl8, 1, :], ADD)
                cur = nxt
            Mf = PM[cur]
            chk("wkv_inv")
            psc = [ps[2][0:64, 0:64], ps[3][0:64, 0:64]]
            psc2 = ps[4][0:64, 0:128].rearrange("p (hd t) -> p hd t", hd=2)
            psz = ps[5]
            psya = [ps[6], ps[7]]
            psyb = ps[1]
            for c in range(8):
                for hd in range(2):
                    pb = hd * 64
                    P.mm(psc[hd], ar[pb:pb + 64, c, 0, :], Zsb[pb:pb + 64, :], signal=(hd == 1))
                for hd in range(2):
                    pb = hd * 64
                    P.mm(psya[hd][pb:pb + 64, c * 64:(c + 1) * 64], Zsb[pb:pb + 64, :], ar[pb:pb + 64, c, 1, :], signal=(hd == 1))
                for hd in range(2):
                    P.tt(Pb[:, hd, :], psc[hd], PV32[:, hd * 8 + c, :], ADD)
                for hd in range(2):
                    P.mm(psc2[:, hd, :], Mf[:, hd * 8 + c, 1, :], Pb[:, hd, :], signal=(hd == 1))
                P.copy(Ub[:, :, :], psc2, eng="act")
                for hd in range(2):
                    pb = hd * 64
                    P.mm(psz[pb:pb + 64, 0:64], bT[:, c, pb:pb + 64], Ub[:, hd, :], start=True, stop=False, signal=False)
                    P.mm(psz[pb:pb + 64, 0:64], kT[:, c, pb:pb + 64], vT[:, c, pb:pb + 64], start=False, stop=True, signal=(hd == 1))
                for hd in range(2):
                    pb = hd * 64
                    P.mm(psyb[pb:pb + 64, c * 64:(c + 1) * 64], Ub[:, hd, :], AM[:, hd, c, 1, :], start=True, stop=False, signal=False)
                    P.mm(psyb[pb:pb + 64, c * 64:(c + 1) * 64], vT[:, c, pb:pb + 64], AM[:, hd, c, 3, :], start=False, stop=True, signal=(hd == 1))
                wc = eP[:, c * 64 + 63:c * 64 + 64]
                P.tt(Tt, psz[:, 0:64], Zs32, ADD)
                P.act(Zsb, Tt, AF.Copy, scale=wc)
                P.ts(Zs32, Tt, wc, MUL)
            chk("wkv_chain")
            for hd in range(2):
                pb = hd * 64
                P.act(Y1[pb:pb + 64, :], psya[hd][pb:pb + 64, :], AF.Copy)
            P.tt(Y32, psyb[:, :], Y1, ADD)
            P.act(ybf, Y32, AF.Copy)
            P.act(ysq, Y32, AF.Square)
            P.mm(pA[:, :], bonesb[:, :], ybf)
            P.mm(pB[:, :], bonesb[:, :], ysq)
            P.act(gt1, pA[:, :], AF.Copy, scale=1.0 / 64)
            P.tt(gt2, gt1, gt1, MUL)
            P.stt(gt2, pB[:, :], 1.0 / 64, gt2, MUL, SUB)
            P.ts(gt2, gt2, 0.0, MAX, GN_EPS, ADD)
            P.act(gt2, gt2, AF.Sqrt)
            P.recip(gt2, gt2)
            P.tt(gt3, Y32, gt1, SUB)
            P.tt(gt3, gt3, gt2, MUL)
            P.ts(gt3, gt3, vcol(V_LW + c4), MUL, vcol(V_LB + c4), ADD)
            P.tt(gt3, gt3, bonv, ADD)
            P.tt(ybuf[:, h, c4, :], gt3, G32, MUL)

        def mixer(j):
            norm_to_hT(V_G + 16)
            for h in range(NH):
                P.copy(hT[:, h, :, 1:2], hprev[:, h, :].unsqueeze(2), eng="dve")
                P.copy(hprev[:, h, :].unsqueeze(2), hT[:, h, :, 513:514], eng="dve")
            for h in range(NH):
                for part in range(2):
                    pp = ps[part]
                    cs0 = part * 128
                    n = 0
                    for k in range(8):
                        P.mm(pp[:, :], la_cur[:, k * 256 + cs0:k * 256 + cs0 + 128], hT[:, h, k, 2:514], start=(n == 0), stop=False)
                        n += 1
                        P.mm(pp[:, :], la_prev[:, k * 256 + cs0:k * 256 + cs0 + 128], hT[:, h, k, 1:513], start=False, stop=(k == 7))
                    if part == 0:
                        P.act(lora1[0:64, h, 0, :], pp[0:64, :], AF.Tanh)
                        P.act(lora1[64:128, h, 0, :], pp[64:128, :], AF.Copy)
                    else:
                        P.act(lora1[:, h, 1, :], pp[:, :], AF.Sigmoid)
            chk("lora_a")
            for c4 in range(4):
                w = ws_get()
                for h in range(NH):
                    wkv_unit(j, h, c4, w)
            chk("wkv")
            for g, wd in enumerate((2, 4, 8, 16)):
                w = ws_get()
                nlev = g + 1
                for h in range(NH):
                    pp = ps[h % 2]
                    for k in range(8):
                        P.mm(pp[:, :], w[:, k * 128:(k + 1) * 128], hT[:, h, k, 2:514], start=(k == 0), stop=(k == 7))
                    P.copy(PB[:, 0:16], poolcar[:, h, g, :], eng="dve")
                    P.act(PB[:, 16:528], pp[:, :], AF.Copy)
                    P.copy(poolcar[:, h, g, :], PB[:, 512:528], eng="dve")
                    src, lo = PB, 0
                    bufs = [PS1, PS2]
                    for lv in range(nlev):
                        sh = 1 << lv
                        dstb = bufs[lv % 2]
                        nlo = lo + sh
                        P.tt(dstb[:, nlo:528], src[:, nlo:528], src[:, nlo - sh:528 - sh], ADD)
                        src, lo = dstb, nlo
                    P.stt(pmix, src[:, 16:528], 1.0 / wd, PB[:, 16:528], MUL, SUB)
                    if j == 0:
                        P.tt(t1[:, 0:16], src[:, 16:32], invc0[:, g * 16:(g + 1) * 16], MUL)
                        P.tt(pmix[:, 0:16], t1[:, 0:16], PB[:, 16:32], SUB)
                    pq = ps[2 + h % 2]
                    P.mm(pq[:, :], poolw[:, g * 128:(g + 1) * 128], pmix)
                    P.act(ypool[:, h, g, :], pq[:, :], AF.Copy, scale=vcol(V_PS + g))
            chk("pool")
            for dch in range(8):
                w = ws_get()
                for h in range(NH):
                    b0 = (h % 2) * 4
                    pg0, pg1, pbr, pbp = ps[b0], ps[b0 + 1], ps[b0 + 2], ps[b0 + 3]
                    for k in range(8):
                        P.mm(pg0[:, :], w[:, k * 128:(k + 1) * 128], hT[:, h, k, 2:514], start=(k == 0), stop=(k == 7))
                    for k in range(8):
                        P.mm(pg1[:, :], w[:, 1024 + k * 128:1024 + (k + 1) * 128], hT[:, h, k, 2:514], start=(k == 0), stop=(k == 7))
                    for k in range(4):
                        P.mm(pbr[:, :], w[:, 2048 + k * 128:2048 + (k + 1) * 128], ybuf[:, h, k, :], start=(k == 0), stop=(k == 3))
                    for k in range(4):
                        P.mm(pbp[:, :], w[:, 2560 + k * 128:2560 + (k + 1) * 128], ypool[:, h, k, :], start=(k == 0), stop=(k == 3))
                    P.act(ms0, pg0[:, :], AF.Sigmoid, bias=vcol(V_GB + dch))
                    P.act(ms1, pg1[:, :], AF.Sigmoid, bias=vcol(V_GB + 8 + dch))
                    P.tt(mm0, pbr[:, :], ms0, MUL)
                    P.tt(mm1, pbp[:, :], ms1, MUL)
                    P.tt(mrg[:, h, dch, :], mm0, mm1, ADD)
            chk("merge")
            out_proj_phase(8, lambda h, k: mrg[:, h, k, :])
            residual_update(V_G + 24)
            chk("mixer")

        def main_loop():
          for j in range(NT):
            for h in range(NH):
                for tb in range(4):
                    si = rot["xio"] % 3
                    rot["xio"] += 1
                    xs = xio[si]
                    P.dma("sp", CH_X[si], xs[:, :], dr["xin"][h, j * TT + tb * 128:j * TT + (tb + 1) * 128, :])
                    for half in range(2):
                        pst = ps[(tb * 2 + half) % 4]
                        for kk in range(4):
                            k = half * 4 + kk
                            P.tr(pst[:, kk * 128:(kk + 1) * 128], xs[:, k * 128:(k + 1) * 128], ident[:, :], signal=(kk == 3))
                        P.copy(xT[:, h, half * 4:half * 4 + 4, tb * 128:(tb + 1) * 128],
                               pst[:, :].rearrange("p (k t) -> p k t", t=128), eng=evac_eng())
            chk("load")
            ffn(V_G + 0, V_HG1)
            if j == 0:
                dbg_dump("x1", xT[:, :, :, :])
            mixer(j)
            if j == 0:
                dbg_dump("ybuf", ybuf)
                dbg_dump("x2", xT[:, :, :, :])
            ffn(V_G + 32, V_HG5)
            for h in range(NH):
                for tb in range(4):
                    si = rot["xio"] % 3
                    rot["xio"] += 1
                    xs = xio[si]
                    for half in range(2):
                        pst = ps[(tb * 2 + half) % 4]
                        for kk in range(4):
                            k = half * 4 + kk
                            P.tr(pst[:, kk * 128:(kk + 1) * 128], xT[:, h, k, tb * 128:(tb + 1) * 128], ident[:, :], signal=(kk == 3))
                        P.copy(xs[:, half * 512:(half + 1) * 512], pst[:, :], eng=evac_eng())
                    P.dma("sp", CH_X[si], dr["out"][h, j * TT + tb * 128:j * TT + (tb + 1) * 128, :], xs[:, :])
        try:
            main_loop()
            assert ws["next"] == len(units), (ws["next"], len(units))
        except StopBuild:
            print("[kernel] build stopped after", stop_after, flush=True)
        P.finish("sp")
        P.emit(block, sems, dsems)
        print(f"[kernel] S={S} ops={P.nops} per-engine={ {e: len(P.ops[e]) for e in ENGS} }", flush=True)
    return nc


_CACHE = {}


def run(inputs, S, ncores, dbg=None, stop_after=None):
    x = np.asarray(inputs["x"], np.float32)
    inp = {k: np.asarray(v, np.float32) for k, v in inputs.items() if k != "x"}
    shared = host_weights(inp)
    shared.update(host_consts())
    key = (S, tuple(sorted(dbg.items())) if dbg else None)
    nc = build(S, dbg, stop_after)
    in_maps = []
    for c in range(ncores):
        m = dict(shared)
        m["xin"] = np.ascontiguousarray(x[c * NH:(c + 1) * NH, :S])
        in_maps.append(m)
    res = run_bass_kernel_spmd(nc, in_maps, core_ids=list(range(ncores)))
    out = np.concatenate([r["out"] for r in res.results], 0)
    return out, res


def kernel(**inputs):
    out, _ = run(inputs, 2048, NCORES)
    return out.astype(np.float32)
```

```python
import numpy as np
from contextlib import ExitStack
import concourse.bass as bass
import concourse.mybir as mybir
from concourse.bass_utils import run_bass_kernel_spmd

F32 = mybir.dt.float32
BF16 = mybir.dt.bfloat16
AF = mybir.ActivationFunctionType
ALU = mybir.AluOpType
ESZ = {F32: 4, BF16: 2}

ENGS = ("pe", "act", "dve", "pool", "sp")
GRAN = 256
NCORES = 8
NH = 2
TT = 512
D = 1024
DFF = 2816
NFU = 22
CH = 64
C0 = float(np.exp(-0.5))
RMS_EPS = 1e-6
GN_EPS = 64e-5


class Prog:
    def __init__(self, nc, n_dma_chan):
        self.nc = nc
        self.ops = {e: [] for e in ENGS}
        self.cnt = {e: 0 for e in ENGS}
        self.pending = {e: False for e in ENGS}
        self.last_w = {}
        self.readers = {}
        self.water = {e: {} for e in ENGS}
        self.dcnt = [0] * n_dma_chan
        self.n_dma_chan = n_dma_chan
        self.tracked = set()
        self.nops = 0

    def keys(self, ap):
        name = ap.tensor.name
        if name not in self.tracked:
            return ()
        esz = ESZ[ap.dtype]
        pat = ap.ap
        ps = pat[0][0]
        off = ap.offset % ps if ps > 0 else ap.offset
        span = 1
        for st, n in pat[1:]:
            span += (n - 1) * abs(st)
        lo = off * esz
        hi = (off + span) * esz
        return [(name, g) for g in range(lo // GRAN, (hi - 1) // GRAN + 1)]

    def _collect(self, eng, rkeys, wkeys):
        deps = {}

        def add(d, raw):
            k, v, pe = d
            if pe == eng and not raw:
                return
            if deps.get(k, 0) < v:
                deps[k] = v

        lw = self.last_w
        for key in rkeys:
            d = lw.get(key)
            if d is not None:
                add(d, True)
        for key in wkeys:
            d = lw.get(key)
            if d is not None:
                add(d, False)
            for d in self.readers.get(key, ()):
                add(d, False)
        out = []
        wm = self.water[eng]
        for k, v in deps.items():
            if wm.get(k, 0) < v:
                wm[k] = v
                out.append((k, v))
        return out

    def _record(self, dep, rkeys, wkeys):
        for key in rkeys:
            lst = self.readers.setdefault(key, [])
            for i, d in enumerate(lst):
                if d[0] == dep[0]:
                    lst[i] = dep
                    break
            else:
                lst.append(dep)
        for key in wkeys:
            self.last_w[key] = dep
            self.readers[key] = []

    def _rw(self, reads, writes):
        rk = []
        for a in reads:
            rk.extend(self.keys(a))
        wk = []
        for a in writes:
            wk.extend(self.keys(a))
        return rk, wk

    def op(self, eng, fn, reads, writes, signal=True):
        rk, wk = self._rw(reads, writes)
        waits = self._collect(eng, rk, wk)
        if signal:
            self.cnt[eng] += 1
            dep = (eng, self.cnt[eng], eng)
            self.pending[eng] = False
        else:
            dep = (eng, self.cnt[eng] + 1, eng)
            self.pending[eng] = True
        self._record(dep, rk, wk)
        self.ops[eng].append((waits, fn, "e" if signal else None))
        self.nops += 1

    def dma(self, eng, chan, out, in_, **kw):
        rk, wk = self._rw([in_], [out])
        waits = self._collect(eng, rk, wk)
        self.dcnt[chan] += 16
        dep = (("dma", chan), self.dcnt[chan], "dma")
        self._record(dep, rk, wk)
        self.ops[eng].append((waits, lambda e: e.dma_start(out=out, in_=in_, **kw), ("dma", chan)))
        self.nops += 1

    def bump(self, chan):
        k = ("dma", chan)
        full = (k, self.dcnt[chan], "dma")
        for key, dep in self.last_w.items():
            if dep[0] == k:
                self.last_w[key] = full

    def finish(self, eng="sp"):
        waits = []
        for e in ENGS:
            if e != eng and self.cnt[e] > 0:
                waits.append((e, self.cnt[e]))
        for c in range(self.n_dma_chan):
            if self.dcnt[c] > 0:
                waits.append((("dma", c), self.dcnt[c]))
        self.ops[eng].append((waits, None, None))

    def emit(self, block, sems, dsems):
        for e in ENGS:
            assert not self.pending[e], f"unsignalled tail on {e}"

        def semof(k):
            return dsems[k[1]] if isinstance(k, tuple) else sems[k]

        def run(name, engine):
            sem = sems[name]
            for waits, fn, inc in self.ops[name]:
                for k, v in waits:
                    engine.wait_ge(semof(k), v)
                if fn is None:
                    continue
                ins = fn(engine)
                if inc is None:
                    continue
                if inc == "e":
                    ins.then_inc(sem, 1)
                else:
                    ins.then_inc(dsems[inc[1]], 16)

        block.tensor(lambda e: run("pe", e))
        block.scalar(lambda e: run("act", e))
        block.vector(lambda e: run("dve", e))
        block.gpsimd(lambda e: run("pool", e))
        block.sync(lambda e: run("sp", e))

    def mm(self, out, lhsT, rhs, start=True, stop=True, signal=True):
        self.op("pe", lambda e: e.matmul(out, lhsT=lhsT, rhs=rhs, start=start, stop=stop),
                [lhsT, rhs], [out], signal)

    def tr(self, out, in_, ident, signal=True):
        self.op("pe", lambda e: e.transpose(out, in_, ident), [in_, ident], [out], signal)

    def act(self, out, in_, func, bias=None, scale=None, eng="act"):
        reads = [in_]
        kw = {}
        if bias is not None:
            kw["bias"] = bias
            if not isinstance(bias, (int, float)):
                reads.append(bias)
        if scale is not None:
            kw["scale"] = scale
            if not isinstance(scale, (int, float)):
                reads.append(scale)
        self.op(eng, lambda e: e.activation(out=out, in_=in_, func=func, **kw), reads, [out])

    def tt(self, out, in0, in1, op, eng="dve"):
        self.op(eng, lambda e: e.tensor_tensor(out=out, in0=in0, in1=in1, op=op), [in0, in1], [out])

    def ts(self, out, in0, s1, op0, s2=None, op1=None, eng="dve"):
        reads = [in0]
        for s in (s1, s2):
            if s is not None and not isinstance(s, (int, float)):
                reads.append(s)
        if op1 is None:
            fn = lambda e: e.tensor_scalar(out=out, in0=in0, scalar1=s1, scalar2=None, op0=op0)
        else:
            fn = lambda e: e.tensor_scalar(out=out, in0=in0, scalar1=s1, scalar2=s2, op0=op0, op1=op1)
        self.op(eng, fn, reads, [out])

    def stt(self, out, in0, scalar, in1, op0, op1):
        reads = [in0, in1]
        if not isinstance(scalar, (int, float)):
            reads.append(scalar)
        self.op("dve", lambda e: e.scalar_tensor_tensor(out=out, in0=in0, scalar=scalar, in1=in1, op0=op0, op1=op1),
                reads, [out])

    def copy(self, out, in_, eng="dve"):
        if eng == "act":
            self.act(out, in_, AF.Copy)
        else:
            self.op(eng, lambda e: e.tensor_copy(out=out, in_=in_), [in_], [out])

    def scan(self, out, d0, d1, init, op0, op1):
        self.op("dve", lambda e: e.tensor_tensor_scan(out=out, data0=d0, data1=d1, initial=init, op0=op0, op1=op1),
                [d0, d1], [out])

    def recip(self, out, in_):
        self.op("dve", lambda e: e.reciprocal(out=out, in_=in_), [in_], [out])

    def memset(self, ap, val, eng="dve"):
        self.op(eng, lambda e: e.memset(ap, val), [], [ap])


V_G = 0
V_GB = 48
V_MU = 64
V_W0 = 76
V_A0 = 80
V_KK = 84
V_KA = 88
V_RK = 92
V_LW = 96
V_LB = 100
V_PS = 104
V_OMU = 108
V_HG1 = 120
V_HG5 = 128
NV = 136
NV_IN = 108


def host_consts():
    c = {}
    c["ident"] = np.eye(128, dtype=np.float32)
    bo = np.zeros((128, 128), np.float32)
    bo[:64, :64] = 1.0
    bo[64:, 64:] = 1.0
    c["blockones"] = bo
    c["ones"] = np.ones((128, 128), np.float32)
    s = np.arange(64)[:, None]
    t = np.arange(64)[None, :]
    strict = (s < t).astype(np.float32)
    incl = (s <= t).astype(np.float32)
    m4 = np.concatenate([strict, incl, strict, incl], 1)
    c["maskA"] = np.tile(m4, (1, 2)).copy()
    c["maskNT"] = np.tile(strict.T, (1, 8)).copy()
    c["identM"] = np.tile(np.eye(64, dtype=np.float32), (1, 16)).copy()
    rm = np.ones((128, TT), np.float32)
    rm[:, ::CH] = 0.0
    c["resetmask"] = rm
    ic = np.zeros((128, 4, 16), np.float32)
    for g, w in enumerate((2, 4, 8, 16)):
        ic[:, g, :] = 1.0 / np.minimum(np.arange(1, 17), w)
    c["invc0"] = ic.reshape(128, 64)
    return c


def host_weights(inp):
    L = 0
    w = {}

    def A(wg, wu):
        g = wg.reshape(8, 128, NFU, 128).transpose(2, 1, 0, 3)
        u = wu.reshape(8, 128, NFU, 128).transpose(2, 1, 0, 3)
        return np.ascontiguousarray(np.stack([g, u], 2).reshape(NFU, 128, 2048))

    def B(wd):
        return np.ascontiguousarray(wd.reshape(NFU, 128, 8, 128).transpose(2, 1, 0, 3).reshape(8, 128, DFF))

    w["wA1"] = A(inp["ffn1_gate"][L], inp["ffn1_up"][L])
    w["wB1"] = B(inp["ffn1_down"][L])
    w["wA2"] = A(inp["ffn2_gate"][L], inp["ffn2_up"][L])
    w["wB2"] = B(inp["ffn2_down"][L])
    w["win"] = np.ascontiguousarray(inp["w_in"][L].reshape(8, 128, 32, 128).transpose(2, 1, 0, 3).reshape(32, 128, 1024))
    cat = np.concatenate([inp["decay_a"][L], inp["aaa_a"][L], inp["gate_a"][L]], 1)
    w["la"] = np.ascontiguousarray(cat.reshape(8, 128, 256).transpose(1, 0, 2).reshape(128, 2048))
    mw = inp["mu_wag"][L]
    mucat = np.concatenate([np.broadcast_to(mw[0][:, None], (1024, 64)), np.broadcast_to(mw[1][:, None], (1024, 64)),
                            np.broadcast_to(mw[2][:, None], (1024, 128))], 1)
    w["mula"] = np.ascontiguousarray(mucat.reshape(8, 128, 256).transpose(1, 0, 2).reshape(128, 2048))
    lb1 = np.concatenate([inp["decay_b"][L], inp["aaa_b"][L]], 0)
    w["lb"] = np.ascontiguousarray(np.concatenate([lb1, inp["gate_b"][L]], 1))
    w["poolw"] = np.ascontiguousarray(inp["pool_w"][L].transpose(1, 0, 2).reshape(128, 512))
    br = inp["w_branch_rwkv"][L].reshape(4, 128, 8, 128).transpose(2, 1, 0, 3)
    bp = inp["w_branch_pool"][L].reshape(4, 128, 8, 128).transpose(2, 1, 0, 3)
    w["wbr"] = np.ascontiguousarray(np.concatenate([br, bp], 2).reshape(8, 128, 1024))
    w["wo"] = np.ascontiguousarray(inp["w_out"][L].reshape(8, 128, 8, 128).transpose(2, 1, 0, 3).reshape(8, 128, 1024))
    v = np.zeros((128, NV_IN), np.float32)

    def put(col, vec):
        n = vec.shape[0] // 128
        v[:, col:col + n] = vec.reshape(n, 128).T

    for i in range(6):
        put(V_G + i * 8, inp["norm_gains"][L][i])
    for b in range(2):
        put(V_GB + b * 8, inp["gate_bias"][L][b])
    for i in range(3):
        put(V_MU + i * 4, inp["mu_rkv"][L][i])
    put(V_W0, inp["w0"][L])
    put(V_A0, inp["a0"][L])
    put(V_KK, inp["k_k"][L])
    put(V_KA, inp["k_a"][L])
    put(V_RK, inp["r_k"][L].reshape(512))
    put(V_LW, inp["ln_x_w"][L])
    put(V_LB, inp["ln_x_b"][L])
    put(V_PS, inp["pool_scale"][L])
    w["vecs"] = v
    return w


DRAM_IN = {
    "wA1": [NFU, 128, 2048], "wB1": [8, 128, DFF], "wA2": [NFU, 128, 2048], "wB2": [8, 128, DFF],
    "win": [32, 128, 1024], "la": [128, 2048], "mula": [128, 2048], "lb": [128, 1024], "poolw": [128, 512],
    "wbr": [8, 128, 1024], "wo": [8, 128, 1024], "vecs": [128, NV_IN],
    "ident": [128, 128], "blockones": [128, 128], "ones": [128, 128], "maskA": [64, 512], "maskNT": [64, 512],
    "identM": [64, 1024], "resetmask": [128, TT], "invc0": [128, 64],
}


class StopBuild(Exception):
    pass


def build(S, dbg=None, stop_after=None):
    NT = S // TT

    def chk(name):
        if stop_after == name:
            raise StopBuild()

    nc = bass.Bass("TRN2", target_bir_lowering=False)
    dr = {}
    dr["xin"] = nc.dram_tensor("xin", [NH, S, D], F32, kind="ExternalInput").ap()
    for name, shp in DRAM_IN.items():
        dr[name] = nc.dram_tensor(name, shp, F32, kind="ExternalInput").ap()
    dr["out"] = nc.dram_tensor("out", [NH, S, D], F32, kind="ExternalOutput").ap()
    dbg_out = {}
    if dbg:
        for name, shp in dbg.items():
            dbg_out[name] = nc.dram_tensor("dbg_" + name, shp, F32, kind="ExternalOutput").ap()

    NCHAN = 24
    with ExitStack() as es:
        P = Prog(nc, NCHAN)

        def sb(name, shape, dt):
            t = es.enter_context(nc.sbuf_tensor("sb_" + name, shape, dt))
            P.tracked.add("sb_" + name)
            return t

        xT = sb("xT", [128, NH, 8, TT], F32)
        hT = sb("hT", [128, NH, 8, 514], BF16)
        SCR = sb("SCR", [128, 76 * 256], F32)
        NSLOT = 4
        wring = [sb(f"wr{i}", [128, 3072], BF16) for i in range(NSLOT)]
        la_cur = sb("la_cur", [128, 2048], BF16)
        la_prev = sb("la_prev", [128, 2048], BF16)
        lb = sb("lb", [128, 1024], BF16)
        poolw = sb("poolw", [128, 512], BF16)
        ident = sb("ident", [128, 128], F32)
        identb = sb("identb", [128, 128], BF16)
        onesb = sb("onesb", [128, 128], BF16)
        bonesb = sb("bonesb", [128, 128], BF16)
        maskA = sb("maskA", [64, 512], F32)
        maskNT = sb("maskNT", [64, 512], F32)
        identM = sb("identM", [64, 1024], BF16)
        resetm = sb("resetm", [128, TT], F32)
        invc0 = sb("invc0", [128, 64], F32)
        vecs = sb("vecs", [128, NV], F32)
        xio = [sb(f"xio{i}", [128, D], F32) for i in range(3)]
        sq = [sb(f"sq{i}", [128, TT], BF16) for i in range(2)]
        sil = [sb(f"sil{i}", [128, TT], BF16) for i in range(2)]
        rstd = sb("rstd", [128, NH, TT], F32)
        rtmp = sb("rtmp", [128, TT], F32)
        lora1 = sb("lora1", [128, NH, 2, TT], BF16)
        hprev = sb("hprev", [128, NH, 8], BF16)
        pcar = sb("pcar", [128, NH, 12], F32)
        poolcar = sb("poolcar", [128, NH, 4, 16], F32)
        Z32 = sb("Z32", [128, NH, 4, 64], F32)
        Zb = sb("Zb", [128, NH, 4, 64], BF16)

        def scr(off_b, nbytes, dt, pattern=None, parts=128, **kw):
            assert off_b % 4 == 0 and nbytes % 4 == 0 and off_b + nbytes <= 76 * 1024
            a = SCR[0:parts, off_b // 4:(off_b + nbytes) // 4]
            if dt != F32:
                a = a.bitcast(dt)
            if pattern:
                a = a.rearrange(pattern, **kw)
            return a

        K = 1024
        hid = scr(0, 44 * K, BF16, "p (h u t) -> p h u t", h=NH, u=NFU)
        f32b = scr(44 * K, 32 * K, F32, "p (h k t) -> p h k t", h=NH, k=8)
        ybuf = scr(0, 8 * K, BF16, "p (h c t) -> p h c t", h=NH, c=4)
        ypool = scr(8 * K, 8 * K, BF16, "p (h c t) -> p h c t", h=NH, c=4)
        eP = scr(16 * K, 2 * K, F32)
        G32 = scr(18 * K, 2 * K, F32)
        bonv = scr(20 * K, 2 * K, F32)
        bk = scr(22 * K, 2 * K, BF16, "p (c two t) -> p c two t", two=2, t=64)
        ar = scr(24 * K, 2 * K, BF16, "p (c two t) -> p c two t", two=2, t=64)
        vT = scr(26 * K, 2 * K, BF16, "p (c x) -> p c x", x=128, parts=64)
        bT = scr(28 * K, 2 * K, BF16, "p (c x) -> p c x", x=128, parts=64)
        kT = scr(30 * K, 2 * K, BF16, "p (c x) -> p c x", x=128, parts=64)
        AM = scr(32 * K, 8 * K, BF16, "p (hd c q t) -> p hd c q t", hd=2, q=4, t=64, parts=64)
        Pst = [scr(40 * K + i * 2304, 2056, F32) for i in range(3)]
        r32 = scr(47 * K, 2 * K, F32)
        k32 = scr(49 * K, 2 * K, F32)
        sw = scr(51 * K, 2 * K, F32)
        a32 = scr(53 * K, 2 * K, F32)
        Lp = scr(55 * K, 2 * K, F32)
        eN = scr(57 * K, 2 * K, F32)
        ePm = scr(59 * K, 2 * K, F32)
        kkn = scr(61 * K, 2 * K, F32)
        kmod = scr(63 * K, 2 * K, F32)
        t1 = scr(65 * K, 2 * K, F32)
        t2 = scr(67 * K, 2 * K, F32)
        vb = scr(69 * K, 1 * K, BF16)
        sqk = scr(70 * K, 1 * K, BF16)
        rbb = scr(71 * K, 1 * K, BF16)
        PM = [scr(40 * K + i * 4 * K, 4 * K, BF16, "p (e two t) -> p e two t", two=2, t=64, parts=64) for i in range(2)]
        PkT = [scr(48 * K + i * 2 * K, 2 * K, BF16, "p (e t) -> p e t", t=64, parts=64) for i in range(2)]
        PV32 = scr(52 * K, 4 * K, F32, "p (e t) -> p e t", t=64, parts=64)
        Pb = scr(56 * K, 256, BF16, "p (hd t) -> p hd t", hd=2, parts=64)
        Ub = scr(56 * K + 256, 256, BF16, "p (hd t) -> p hd t", hd=2, parts=64)
        Tt = scr(56 * K + 512, 256, F32)
        Y1 = scr(57 * K, 2 * K, F32)
        Y32 = scr(59 * K, 2 * K, F32)
        gt1 = scr(61 * K, 2 * K, F32)
        gt2 = scr(63 * K, 2 * K, F32)
        gt3 = scr(65 * K, 2 * K, F32)
        ybf = scr(67 * K, 1 * K, BF16)
        ysq = scr(68 * K, 1 * K, BF16)
        PB = scr(40 * K, 2112, F32)
        PS1 = scr(43 * K, 2112, F32)
        PS2 = scr(46 * K, 2112, F32)
        pmix = scr(49 * K, 1 * K, BF16)
        mrg = scr(16 * K, 16 * K, BF16, "p (h k t) -> p h k t", h=NH, k=8)
        ms0 = scr(40 * K, 2 * K, F32)
        ms1 = scr(42 * K, 2 * K, F32)
        mm0 = scr(44 * K, 2 * K, F32)
        mm1 = scr(46 * K, 2 * K, F32)
        la_st = scr(0, 8 * K, F32)
        mula_st = scr(8 * K, 8 * K, F32)
        la_t = scr(16 * K, 8 * K, F32)

        ps = []
        for i in range(8):
            t = es.enter_context(nc.psum_tensor(f"ps{i}", [128, 512], F32))
            P.tracked.add(f"ps{i}")
            ps.append(t)

        sems = {e: es.enter_context(nc.semaphore("s_" + e)) for e in ENGS}
        dsems = [es.enter_context(nc.semaphore(f"d{i}")) for i in range(NCHAN)]
        block = es.enter_context(nc.Block())

        MUL, ADD, SUB, MAX = ALU.mult, ALU.add, ALU.subtract, ALU.max
        CH_W = list(range(0, NSLOT))
        CH_X = [NSLOT + i for i in range(3)]
        CH_MISC = NSLOT + 3
        CH_DBG = NSLOT + 4
        rot = {"evac": 0, "xio": 0}

        def vcol(c, n=1):
            return vecs[:, c:c + n]

        def evac_eng():
            rot["evac"] += 1
            return "act" if rot["evac"] % 2 else "dve"

        P.dma("sp", CH_MISC, ident[:, :], dr["ident"][:, :])
        P.dma("sp", CH_MISC, maskA[:, :], dr["maskA"][:, :])
        P.dma("sp", CH_MISC, maskNT[:, :], dr["maskNT"][:, :])
        P.dma("sp", CH_MISC, resetm[:, :], dr["resetmask"][:, :])
        P.dma("sp", CH_MISC, invc0[:, :], dr["invc0"][:, :])
        P.dma("sp", CH_MISC, vecs[:, 0:NV_IN], dr["vecs"][:, :])
        P.dma("sp", CH_MISC, la_st, dr["la"][:, :])
        P.dma("sp", CH_MISC, mula_st, dr["mula"][:, :])
        P.dma("pool", CH_MISC + 2, identb[:, :], dr["ident"][:, :], max_dma_last_dim=4096)
        P.dma("pool", CH_MISC + 2, onesb[:, :], dr["ones"][:, :], max_dma_last_dim=4096)
        P.dma("pool", CH_MISC + 2, bonesb[:, :], dr["blockones"][:, :], max_dma_last_dim=4096)
        P.dma("pool", CH_MISC + 2, identM[:, :], dr["identM"][:, :], max_dma_last_dim=4096)
        P.dma("pool", CH_MISC + 2, lb[:, :], dr["lb"][:, :], max_dma_last_dim=4096)
        P.dma("pool", CH_MISC + 2, poolw[:, :], dr["poolw"][:, :], max_dma_last_dim=4096)
        P.bump(CH_MISC)
        P.bump(CH_MISC + 2)
        P.ts(vcol(V_OMU, 12), vcol(V_MU, 12), -1.0, MUL, 1.0, ADD)
        P.ts(vcol(V_HG1, 8), vcol(V_G + 8, 8), 0.5, MUL)
        P.ts(vcol(V_HG5, 8), vcol(V_G + 40, 8), 0.5, MUL)
        P.tt(la_t, la_st, mula_st, MUL)
        P.copy(la_prev[:, :], la_t)
        P.tt(la_cur[:, :], la_st, la_t, SUB)
        P.memset(hprev[:, :, :], 0.0)
        P.memset(pcar[:, :, :], 0.0)
        P.memset(poolcar[:, :, :, :], 0.0)
        P.memset(Z32[:, :, :, :], 0.0)
        P.memset(Zb[:, :, :, :], 0.0)

        units = []
        for j in range(NT):
            for u in range(NFU):
                units.append((dr["wA1"][u, :, :], 2048))
            for d_ in range(8):
                units.append((dr["wB1"][d_, :, :], DFF))
            for c4 in range(4):
                units.append(("R", c4))
            for g in range(4):
                units.append((dr["win"][12 + g, :, :], 1024))
            for d_ in range(8):
                units.append(("M", d_))
            for d_ in range(8):
                units.append((dr["wo"][d_, :, :], 1024))
            for u in range(NFU):
                units.append((dr["wA2"][u, :, :], 2048))
            for d_ in range(8):
                units.append((dr["wB2"][d_, :, :], DFF))
        ws = {"issued": 0, "next": 0}

        def ws_issue(i):
            slot = wring[i % NSLOT]
            ch = CH_W[i % NSLOT]
            u = units[i]
            if u[0] == "R":
                c4 = u[1]
                for q in range(3):
                    P.dma("pool", ch, slot[:, q * 1024:(q + 1) * 1024], dr["win"][q * 4 + c4, :, :], max_dma_last_dim=4096)
                P.bump(ch)
            elif u[0] == "M":
                d_ = u[1]
                P.dma("pool", ch, slot[:, 0:1024], dr["win"][16 + d_, :, :], max_dma_last_dim=4096)
                P.dma("pool", ch, slot[:, 1024:2048], dr["win"][24 + d_, :, :], max_dma_last_dim=4096)
                P.dma("pool", ch, slot[:, 2048:3072], dr["wbr"][d_, :, :], max_dma_last_dim=4096)
                P.bump(ch)
            else:
                src, n = u
                P.dma("pool", ch, slot[:, 0:n], src, max_dma_last_dim=4096)

        def ws_get():
            i = ws["next"]
            ws["next"] += 1
            while ws["issued"] <= min(len(units) - 1, i + NSLOT - 1):
                ws_issue(ws["issued"])
                ws["issued"] += 1
            return wring[i % NSLOT]

        def dbg_dump(name, src_ap):
            if name in dbg_out:
                P.dma("sp", CH_DBG, dbg_out[name], src_ap)

        def rms_stats(src, h, psn):
            for k in range(8):
                s = sq[k % 2]
                P.act(s[:, :], src[:, h, k, :], AF.Square)
                P.mm(psn[:, :], onesb[:, :], s[:, :], start=(k == 0), stop=(k == 7))

        def rstd_from(psn, h, n, eps):
            P.act(rtmp[:, :], psn[:, :], AF.Sqrt, scale=1.0 / n, bias=eps)
            P.recip(rstd[:, h, :], rtmp[:, :])

        def norm_to_hT(gcol):
            for h in range(NH):
                psn = ps[6 + h]
                rms_stats(xT, h, psn)
                rstd_from(psn, h, D, RMS_EPS)
                for k in range(8):
                    P.stt(hT[:, h, k, 2:514], xT[:, h, k, :], vcol(gcol + k), rstd[:, h, :], MUL, MUL)

        def residual_update(hgcol):
            for h in range(NH):
                rstd_from(ps[6 + h], h, D, RMS_EPS)
            for h in range(NH):
                for k in range(8):
                    P.tt(f32b[:, h, k, :], f32b[:, h, k, :], rstd[:, h, :], MUL)
                    P.stt(xT[:, h, k, :], f32b[:, h, k, :], vcol(hgcol + k), xT[:, h, k, :], MUL, ADD)

        def out_proj_phase(nk, rhs_of):
            for dch in range(8):
                w = ws_get()
                for h in range(NH):
                    pso = ps[4 + (dch * NH + h) % 2]
                    for k in range(nk):
                        P.mm(pso[:, :], w[:, k * 128:(k + 1) * 128], rhs_of(h, k), start=(k == 0), stop=(k == nk - 1))
                    P.act(f32b[:, h, dch, :], pso[:, :], AF.Copy)
                    s = sq[(dch * NH + h) % 2]
                    P.act(s[:, :], pso[:, :], AF.Square)
                    P.mm(ps[6 + h][:, :], onesb[:, :], s[:, :], start=(dch == 0), stop=(dch == 7))

        def ffn(gcol_in, hgcol_out):
            norm_to_hT(gcol_in)
            chk("ffn_norm")
            for u in range(NFU):
                if u == 1:
                    chk("ffnA0")
                w = ws_get()
                for h in range(NH):
                    i = (u * NH + h) % 2
                    psg, psu = ps[2 * i], ps[2 * i + 1]
                    for k in range(8):
                        P.mm(psg[:, :], w[:, k * 128:(k + 1) * 128], hT[:, h, k, 2:514], start=(k == 0), stop=(k == 7))
                    for k in range(8):
                        P.mm(psu[:, :], w[:, 1024 + k * 128:1024 + (k + 1) * 128], hT[:, h, k, 2:514], start=(k == 0), stop=(k == 7))
                    P.act(sil[i][:, :], psg[:, :], AF.Silu)
                    P.tt(hid[:, h, u, :], psu[:, :], sil[i][:, :], MUL)
            chk("ffnA")
            out_proj_phase(NFU, lambda h, k: hid[:, h, k, :])
            chk("ffnB")
            residual_update(hgcol_out)
            chk("ffn")

        NCk = 4
        TW = NCk * CH
        NE = 2 * NCk

        def half_bufs(hh):
            b0 = 16 * K + hh * 30 * K
            Bf = {}
            Bf["eP"] = scr(b0, 1 * K, F32)
            Bf["G32"] = scr(b0 + 1 * K, 1 * K, F32)
            Bf["bonv"] = scr(b0 + 2 * K, 1 * K, F32)
            Bf["bk"] = scr(b0 + 3 * K, 1 * K, BF16, "p (c two t) -> p c two t", two=2, t=64)
            Bf["ar"] = scr(b0 + 4 * K, 1 * K, BF16, "p (c two t) -> p c two t", two=2, t=64)
            Bf["vT"] = scr(b0 + 5 * K, 1 * K, BF16, "p (c x) -> p c x", x=128, parts=64)
            Bf["bT"] = scr(b0 + 6 * K, 1 * K, BF16, "p (c x) -> p c x", x=128, parts=64)
            Bf["kT"] = scr(b0 + 7 * K, 1 * K, BF16, "p (c x) -> p c x", x=128, parts=64)
            Bf["AM"] = scr(b0 + 8 * K, 4 * K, BF16, "p (hd c q t) -> p hd c q t", hd=2, q=4, t=64, parts=64)
            p0 = b0 + 12 * K
            Bf["Pst"] = [scr(p0 + i_ * 1152, 1032, F32) for i_ in range(3)]
            q0 = p0 + 3456
            names = ["r32", "k32", "sw", "a32", "Lp", "eN", "ePm", "kkn", "kmod", "t1", "t2"]
            for n_, nm in enumerate(names):
                Bf[nm] = scr(q0 + n_ * K, 1 * K, F32)
            q1 = q0 + len(names) * K
            Bf["vb"] = scr(q1, 512, BF16)
            Bf["sqk"] = scr(q1 + 512, 512, BF16)
            Bf["rbb"] = scr(q1 + 1024, 512, BF16)
            assert q1 + 1536 <= b0 + 30 * K
            Bf["PM"] = [scr(p0 + i_ * 2 * K, 2 * K, BF16, "p (e two t) -> p e two t", two=2, t=64, parts=64) for i_ in range(2)]
            Bf["PkT"] = [scr(p0 + 4 * K + i_ * K, 1 * K, BF16, "p (e t) -> p e t", t=64, parts=64) for i_ in range(2)]
            Bf["PV32"] = scr(p0 + 6 * K, 2 * K, F32, "p (e t) -> p e t", t=64, parts=64)
            Bf["Pb"] = scr(p0 + 8 * K, 256, BF16, "p (hd t) -> p hd t", hd=2, parts=64)
            Bf["Ub"] = scr(p0 + 8 * K + 256, 256, BF16, "p (hd t) -> p hd t", hd=2, parts=64)
            Bf["Tt"] = scr(p0 + 8 * K + 512, 256, F32)
            Bf["Y1"] = scr(p0 + 9 * K, 1 * K, F32)
            Bf["Y32"] = scr(p0 + 10 * K, 1 * K, F32)
            Bf["gt1"] = scr(p0 + 11 * K, 1 * K, F32)
            Bf["gt2"] = scr(p0 + 12 * K, 1 * K, F32)
            Bf["gt3"] = scr(p0 + 13 * K, 1 * K, F32)
            Bf["ybf"] = scr(p0 + 14 * K, 512, BF16)
            Bf["ysq"] = scr(p0 + 14 * K + 512, 512, BF16)
            Bf["ps"] = [ps[4 * hh + i_] for i_ in range(4)]
            return Bf

        HB = [half_bufs(0), half_bufs(1)]

        def wkv_gen(j, h, c4, sub, w):
            Bf = HB[h]
            eP, G32, bonv, bk, ar, vT, bT, kT, AM = (Bf[n_] for n_ in ("eP", "G32", "bonv", "bk", "ar", "vT", "bT", "kT", "AM"))
            Pst, r32, k32, sw, a32, Lp, eN, ePm, kkn, kmod, t1, t2, vb, sqk, rbb = (Bf[n_] for n_ in (
                "Pst", "r32", "k32", "sw", "a32", "Lp", "eN", "ePm", "kkn", "kmod", "t1", "t2", "vb", "sqk", "rbb"))
            PM, PkT, PV32, Pb, Ub, Tt, Y1, Y32, gt1, gt2, gt3, ybf, ysq = (Bf[n_] for n_ in (
                "PM", "PkT", "PV32", "Pb", "Ub", "Tt", "Y1", "Y32", "gt1", "gt2", "gt3", "ybf", "ysq"))
            b0_, b1_, b2_, b3_ = Bf["ps"]
            t0 = sub * TW
            W_ = slice(0, TW)
            Zs32 = Z32[:, h, c4, :]
            Zsb = Zb[:, h, c4, :]
            dst = [r32, k32, vb]
            for i in range(3):
                pp = (b0_, b1_)[i % 2]
                for k in range(8):
                    P.mm(pp[:, W_], w[:, i * 1024 + k * 128:i * 1024 + (k + 1) * 128], hT[:, h, k, 2 + t0:2 + t0 + TW], start=(k == 0), stop=(k == 7))
                st = Pst[i]
                P.copy(st[:, 0:1], pcar[:, h, c4 * 3 + i:c4 * 3 + i + 1], eng="dve")
                P.act(st[:, 1:TW + 1], pp[:, W_], AF.Copy)
                P.copy(pcar[:, h, c4 * 3 + i:c4 * 3 + i + 1], st[:, TW:TW + 1], eng="dve")
                P.ts(t1, st[:, 0:TW], vcol(V_MU + i * 4 + c4), MUL)
                P.stt(dst[i], st[:, 1:TW + 1], vcol(V_OMU + i * 4 + c4), t1, MUL, ADD)
                yield
            cs = slice(c4 * 128, (c4 + 1) * 128)
            P.mm(b0_[:, W_], lb[0:64, cs], lora1[0:64, h, 0, t0:t0 + TW])
            P.act(sw, b0_[:, W_], AF.Sigmoid, bias=vcol(V_W0 + c4))
            P.mm(b1_[:, W_], lb[64:128, cs], lora1[64:128, h, 0, t0:t0 + TW])
            P.act(a32, b1_[:, W_], AF.Sigmoid, bias=vcol(V_A0 + c4))
            P.mm(b2_[:, W_], lb[:, 512 + c4 * 128:512 + (c4 + 1) * 128], lora1[:, h, 1, t0:t0 + TW])
            P.act(G32, b2_[:, W_], AF.Copy)
            yield
            P.scan(Lp, resetm[:, W_], sw, 0.0, MUL, ADD)
            P.tt(t2, Lp, sw, SUB)
            P.act(eP, Lp, AF.Exp, scale=-C0)
            P.act(eN, Lp, AF.Exp, scale=C0)
            P.act(ePm, t2, AF.Exp, scale=-C0)
            P.act(sqk, k32, AF.Square, scale=vcol(V_KK + c4))
            P.mm(b1_[:, W_], bonesb[:, :], sqk)
            yield
            P.act(t1, b1_[:, W_], AF.Sqrt)
            P.ts(t1, t1, 1e-12, MAX)
            P.recip(t1, t1)
            P.stt(kkn, k32, vcol(V_KK + c4), t1, MUL, MUL)
            P.ts(t2, a32, -1.0, ADD, vcol(V_KA + c4), MUL)
            P.stt(kmod, t2, 1.0, k32, ADD, MUL)
            yield
            v3 = lambda a: a.rearrange("p (c t) -> p c t", t=64)
            P.tt(bk[:, :, 1, :], v3(kmod), v3(eN), MUL)
            P.tt(t2, kkn, a32, MUL)
            P.tt(bk[:, :, 0, :], v3(t2), v3(eN), MUL)
            P.stt(ar[:, :, 0, :], v3(kkn), -1.0, v3(ePm), MUL, MUL)
            P.tt(ar[:, :, 1, :], v3(r32), v3(eP), MUL)
            P.stt(rbb, r32, vcol(V_RK + c4), kmod, MUL, MUL)
            P.mm(b0_[:, W_], bonesb[:, :], rbb)
            P.tt(bonv, b0_[:, W_], vb, MUL)
            yield
            for (src_of, dstT, pst) in ((lambda c: vb[:, c * 64:(c + 1) * 64], vT, b2_),
                                        (lambda c: bk[:, c, 0, :], bT, b3_),
                                        (lambda c: bk[:, c, 1, :], kT, b1_)):
                pv = pst[0:64, 0:256].bitcast(BF16).rearrange("p (c x) -> p c x", x=128)
                for c in range(NCk):
                    P.tr(pv[:, c, :], src_of(c), identb[:, :], signal=(c == NCk - 1))
                P.copy(dstT[:, :, :], pv[:, :, :], eng=evac_eng())
            yield
            for cp in range(NCk // 2):
                for hd in range(2):
                    pb = hd * 64
                    bank = (b2_, b3_)[hd]
                    pa = bank[0:64, :].rearrange("p (cc x) -> p cc x", cc=2)
                    for cc in range(2):
                        c = cp * 2 + cc
                        rhs = ar[pb:pb + 64, c, :, :].rearrange("p two t -> p (two t)")
                        P.mm(pa[:, cc, 0:128], bk[pb:pb + 64, c, 0, :], rhs, signal=False)
                        P.mm(pa[:, cc, 128:256], bk[pb:pb + 64, c, 1, :], rhs, signal=(cc == 1))
                    P.tt(AM[:, hd, cp * 2:cp * 2 + 2, :, :].rearrange("p c q t -> p (c q t)"), bank[0:64, :], maskA[:, :], MUL)
                yield
            pnt = [b0_[0:64, 0:256].rearrange("p (e t) -> p e t", t=64), b1_[0:64, 0:256].rearrange("p (e t) -> p e t", t=64)]
            for c in range(NCk):
                for hd in range(2):
                    pb = hd * 64
                    P.mm(pnt[hd][:, c, :], ar[pb:pb + 64, c, 0, :], bk[pb:pb + 64, c, 0, :], signal=(c == NCk - 1))
            for hd in range(2):
                P.tt(PkT[0][:, hd * NCk:(hd + 1) * NCk, :].rearrange("p e t -> p (e t)"), (b0_, b1_)[hd][0:64, 0:256], maskNT[:, 0:256], MUL)
            Nview = AM[:, :, :, 0, :].rearrange("p hd c t -> p (hd c) t")
            P.copy(PM[0][:, :, 0, :], Nview, eng="act")
            P.tt(PM[0][:, :, 1, :], Nview, identM[:, 0:NE * 64].rearrange("p (e t) -> p e t", t=64), ADD)
            ppv = b2_[0:64, :].rearrange("p (e t) -> p e t", t=64)
            for hd in range(2):
                for c in range(NCk):
                    P.mm(ppv[:, hd * NCk + c, :], AM[:, hd, c, 2, :], vT[:, c, hd * 64:(hd + 1) * 64], signal=(c == NCk - 1))
            P.copy(PV32[:, :, :], ppv, eng=evac_eng())
            yield
            cur = 0
            for lvl in range(6):
                nxt = 1 - cur
                for sbi in range(2):
                    es_ = range(sbi * NCk, (sbi + 1) * NCk)
                    p1 = (b2_, b3_)[sbi][0:64, :].rearrange("p (e x) -> p e x", x=128)
                    p2 = (b0_, b1_)[sbi][0:64, 0:256].rearrange("p (e t) -> p e t", t=64)
                    sl = slice(sbi * NCk, (sbi + 1) * NCk)
                    if lvl == 0:
                        for i, e in enumerate(es_):
                            P.mm(p1[:, i, 0:64], PkT[cur][:, e, :], PM[cur][:, e, 0, :], signal=(i == NCk - 1))
                        for i, e in enumerate(es_):
                            P.mm(p2[:, i, :], PM[cur][:, e, 0, :], PkT[cur][:, e, :], signal=(i == NCk - 1))
                        P.copy(PM[nxt][:, sl, 0, :], p1[:, :, 0:64], eng="act")
                        P.copy(PM[nxt][:, sl, 1, :], PM[cur][:, sl, 1, :], eng="dve")
                        P.copy(PkT[nxt][:, sl, :], p2, eng="dve")
                    elif lvl < 5:
                        for i, e in enumerate(es_):
                            P.mm(p1[:, i, :], PkT[cur][:, e, :], PM[cur][:, e, :, :].rearrange("p two t -> p (two t)"), signal=(i == NCk - 1))
                        for i, e in enumerate(es_):
                            P.mm(p2[:, i, :], PM[cur][:, e, 0, :], PkT[cur][:, e, :], signal=(i == NCk - 1))
                        P.copy(PM[nxt][:, sl, 0, :], p1[:, :, 0:64], eng="act")
                        P.tt(PM[nxt][:, sl, 1, :], p1[:, :, 64:128], PM[cur][:, sl, 1, :], ADD)
                        P.copy(PkT[nxt][:, sl, :], p2, eng="act")
                    else:
                        for i, e in enumerate(es_):
                            P.mm(p2[:, i, :], PkT[cur][:, e, :], PM[cur][:, e, 1, :], signal=(i == NCk - 1))
                        P.tt(PM[nxt][:, sl, 1, :], p2, PM[cur][:, sl, 1, :], ADD)
                    yield
                cur = nxt
            Mf = PM[cur]
            psc = [b2_[0:64, 0:64], b3_[0:64, 0:64]]
            psc2 = b0_[0:64, 0:128].rearrange("p (hd t) -> p hd t", hd=2)
            psz = b1_
            psya = [b2_, b3_]
            psyb = b1_
            for c in range(NCk):
                yc = slice(256 + c * 64, 256 + (c + 1) * 64)
                for hd in range(2):
                    pb = hd * 64
                    P.mm(psc[hd], ar[pb:pb + 64, c, 0, :], Zsb[pb:pb + 64, :], signal=(hd == 1))
                for hd in range(2):
                    pb = hd * 64
                    P.mm(psya[hd][pb:pb + 64, yc], Zsb[pb:pb + 64, :], ar[pb:pb + 64, c, 1, :], signal=(hd == 1))
                for hd in range(2):
                    P.tt(Pb[:, hd, :], psc[hd], PV32[:, hd * NCk + c, :], ADD)
                yield
                for hd in range(2):
                    P.mm(psc2[:, hd, :], Mf[:, hd * NCk + c, 1, :], Pb[:, hd, :], signal=(hd == 1))
                P.copy(Ub[:, :, :], psc2, eng="act")
                yield
                for hd in range(2):
                    pb = hd * 64
                    P.mm(psz[pb:pb + 64, 0:64], bT[:, c, pb:pb + 64], Ub[:, hd, :], start=True, stop=False, signal=False)
                    P.mm(psz[pb:pb + 64, 0:64], kT[:, c, pb:pb + 64], vT[:, c, pb:pb + 64], start=False, stop=True, signal=(hd == 1))
                for hd in range(2):
                    pb = hd * 64
                    P.mm(psyb[pb:pb + 64, yc], Ub[:, hd, :], AM[:, hd, c, 1, :], start=True, stop=False, signal=False)
                    P.mm(psyb[pb:pb + 64, yc], vT[:, c, pb:pb + 64], AM[:, hd, c, 3, :], start=False, stop=True, signal=(hd == 1))
                wc = eP[:, c * 64 + 63:c * 64 + 64]
                P.tt(Tt, psz[:, 0:64], Zs32, ADD)
                P.act(Zsb, Tt, AF.Copy, scale=wc)
                P.ts(Zs32, Tt, wc, MUL)
                yield
            for hd in range(2):
                pb = hd * 64
                P.act(Y1[pb:pb + 64, :], psya[hd][pb:pb + 64, 256:512], AF.Copy)
            P.tt(Y32, psyb[:, 256:512], Y1, ADD)
            P.act(ybf, Y32, AF.Copy)
            P.act(ysq, Y32, AF.Square)
            yield
            P.mm(b0_[:, W_], bonesb[:, :], ybf)
            P.mm(b2_[:, W_], bonesb[:, :], ysq)
            P.act(gt1, b0_[:, W_], AF.Copy, scale=1.0 / 64)
            P.tt(gt2, gt1, gt1, MUL)
            P.stt(gt2, b2_[:, W_], 1.0 / 64, gt2, MUL, SUB)
            P.ts(gt2, gt2, 0.0, MAX, GN_EPS, ADD)
            P.act(gt2, gt2, AF.Sqrt)
            P.recip(gt2, gt2)
            yield
            P.tt(gt3, Y32, gt1, SUB)
            P.tt(gt3, gt3, gt2, MUL)
            P.ts(gt3, gt3, vcol(V_LW + c4), MUL, vcol(V_LB + c4), ADD)
            P.tt(gt3, gt3, bonv, ADD)
            P.tt(ybuf[:, h, c4, t0:t0 + TW], gt3, G32, MUL)
            yield

        def run_lockstep(gens):
            live = list(gens)
            while live:
                nxt_live = []
                for g_ in live:
                    try:
                        next(g_)
                        nxt_live.append(g_)
                    except StopIteration:
                        pass
                live = nxt_live

        def mixer(j):
            norm_to_hT(V_G + 16)
            for h in range(NH):
                P.copy(hT[:, h, :, 1:2], hprev[:, h, :].unsqueeze(2), eng="dve")
                P.copy(hprev[:, h, :].unsqueeze(2), hT[:, h, :, 513:514], eng="dve")
            for h in range(NH):
                for part in range(2):
                    pp = ps[part]
                    cs0 = part * 128
                    n = 0
                    for k in range(8):
                        P.mm(pp[:, :], la_cur[:, k * 256 + cs0:k * 256 + cs0 + 128], hT[:, h, k, 2:514], start=(n == 0), stop=False)
                        n += 1
                        P.mm(pp[:, :], la_prev[:, k * 256 + cs0:k * 256 + cs0 + 128], hT[:, h, k, 1:513], start=False, stop=(k == 7))
                    if part == 0:
                        P.act(lora1[0:64, h, 0, :], pp[0:64, :], AF.Tanh)
                        P.act(lora1[64:128, h, 0, :], pp[64:128, :], AF.Copy)
                    else:
                        P.act(lora1[:, h, 1, :], pp[:, :], AF.Sigmoid)
            chk("lora_a")
            for c4 in range(4):
                w = ws_get()
                for sub in range(TT // TW):
                    run_lockstep([wkv_gen(j, h, c4, sub, w) for h in range(NH)])
            chk("wkv")
            for g, wd in enumerate((2, 4, 8, 16)):
                w = ws_get()
                nlev = g + 1
                for h in range(NH):
                    pp = ps[h % 2]
                    for k in range(8):
                        P.mm(pp[:, :], w[:, k * 128:(k + 1) * 128], hT[:, h, k, 2:514], start=(k == 0), stop=(k == 7))
                    P.copy(PB[:, 0:16], poolcar[:, h, g, :], eng="dve")
                    P.act(PB[:, 16:528], pp[:, :], AF.Copy)
                    P.copy(poolcar[:, h, g, :], PB[:, 512:528], eng="dve")
                    src, lo = PB, 0
                    bufs = [PS1, PS2]
                    for lv in range(nlev):
                        sh = 1 << lv
                        dstb = bufs[lv % 2]
                        nlo = lo + sh
                        P.tt(dstb[:, nlo:528], src[:, nlo:528], src[:, nlo - sh:528 - sh], ADD)
                        src, lo = dstb, nlo
                    P.stt(pmix, src[:, 16:528], 1.0 / wd, PB[:, 16:528], MUL, SUB)
                    if j == 0:
                        P.tt(t1[:, 0:16], src[:, 16:32], invc0[:, g * 16:(g + 1) * 16], MUL)
                        P.tt(pmix[:, 0:16], t1[:, 0:16], PB[:, 16:32], SUB)
                    pq = ps[2 + h % 2]
                    P.mm(pq[:, :], poolw[:, g * 128:(g + 1) * 128], pmix)
                    P.act(ypool[:, h, g, :], pq[:, :], AF.Copy, scale=vcol(V_PS + g))
            chk("pool")
            for dch in range(8):
                w = ws_get()
                for h in range(NH):
                    b0 = (h % 2) * 4
                    pg0, pg1, pbr, pbp = ps[b0], ps[b0 + 1], ps[b0 + 2], ps[b0 + 3]
                    for k in range(8):
                        P.mm(pg0[:, :], w[:, k * 128:(k + 1) * 128], hT[:, h, k, 2:514], start=(k == 0), stop=(k == 7))
                    for k in range(8):
                        P.mm(pg1[:, :], w[:, 1024 + k * 128:1024 + (k + 1) * 128], hT[:, h, k, 2:514], start=(k == 0), stop=(k == 7))
                    for k in range(4):
                        P.mm(pbr[:, :], w[:, 2048 + k * 128:2048 + (k + 1) * 128], ybuf[:, h, k, :], start=(k == 0), stop=(k == 3))
                    for k in range(4):
                        P.mm(pbp[:, :], w[:, 2560 + k * 128:2560 + (k + 1) * 128], ypool[:, h, k, :], start=(k == 0), stop=(k == 3))
                    P.act(ms0, pg0[:, :], AF.Sigmoid, bias=vcol(V_GB + dch))
                    P.act(ms1, pg1[:, :], AF.Sigmoid, bias=vcol(V_GB + 8 + dch))
                    P.tt(mm0, pbr[:, :], ms0, MUL)
                    P.tt(mm1, pbp[:, :], ms1, MUL)
                    P.tt(mrg[:, h, dch, :], mm0, mm1, ADD)
            chk("merge")
            out_proj_phase(8, lambda h, k: mrg[:, h, k, :])
            residual_update(V_G + 24)
            chk("mixer")

        def main_loop():
          for j in range(NT):
            for h in range(NH):
                for tb in range(4):
                    si = rot["xio"] % 3
                    rot["xio"] += 1
                    xs = xio[si]
                    P.dma("sp", CH_X[si], xs[:, :], dr["xin"][h, j * TT + tb * 128:j * TT + (tb + 1) * 128, :])
                    for half in range(2):
                        pst = ps[(tb * 2 + half) % 4]
                        for kk in range(4):
                            k = half * 4 + kk
                            P.tr(pst[:, kk * 128:(kk + 1) * 128], xs[:, k * 128:(k + 1) * 128], ident[:, :], signal=(kk == 3))
                        P.copy(xT[:, h, half * 4:half * 4 + 4, tb * 128:(tb + 1) * 128],
                               pst[:, :].rearrange("p (k t) -> p k t", t=128), eng=evac_eng())
            chk("load")
            ffn(V_G + 0, V_HG1)
            if j == 0:
                dbg_dump("x1", xT[:, :, :, :])
            mixer(j)
            if j == 0:
                dbg_dump("ybuf", ybuf)
                dbg_dump("x2", xT[:, :, :, :])
            ffn(V_G + 32, V_HG5)
            for h in range(NH):
                for tb in range(4):
                    si = rot["xio"] % 3
                    rot["xio"] += 1
                    xs = xio[si]
                    for half in range(2):
                        pst = ps[(tb * 2 + half) % 4]
                        for kk in range(4):
                            k = half * 4 + kk
                            P.tr(pst[:, kk * 128:(kk + 1) * 128], xT[:, h, k, tb * 128:(tb + 1) * 128], ident[:, :], signal=(kk == 3))
                        P.copy(xs[:, half * 512:(half + 1) * 512], pst[:, :], eng=evac_eng())
                    P.dma("sp", CH_X[si], dr["out"][h, j * TT + tb * 128:j * TT + (tb + 1) * 128, :], xs[:, :])
        try:
            main_loop()
            assert ws["next"] == len(units), (ws["next"], len(units))
        except StopBuild:
            print("[kernel] build stopped after", stop_after, flush=True)
        P.finish("sp")
        P.emit(block, sems, dsems)
        print(f"[kernel] S={S} ops={P.nops} per-engine={ {e: len(P.ops[e]) for e in ENGS} }", flush=True)
    return nc


_CACHE = {}


def run(inputs, S, ncores, dbg=None, stop_after=None):
    x = np.asarray(inputs["x"], np.float32)
    inp = {k: np.asarray(v, np.float32) for k, v in inputs.items() if k != "x"}
    shared = host_weights(inp)
    shared.update(host_consts())
    key = (S, tuple(sorted(dbg.items())) if dbg else None)
    nc = build(S, dbg, stop_after)
    in_maps = []
    for c in range(ncores):
        m = dict(shared)
        m["xin"] = np.ascontiguousarray(x[c * NH:(c + 1) * NH, :S])
        in_maps.append(m)
    res = run_bass_kernel_spmd(nc, in_maps, core_ids=list(range(ncores)))
    out = np.concatenate([r["out"] for r in res.results], 0)
    return out, res


def kernel(**inputs):
    out, _ = run(inputs, 2048, NCORES)
    return out.astype(np.float32)
```

```python
import numpy as np
from contextlib import ExitStack
import concourse.bass as bass
import concourse.mybir as mybir
from concourse.bass_utils import run_bass_kernel_spmd

F32 = mybir.dt.float32
BF16 = mybir.dt.bfloat16
AF = mybir.ActivationFunctionType
ALU = mybir.AluOpType
ESZ = {F32: 4, BF16: 2}

ENGS = ("pe", "act", "dve", "pool", "sp")
GRAN = 256
NCORES = 8
NH = 2
TT = 512
D = 1024
DFF = 2816
NFU = 22
CH = 64
C0 = float(np.exp(-0.5))
RMS_EPS = 1e-6
GN_EPS = 64e-5


class Prog:
    def __init__(self, nc, n_dma_chan):
        self.nc = nc
        self.ops = {e: [] for e in ENGS}
        self.cnt = {e: 0 for e in ENGS}
        self.pending = {e: False for e in ENGS}
        self.last_w = {}
        self.readers = {}
        self.water = {e: {} for e in ENGS}
        self.dcnt = [0] * n_dma_chan
        self.n_dma_chan = n_dma_chan
        self.tracked = set()
        self.nops = 0

    def keys(self, ap):
        name = ap.tensor.name
        if name not in self.tracked:
            return ()
        esz = ESZ[ap.dtype]
        pat = ap.ap
        ps = pat[0][0]
        off = ap.offset % ps if ps > 0 else ap.offset
        span = 1
        for st, n in pat[1:]:
            span += (n - 1) * abs(st)
        lo = off * esz
        hi = (off + span) * esz
        return [(name, g) for g in range(lo // GRAN, (hi - 1) // GRAN + 1)]

    def _collect(self, eng, rkeys, wkeys):
        deps = {}

        def add(d, raw):
            k, v, pe = d
            if pe == eng and not raw:
                return
            if deps.get(k, 0) < v:
                deps[k] = v

        lw = self.last_w
        for key in rkeys:
            d = lw.get(key)
            if d is not None:
                add(d, True)
        for key in wkeys:
            d = lw.get(key)
            if d is not None:
                add(d, False)
            for d in self.readers.get(key, ()):
                add(d, False)
        out = []
        wm = self.water[eng]
        for k, v in deps.items():
            if wm.get(k, 0) < v:
                wm[k] = v
                out.append((k, v))
        return out

    def _record(self, dep, rkeys, wkeys):
        for key in rkeys:
            lst = self.readers.setdefault(key, [])
            for i, d in enumerate(lst):
                if d[0] == dep[0]:
                    lst[i] = dep
                    break
            else:
                lst.append(dep)
        for key in wkeys:
            self.last_w[key] = dep
            self.readers[key] = []

    def _rw(self, reads, writes):
        rk = []
        for a in reads:
            rk.extend(self.keys(a))
        wk = []
        for a in writes:
            wk.extend(self.keys(a))
        return rk, wk

    def op(self, eng, fn, reads, writes, signal=True):
        rk, wk = self._rw(reads, writes)
        waits = self._collect(eng, rk, wk)
        if signal:
            self.cnt[eng] += 1
            dep = (eng, self.cnt[eng], eng)
            self.pending[eng] = False
        else:
            dep = (eng, self.cnt[eng] + 1, eng)
            self.pending[eng] = True
        self._record(dep, rk, wk)
        self.ops[eng].append((waits, fn, "e" if signal else None))
        self.nops += 1

    def dma(self, eng, chan, out, in_, **kw):
        rk, wk = self._rw([in_], [out])
        waits = self._collect(eng, rk, wk)
        self.dcnt[chan] += 16
        dep = (("dma", chan), self.dcnt[chan], "dma")
        self._record(dep, rk, wk)
        self.ops[eng].append((waits, lambda e: e.dma_start(out=out, in_=in_, **kw), ("dma", chan)))
        self.nops += 1

    def bump(self, chan):
        k = ("dma", chan)
        full = (k, self.dcnt[chan], "dma")
        for key, dep in self.last_w.items():
            if dep[0] == k:
                self.last_w[key] = full

    def finish(self, eng="sp"):
        waits = []
        for e in ENGS:
            if e != eng and self.cnt[e] > 0:
                waits.append((e, self.cnt[e]))
        for c in range(self.n_dma_chan):
            if self.dcnt[c] > 0:
                waits.append((("dma", c), self.dcnt[c]))
        self.ops[eng].append((waits, None, None))

    def emit(self, block, sems, dsems):
        for e in ENGS:
            assert not self.pending[e], f"unsignalled tail on {e}"

        def semof(k):
            return dsems[k[1]] if isinstance(k, tuple) else sems[k]

        def run(name, engine):
            sem = sems[name]
            for waits, fn, inc in self.ops[name]:
                for k, v in waits:
                    engine.wait_ge(semof(k), v)
                if fn is None:
                    continue
                ins = fn(engine)
                if inc is None:
                    continue
                if inc == "e":
                    ins.then_inc(sem, 1)
                else:
                    ins.then_inc(dsems[inc[1]], 16)

        block.tensor(lambda e: run("pe", e))
        block.scalar(lambda e: run("act", e))
        block.vector(lambda e: run("dve", e))
        block.gpsimd(lambda e: run("pool", e))
        block.sync(lambda e: run("sp", e))

    def mm(self, out, lhsT, rhs, start=True, stop=True, signal=True):
        self.op("pe", lambda e: e.matmul(out, lhsT=lhsT, rhs=rhs, start=start, stop=stop),
                [lhsT, rhs], [out], signal)

    def tr(self, out, in_, ident, signal=True):
        self.op("pe", lambda e: e.transpose(out, in_, ident), [in_, ident], [out], signal)

    def act(self, out, in_, func, bias=None, scale=None, eng="act"):
        reads = [in_]
        kw = {}
        if bias is not None:
            kw["bias"] = bias
            if not isinstance(bias, (int, float)):
                reads.append(bias)
        if scale is not None:
            kw["scale"] = scale
            if not isinstance(scale, (int, float)):
                reads.append(scale)
        self.op(eng, lambda e: e.activation(out=out, in_=in_, func=func, **kw), reads, [out])

    def tt(self, out, in0, in1, op, eng="dve"):
        self.op(eng, lambda e: e.tensor_tensor(out=out, in0=in0, in1=in1, op=op), [in0, in1], [out])

    def ts(self, out, in0, s1, op0, s2=None, op1=None, eng="dve"):
        reads = [in0]
        for s in (s1, s2):
            if s is not None and not isinstance(s, (int, float)):
                reads.append(s)
        if op1 is None:
            fn = lambda e: e.tensor_scalar(out=out, in0=in0, scalar1=s1, scalar2=None, op0=op0)
        else:
            fn = lambda e: e.tensor_scalar(out=out, in0=in0, scalar1=s1, scalar2=s2, op0=op0, op1=op1)
        self.op(eng, fn, reads, [out])

    def stt(self, out, in0, scalar, in1, op0, op1):
        reads = [in0, in1]
        if not isinstance(scalar, (int, float)):
            reads.append(scalar)
        self.op("dve", lambda e: e.scalar_tensor_tensor(out=out, in0=in0, scalar=scalar, in1=in1, op0=op0, op1=op1),
                reads, [out])

    def copy(self, out, in_, eng="dve"):
        if eng == "act":
            self.act(out, in_, AF.Copy)
        else:
            self.op(eng, lambda e: e.tensor_copy(out=out, in_=in_), [in_], [out])

    def scan(self, out, d0, d1, init, op0, op1):
        self.op("dve", lambda e: e.tensor_tensor_scan(out=out, data0=d0, data1=d1, initial=init, op0=op0, op1=op1),
                [d0, d1], [out])

    def recip(self, out, in_):
        self.op("dve", lambda e: e.reciprocal(out=out, in_=in_), [in_], [out])

    def memset(self, ap, val, eng="dve"):
        self.op(eng, lambda e: e.memset(ap, val), [], [ap])


V_G = 0
V_GB = 48
V_MU = 64
V_W0 = 76
V_A0 = 80
V_KK = 84
V_KA = 88
V_RK = 92
V_LW = 96
V_LB = 100
V_PS = 104
V_OMU = 108
V_HG1 = 120
V_HG5 = 128
NV = 136
NV_IN = 108


def host_consts():
    c = {}
    c["ident"] = np.eye(128, dtype=np.float32)
    bo = np.zeros((128, 128), np.float32)
    bo[:64, :64] = 1.0
    bo[64:, 64:] = 1.0
    c["blockones"] = bo
    c["ones"] = np.ones((128, 128), np.float32)
    s = np.arange(64)[:, None]
    t = np.arange(64)[None, :]
    strict = (s < t).astype(np.float32)
    incl = (s <= t).astype(np.float32)
    m4 = np.concatenate([strict, incl, strict, incl], 1)
    c["maskA"] = np.tile(m4, (1, 2)).copy()
    c["maskNT"] = np.tile(strict.T, (1, 8)).copy()
    c["identM"] = np.tile(np.eye(64, dtype=np.float32), (1, 16)).copy()
    rm = np.ones((128, TT), np.float32)
    rm[:, ::CH] = 0.0
    c["resetmask"] = rm
    ic = np.zeros((128, 4, 16), np.float32)
    for g, w in enumerate((2, 4, 8, 16)):
        ic[:, g, :] = 1.0 / np.minimum(np.arange(1, 17), w)
    c["invc0"] = ic.reshape(128, 64)
    return c


def host_weights(inp):
    L = 0
    w = {}

    def A(wg, wu):
        g = wg.reshape(8, 128, NFU, 128).transpose(2, 1, 0, 3)
        u = wu.reshape(8, 128, NFU, 128).transpose(2, 1, 0, 3)
        return np.ascontiguousarray(np.stack([g, u], 2).reshape(NFU, 128, 2048))

    def B(wd):
        return np.ascontiguousarray(wd.reshape(NFU, 128, 8, 128).transpose(2, 1, 0, 3).reshape(8, 128, DFF))

    w["wA1"] = A(inp["ffn1_gate"][L], inp["ffn1_up"][L])
    w["wB1"] = B(inp["ffn1_down"][L])
    w["wA2"] = A(inp["ffn2_gate"][L], inp["ffn2_up"][L])
    w["wB2"] = B(inp["ffn2_down"][L])
    w["win"] = np.ascontiguousarray(inp["w_in"][L].reshape(8, 128, 32, 128).transpose(2, 1, 0, 3).reshape(32, 128, 1024))
    cat = np.concatenate([inp["decay_a"][L], inp["aaa_a"][L], inp["gate_a"][L]], 1)
    w["la"] = np.ascontiguousarray(cat.reshape(8, 128, 256).transpose(1, 0, 2).reshape(128, 2048))
    mw = inp["mu_wag"][L]
    mucat = np.concatenate([np.broadcast_to(mw[0][:, None], (1024, 64)), np.broadcast_to(mw[1][:, None], (1024, 64)),
                            np.broadcast_to(mw[2][:, None], (1024, 128))], 1)
    w["mula"] = np.ascontiguousarray(mucat.reshape(8, 128, 256).transpose(1, 0, 2).reshape(128, 2048))
    lb1 = np.concatenate([inp["decay_b"][L], inp["aaa_b"][L]], 0)
    w["lb"] = np.ascontiguousarray(np.concatenate([lb1, inp["gate_b"][L]], 1))
    w["poolw"] = np.ascontiguousarray(inp["pool_w"][L].transpose(1, 0, 2).reshape(128, 512))
    br = inp["w_branch_rwkv"][L].reshape(4, 128, 8, 128).transpose(2, 1, 0, 3)
    bp = inp["w_branch_pool"][L].reshape(4, 128, 8, 128).transpose(2, 1, 0, 3)
    w["wbr"] = np.ascontiguousarray(np.concatenate([br, bp], 2).reshape(8, 128, 1024))
    w["wo"] = np.ascontiguousarray(inp["w_out"][L].reshape(8, 128, 8, 128).transpose(2, 1, 0, 3).reshape(8, 128, 1024))
    v = np.zeros((128, NV_IN), np.float32)

    def put(col, vec):
        n = vec.shape[0] // 128
        v[:, col:col + n] = vec.reshape(n, 128).T

    for i in range(6):
        put(V_G + i * 8, inp["norm_gains"][L][i])
    for b in range(2):
        put(V_GB + b * 8, inp["gate_bias"][L][b])
    for i in range(3):
        put(V_MU + i * 4, inp["mu_rkv"][L][i])
    put(V_W0, inp["w0"][L])
    put(V_A0, inp["a0"][L])
    put(V_KK, inp["k_k"][L])
    put(V_KA, inp["k_a"][L])
    put(V_RK, inp["r_k"][L].reshape(512))
    put(V_LW, inp["ln_x_w"][L])
    put(V_LB, inp["ln_x_b"][L])
    put(V_PS, inp["pool_scale"][L])
    w["vecs"] = v
    return w


DRAM_IN = {
    "wA1": [NFU, 128, 2048], "wB1": [8, 128, DFF], "wA2": [NFU, 128, 2048], "wB2": [8, 128, DFF],
    "win": [32, 128, 1024], "la": [128, 2048], "mula": [128, 2048], "lb": [128, 1024], "poolw": [128, 512],
    "wbr": [8, 128, 1024], "wo": [8, 128, 1024], "vecs": [128, NV_IN],
    "ident": [128, 128], "blockones": [128, 128], "ones": [128, 128], "maskA": [64, 512], "maskNT": [64, 512],
    "identM": [64, 1024], "resetmask": [128, TT], "invc0": [128, 64],
}


class StopBuild(Exception):
    pass


def build(S, dbg=None, stop_after=None):
    NT = S // TT

    def chk(name):
        if stop_after == name:
            raise StopBuild()

    nc = bass.Bass("TRN2", target_bir_lowering=False)
    dr = {}
    dr["xin"] = nc.dram_tensor("xin", [NH, S, D], F32, kind="ExternalInput").ap()
    for name, shp in DRAM_IN.items():
        dr[name] = nc.dram_tensor(name, shp, F32, kind="ExternalInput").ap()
    dr["out"] = nc.dram_tensor("out", [NH, S, D], F32, kind="ExternalOutput").ap()
    dbg_out = {}
    if dbg:
        for name, shp in dbg.items():
            dbg_out[name] = nc.dram_tensor("dbg_" + name, shp, F32, kind="ExternalOutput").ap()

    NCHAN = 24
    with ExitStack() as es:
        P = Prog(nc, NCHAN)

        def sb(name, shape, dt):
            t = es.enter_context(nc.sbuf_tensor("sb_" + name, shape, dt))
            P.tracked.add("sb_" + name)
            return t

        xT = sb("xT", [128, NH, 8, TT], F32)
        hT = sb("hT", [128, NH, 8, 514], BF16)
        SCR = sb("SCR", [128, 76 * 256], F32)
        NSLOT = 4
        wring = [sb(f"wr{i}", [128, 3072], BF16) for i in range(NSLOT)]
        la_cur = sb("la_cur", [128, 2048], BF16)
        la_prev = sb("la_prev", [128, 2048], BF16)
        lb = sb("lb", [128, 1024], BF16)
        poolw = sb("poolw", [128, 512], BF16)
        ident = sb("ident", [128, 128], F32)
        identb = sb("identb", [128, 128], BF16)
        onesb = sb("onesb", [128, 128], BF16)
        bonesb = sb("bonesb", [128, 128], BF16)
        maskA = sb("maskA", [64, 512], F32)
        maskNT = sb("maskNT", [64, 512], F32)
        identM = sb("identM", [64, 1024], BF16)
        resetm = sb("resetm", [128, TT], F32)
        invc0 = sb("invc0", [128, 64], F32)
        vecs = sb("vecs", [128, NV], F32)
        xio = [sb(f"xio{i}", [128, D], F32) for i in range(3)]
        sq = [sb(f"sq{i}", [128, TT], BF16) for i in range(2)]
        sil = [sb(f"sil{i}", [128, TT], BF16) for i in range(2)]
        rstd = sb("rstd", [128, NH, TT], F32)
        rtmp = sb("rtmp", [128, TT], F32)
        lora1 = sb("lora1", [128, NH, 2, TT], BF16)
        hprev = sb("hprev", [128, NH, 8], BF16)
        pcar = sb("pcar", [128, NH, 12], F32)
        poolcar = sb("poolcar", [128, NH, 4, 16], F32)
        Z32 = sb("Z32", [128, NH, 4, 64], F32)
        Zb = sb("Zb", [128, NH, 4, 64], BF16)

        def scr(off_b, nbytes, dt, pattern=None, parts=128, **kw):
            assert off_b % 4 == 0 and nbytes % 4 == 0 and off_b + nbytes <= 76 * 1024
            a = SCR[0:parts, off_b // 4:(off_b + nbytes) // 4]
            if dt != F32:
                a = a.bitcast(dt)
            if pattern:
                a = a.rearrange(pattern, **kw)
            return a

        K = 1024
        hid = scr(0, 44 * K, BF16, "p (h u t) -> p h u t", h=NH, u=NFU)
        f32b = scr(44 * K, 32 * K, F32, "p (h k t) -> p h k t", h=NH, k=8)
        ybuf = scr(0, 8 * K, BF16, "p (h c t) -> p h c t", h=NH, c=4)
        ypool = scr(8 * K, 8 * K, BF16, "p (h c t) -> p h c t", h=NH, c=4)
        eP = scr(16 * K, 2 * K, F32)
        G32 = scr(18 * K, 2 * K, F32)
        bonv = scr(20 * K, 2 * K, F32)
        bk = scr(22 * K, 2 * K, BF16, "p (c two t) -> p c two t", two=2, t=64)
        ar = scr(24 * K, 2 * K, BF16, "p (c two t) -> p c two t", two=2, t=64)
        vT = scr(26 * K, 2 * K, BF16, "p (c x) -> p c x", x=128, parts=64)
        bT = scr(28 * K, 2 * K, BF16, "p (c x) -> p c x", x=128, parts=64)
        kT = scr(30 * K, 2 * K, BF16, "p (c x) -> p c x", x=128, parts=64)
        AM = scr(32 * K, 8 * K, BF16, "p (hd c q t) -> p hd c q t", hd=2, q=4, t=64, parts=64)
        Pst = [scr(40 * K + i * 2304, 2056, F32) for i in range(3)]
        r32 = scr(47 * K, 2 * K, F32)
        k32 = scr(49 * K, 2 * K, F32)
        sw = scr(51 * K, 2 * K, F32)
        a32 = scr(53 * K, 2 * K, F32)
        Lp = scr(55 * K, 2 * K, F32)
        eN = scr(57 * K, 2 * K, F32)
        ePm = scr(59 * K, 2 * K, F32)
        kkn = scr(61 * K, 2 * K, F32)
        kmod = scr(63 * K, 2 * K, F32)
        t1 = scr(65 * K, 2 * K, F32)
        t2 = scr(67 * K, 2 * K, F32)
        vb = scr(69 * K, 1 * K, BF16)
        sqk = scr(70 * K, 1 * K, BF16)
        rbb = scr(71 * K, 1 * K, BF16)
        PM = [scr(40 * K + i * 4 * K, 4 * K, BF16, "p (e two t) -> p e two t", two=2, t=64, parts=64) for i in range(2)]
        PkT = [scr(48 * K + i * 2 * K, 2 * K, BF16, "p (e t) -> p e t", t=64, parts=64) for i in range(2)]
        PV32 = scr(52 * K, 4 * K, F32, "p (e t) -> p e t", t=64, parts=64)
        Pb = scr(56 * K, 256, BF16, "p (hd t) -> p hd t", hd=2, parts=64)
        Ub = scr(56 * K + 256, 256, BF16, "p (hd t) -> p hd t", hd=2, parts=64)
        Tt = scr(56 * K + 512, 256, F32)
        Y1 = scr(57 * K, 2 * K, F32)
        Y32 = scr(59 * K, 2 * K, F32)
        gt1 = scr(61 * K, 2 * K, F32)
        gt2 = scr(63 * K, 2 * K, F32)
        gt3 = scr(65 * K, 2 * K, F32)
        ybf = scr(67 * K, 1 * K, BF16)
        ysq = scr(68 * K, 1 * K, BF16)
        PB = scr(40 * K, 2112, F32)
        PS1 = scr(43 * K, 2112, F32)
        PS2 = scr(46 * K, 2112, F32)
        pmix = scr(49 * K, 1 * K, BF16)
        mrg = scr(16 * K, 16 * K, BF16, "p (h k t) -> p h k t", h=NH, k=8)
        ms0 = scr(40 * K, 2 * K, F32)
        ms1 = scr(42 * K, 2 * K, F32)
        mm0 = scr(44 * K, 2 * K, F32)
        mm1 = scr(46 * K, 2 * K, F32)
        la_st = scr(0, 8 * K, F32)
        mula_st = scr(8 * K, 8 * K, F32)
        la_t = scr(16 * K, 8 * K, F32)

        ps = []
        for i in range(8):
            t = es.enter_context(nc.psum_tensor(f"ps{i}", [128, 512], F32))
            P.tracked.add(f"ps{i}")
            ps.append(t)

        sems = {e: es.enter_context(nc.semaphore("s_" + e)) for e in ENGS}
        dsems = [es.enter_context(nc.semaphore(f"d{i}")) for i in range(NCHAN)]
        block = es.enter_context(nc.Block())

        MUL, ADD, SUB, MAX = ALU.mult, ALU.add, ALU.subtract, ALU.max
        CH_W = list(range(0, NSLOT))
        CH_X = [NSLOT + i for i in range(3)]
        CH_MISC = NSLOT + 3
        CH_DBG = NSLOT + 4
        rot = {"evac": 0, "xio": 0}

        def vcol(c, n=1):
            return vecs[:, c:c + n]

        def evac_eng():
            rot["evac"] += 1
            return "act" if rot["evac"] % 2 else "dve"

        P.dma("sp", CH_MISC, ident[:, :], dr["ident"][:, :])
        P.dma("sp", CH_MISC, maskA[:, :], dr["maskA"][:, :])
        P.dma("sp", CH_MISC, maskNT[:, :], dr["maskNT"][:, :])
        P.dma("sp", CH_MISC, resetm[:, :], dr["resetmask"][:, :])
        P.dma("sp", CH_MISC, invc0[:, :], dr["invc0"][:, :])
        P.dma("sp", CH_MISC, vecs[:, 0:NV_IN], dr["vecs"][:, :])
        P.dma("sp", CH_MISC, la_st, dr["la"][:, :])
        P.dma("sp", CH_MISC, mula_st, dr["mula"][:, :])
        P.dma("pool", CH_MISC + 2, identb[:, :], dr["ident"][:, :], max_dma_last_dim=4096)
        P.dma("pool", CH_MISC + 2, onesb[:, :], dr["ones"][:, :], max_dma_last_dim=4096)
        P.dma("pool", CH_MISC + 2, bonesb[:, :], dr["blockones"][:, :], max_dma_last_dim=4096)
        P.dma("pool", CH_MISC + 2, identM[:, :], dr["identM"][:, :], max_dma_last_dim=4096)
        P.dma("pool", CH_MISC + 2, lb[:, :], dr["lb"][:, :], max_dma_last_dim=4096)
        P.dma("pool", CH_MISC + 2, poolw[:, :], dr["poolw"][:, :], max_dma_last_dim=4096)
        P.bump(CH_MISC)
        P.bump(CH_MISC + 2)
        P.ts(vcol(V_OMU, 12), vcol(V_MU, 12), -1.0, MUL, 1.0, ADD)
        P.ts(vcol(V_HG1, 8), vcol(V_G + 8, 8), 0.5, MUL)
        P.ts(vcol(V_HG5, 8), vcol(V_G + 40, 8), 0.5, MUL)
        P.tt(la_t, la_st, mula_st, MUL)
        P.copy(la_prev[:, :], la_t)
        P.tt(la_cur[:, :], la_st, la_t, SUB)
        P.memset(hprev[:, :, :], 0.0)
        P.memset(pcar[:, :, :], 0.0)
        P.memset(poolcar[:, :, :, :], 0.0)
        P.memset(Z32[:, :, :, :], 0.0)
        P.memset(Zb[:, :, :, :], 0.0)

        units = []
        for j in range(NT):
            for u in range(NFU):
                units.append((dr["wA1"][u, :, :], 2048))
            for d_ in range(8):
                units.append((dr["wB1"][d_, :, :], DFF))
            for c4 in range(4):
                units.append(("R", c4))
            for g in range(4):
                units.append((dr["win"][12 + g, :, :], 1024))
            for d_ in range(8):
                units.append(("M", d_))
            for d_ in range(8):
                units.append((dr["wo"][d_, :, :], 1024))
            for u in range(NFU):
                units.append((dr["wA2"][u, :, :], 2048))
            for d_ in range(8):
                units.append((dr["wB2"][d_, :, :], DFF))
        ws = {"issued": 0, "next": 0}

        def ws_issue(i):
            slot = wring[i % NSLOT]
            ch = CH_W[i % NSLOT]
            u = units[i]
            if u[0] == "R":
                c4 = u[1]
                for q in range(3):
                    P.dma("pool", ch, slot[:, q * 1024:(q + 1) * 1024], dr["win"][q * 4 + c4, :, :], max_dma_last_dim=4096)
                P.bump(ch)
            elif u[0] == "M":
                d_ = u[1]
                P.dma("pool", ch, slot[:, 0:1024], dr["win"][16 + d_, :, :], max_dma_last_dim=4096)
                P.dma("pool", ch, slot[:, 1024:2048], dr["win"][24 + d_, :, :], max_dma_last_dim=4096)
                P.dma("pool", ch, slot[:, 2048:3072], dr["wbr"][d_, :, :], max_dma_last_dim=4096)
                P.bump(ch)
            else:
                src, n = u
                P.dma("pool", ch, slot[:, 0:n], src, max_dma_last_dim=4096)

        def ws_get():
            i = ws["next"]
            ws["next"] += 1
            while ws["issued"] <= min(len(units) - 1, i + NSLOT - 1):
                ws_issue(ws["issued"])
                ws["issued"] += 1
            return wring[i % NSLOT]

        def dbg_dump(name, src_ap):
            if name in dbg_out:
                P.dma("sp", CH_DBG, dbg_out[name], src_ap)

        def rms_stats(src, h, psn):
            for k in range(8):
                s = sq[k % 2]
                P.act(s[:, :], src[:, h, k, :], AF.Square)
                P.mm(psn[:, :], onesb[:, :], s[:, :], start=(k == 0), stop=(k == 7))

        def rstd_from(psn, h, n, eps):
            P.act(rtmp[:, :], psn[:, :], AF.Sqrt, scale=1.0 / n, bias=eps)
            P.recip(rstd[:, h, :], rtmp[:, :])

        def norm_to_hT(gcol):
            for h in range(NH):
                psn = ps[6 + h]
                rms_stats(xT, h, psn)
                rstd_from(psn, h, D, RMS_EPS)
                for k in range(8):
                    P.stt(hT[:, h, k, 2:514], xT[:, h, k, :], vcol(gcol + k), rstd[:, h, :], MUL, MUL)

        def residual_update(hgcol):
            for h in range(NH):
                rstd_from(ps[6 + h], h, D, RMS_EPS)
            for h in range(NH):
                for k in range(8):
                    P.tt(f32b[:, h, k, :], f32b[:, h, k, :], rstd[:, h, :], MUL)
                    P.stt(xT[:, h, k, :], f32b[:, h, k, :], vcol(hgcol + k), xT[:, h, k, :], MUL, ADD)

        def out_proj_phase(nk, rhs_of):
            for dch in range(8):
                w = ws_get()
                for h in range(NH):
                    pso = ps[4 + (dch * NH + h) % 2]
                    for k in range(nk):
                        P.mm(pso[:, :], w[:, k * 128:(k + 1) * 128], rhs_of(h, k), start=(k == 0), stop=(k == nk - 1))
                    P.act(f32b[:, h, dch, :], pso[:, :], AF.Copy)
            for h in range(NH):
                rms_stats(f32b, h, ps[6 + h])

        def ffn(gcol_in, hgcol_out):
            norm_to_hT(gcol_in)
            chk("ffn_norm")
            for u in range(NFU):
                if u == 1:
                    chk("ffnA0")
                w = ws_get()
                for h in range(NH):
                    i = (u * NH + h) % 2
                    psg, psu = ps[2 * i], ps[2 * i + 1]
                    for k in range(8):
                        P.mm(psg[:, :], w[:, k * 128:(k + 1) * 128], hT[:, h, k, 2:514], start=(k == 0), stop=(k == 7))
                    for k in range(8):
                        P.mm(psu[:, :], w[:, 1024 + k * 128:1024 + (k + 1) * 128], hT[:, h, k, 2:514], start=(k == 0), stop=(k == 7))
                    P.act(sil[i][:, :], psg[:, :], AF.Silu)
                    P.tt(hid[:, h, u, :], psu[:, :], sil[i][:, :], MUL)
            chk("ffnA")
            out_proj_phase(NFU, lambda h, k: hid[:, h, k, :])
            chk("ffnB")
            residual_update(hgcol_out)
            chk("ffn")

        NCk = 4
        TW = NCk * CH
        NE = 2 * NCk

        def half_bufs(hh):
            b0 = 16 * K + hh * 30 * K
            Bf = {}
            Bf["eP"] = scr(b0, 1 * K, F32)
            Bf["G32"] = scr(b0 + 1 * K, 1 * K, F32)
            Bf["bonv"] = scr(b0 + 2 * K, 1 * K, F32)
            Bf["bk"] = scr(b0 + 3 * K, 1 * K, BF16, "p (c two t) -> p c two t", two=2, t=64)
            Bf["ar"] = scr(b0 + 4 * K, 1 * K, BF16, "p (c two t) -> p c two t", two=2, t=64)
            Bf["vT"] = scr(b0 + 5 * K, 1 * K, BF16, "p (c x) -> p c x", x=128, parts=64)
            Bf["bT"] = scr(b0 + 6 * K, 1 * K, BF16, "p (c x) -> p c x", x=128, parts=64)
            Bf["kT"] = scr(b0 + 7 * K, 1 * K, BF16, "p (c x) -> p c x", x=128, parts=64)
            Bf["AM"] = scr(b0 + 8 * K, 4 * K, BF16, "p (hd c q t) -> p hd c q t", hd=2, q=4, t=64, parts=64)
            p0 = b0 + 12 * K
            Bf["Pst"] = [scr(p0 + i_ * 1152, 1032, F32) for i_ in range(3)]
            q0 = p0 + 3456
            names = ["r32", "k32", "sw", "a32", "Lp", "eN", "ePm", "kkn", "kmod", "t1", "t2"]
            for n_, nm in enumerate(names):
                Bf[nm] = scr(q0 + n_ * K, 1 * K, F32)
            q1 = q0 + len(names) * K
            Bf["vb"] = scr(q1, 512, BF16)
            Bf["sqk"] = scr(q1 + 512, 512, BF16)
            Bf["rbb"] = scr(q1 + 1024, 512, BF16)
            assert q1 + 1536 <= b0 + 30 * K
            Bf["PM"] = [scr(p0 + i_ * 2 * K, 2 * K, BF16, "p (e two t) -> p e two t", two=2, t=64, parts=64) for i_ in range(2)]
            Bf["PkT"] = [scr(p0 + 4 * K + i_ * K, 1 * K, BF16, "p (e t) -> p e t", t=64, parts=64) for i_ in range(2)]
            Bf["PV32"] = scr(p0 + 6 * K, 2 * K, F32, "p (e t) -> p e t", t=64, parts=64)
            Bf["Pb"] = scr(p0 + 8 * K, 256, BF16, "p (hd t) -> p hd t", hd=2, parts=64)
            Bf["Ub"] = scr(p0 + 8 * K + 256, 256, BF16, "p (hd t) -> p hd t", hd=2, parts=64)
            Bf["Tt"] = scr(p0 + 8 * K + 512, 256, F32)
            Bf["Y1"] = scr(p0 + 9 * K, 1 * K, F32)
            Bf["Y32"] = scr(p0 + 10 * K, 1 * K, F32)
            Bf["gt1"] = scr(p0 + 11 * K, 1 * K, F32)
            Bf["gt2"] = scr(p0 + 12 * K, 1 * K, F32)
            Bf["gt3"] = scr(p0 + 13 * K, 1 * K, F32)
            Bf["ybf"] = scr(p0 + 14 * K, 512, BF16)
            Bf["ysq"] = scr(p0 + 14 * K + 512, 512, BF16)
            Bf["ps"] = [ps[4 * hh + i_] for i_ in range(4)]
            return Bf

        HB = [half_bufs(0), half_bufs(1)]

        def wkv_gen(j, h, c4, sub, w):
            Bf = HB[h]
            eP, G32, bonv, bk, ar, vT, bT, kT, AM = (Bf[n_] for n_ in ("eP", "G32", "bonv", "bk", "ar", "vT", "bT", "kT", "AM"))
            Pst, r32, k32, sw, a32, Lp, eN, ePm, kkn, kmod, t1, t2, vb, sqk, rbb = (Bf[n_] for n_ in (
                "Pst", "r32", "k32", "sw", "a32", "Lp", "eN", "ePm", "kkn", "kmod", "t1", "t2", "vb", "sqk", "rbb"))
            PM, PkT, PV32, Pb, Ub, Tt, Y1, Y32, gt1, gt2, gt3, ybf, ysq = (Bf[n_] for n_ in (
                "PM", "PkT", "PV32", "Pb", "Ub", "Tt", "Y1", "Y32", "gt1", "gt2", "gt3", "ybf", "ysq"))
            b0_, b1_, b2_, b3_ = Bf["ps"]
            t0 = sub * TW
            W_ = slice(0, TW)
            Zs32 = Z32[:, h, c4, :]
            Zsb = Zb[:, h, c4, :]
            dst = [r32, k32, vb]
            for i in range(3):
                pp = (b0_, b1_)[i % 2]
                for k in range(8):
                    P.mm(pp[:, W_], w[:, i * 1024 + k * 128:i * 1024 + (k + 1) * 128], hT[:, h, k, 2 + t0:2 + t0 + TW], start=(k == 0), stop=(k == 7))
                st = Pst[i]
                P.copy(st[:, 0:1], pcar[:, h, c4 * 3 + i:c4 * 3 + i + 1], eng="dve")
                P.act(st[:, 1:TW + 1], pp[:, W_], AF.Copy)
                P.copy(pcar[:, h, c4 * 3 + i:c4 * 3 + i + 1], st[:, TW:TW + 1], eng="dve")
                P.ts(t1, st[:, 0:TW], vcol(V_MU + i * 4 + c4), MUL)
                P.stt(dst[i], st[:, 1:TW + 1], vcol(V_OMU + i * 4 + c4), t1, MUL, ADD)
                yield
            cs = slice(c4 * 128, (c4 + 1) * 128)
            P.mm(b0_[:, W_], lb[0:64, cs], lora1[0:64, h, 0, t0:t0 + TW])
            P.act(sw, b0_[:, W_], AF.Sigmoid, bias=vcol(V_W0 + c4))
            P.mm(b1_[:, W_], lb[64:128, cs], lora1[64:128, h, 0, t0:t0 + TW])
            P.act(a32, b1_[:, W_], AF.Sigmoid, bias=vcol(V_A0 + c4))
            P.mm(b2_[:, W_], lb[:, 512 + c4 * 128:512 + (c4 + 1) * 128], lora1[:, h, 1, t0:t0 + TW])
            P.act(G32, b2_[:, W_], AF.Copy)
            yield
            P.scan(Lp, resetm[:, W_], sw, 0.0, MUL, ADD)
            P.tt(t2, Lp, sw, SUB)
            P.act(eP, Lp, AF.Exp, scale=-C0)
            P.act(eN, Lp, AF.Exp, scale=C0)
            P.act(ePm, t2, AF.Exp, scale=-C0)
            P.act(sqk, k32, AF.Square, scale=vcol(V_KK + c4))
            P.mm(b1_[:, W_], bonesb[:, :], sqk)
            yield
            P.act(t1, b1_[:, W_], AF.Sqrt)
            P.ts(t1, t1, 1e-12, MAX)
            P.recip(t1, t1)
            P.stt(kkn, k32, vcol(V_KK + c4), t1, MUL, MUL)
            P.ts(t2, a32, -1.0, ADD, vcol(V_KA + c4), MUL)
            P.stt(kmod, t2, 1.0, k32, ADD, MUL)
            yield
            v3 = lambda a: a.rearrange("p (c t) -> p c t", t=64)
            P.tt(bk[:, :, 1, :], v3(kmod), v3(eN), MUL)
            P.tt(t2, kkn, a32, MUL)
            P.tt(bk[:, :, 0, :], v3(t2), v3(eN), MUL)
            P.stt(ar[:, :, 0, :], v3(kkn), -1.0, v3(ePm), MUL, MUL)
            P.tt(ar[:, :, 1, :], v3(r32), v3(eP), MUL)
            P.stt(rbb, r32, vcol(V_RK + c4), kmod, MUL, MUL)
            P.mm(b0_[:, W_], bonesb[:, :], rbb)
            P.tt(bonv, b0_[:, W_], vb, MUL)
            yield
            for (src_of, dstT, pst) in ((lambda c: vb[:, c * 64:(c + 1) * 64], vT, b2_),
                                        (lambda c: bk[:, c, 0, :], bT, b3_),
                                        (lambda c: bk[:, c, 1, :], kT, b1_)):
                pv = pst[0:64, 0:256].bitcast(BF16).rearrange("p (c x) -> p c x", x=128)
                for c in range(NCk):
                    P.tr(pv[:, c, :], src_of(c), identb[:, :], signal=(c == NCk - 1))
                P.copy(dstT[:, :, :], pv[:, :, :], eng=evac_eng())
            yield
            for cp in range(NCk // 2):
                for hd in range(2):
                    pb = hd * 64
                    bank = (b2_, b3_)[hd]
                    pa = bank[0:64, :].rearrange("p (cc x) -> p cc x", cc=2)
                    for cc in range(2):
                        c = cp * 2 + cc
                        rhs = ar[pb:pb + 64, c, :, :].rearrange("p two t -> p (two t)")
                        P.mm(pa[:, cc, 0:128], bk[pb:pb + 64, c, 0, :], rhs, signal=False)
                        P.mm(pa[:, cc, 128:256], bk[pb:pb + 64, c, 1, :], rhs, signal=(cc == 1))
                    P.tt(AM[:, hd, cp * 2:cp * 2 + 2, :, :].rearrange("p c q t -> p (c q t)"), bank[0:64, :], maskA[:, :], MUL)
                yield
            pnt = [b0_[0:64, 0:256].rearrange("p (e t) -> p e t", t=64), b1_[0:64, 0:256].rearrange("p (e t) -> p e t", t=64)]
            for c in range(NCk):
                for hd in range(2):
                    pb = hd * 64
                    P.mm(pnt[hd][:, c, :], ar[pb:pb + 64, c, 0, :], bk[pb:pb + 64, c, 0, :], signal=(c == NCk - 1))
            for hd in range(2):
                P.tt(PkT[0][:, hd * NCk:(hd + 1) * NCk, :].rearrange("p e t -> p (e t)"), (b0_, b1_)[hd][0:64, 0:256], maskNT[:, 0:256], MUL)
            Nview = AM[:, :, :, 0, :].rearrange("p hd c t -> p (hd c) t")
            P.copy(PM[0][:, :, 0, :], Nview, eng="act")
            P.tt(PM[0][:, :, 1, :], Nview, identM[:, 0:NE * 64].rearrange("p (e t) -> p e t", t=64), ADD)
            ppv = b2_[0:64, :].rearrange("p (e t) -> p e t", t=64)
            for hd in range(2):
                for c in range(NCk):
                    P.mm(ppv[:, hd * NCk + c, :], AM[:, hd, c, 2, :], vT[:, c, hd * 64:(hd + 1) * 64], signal=(c == NCk - 1))
            P.copy(PV32[:, :, :], ppv, eng=evac_eng())
            yield
            cur = 0
            for lvl in range(6):
                nxt = 1 - cur
                for sbi in range(2):
                    es_ = range(sbi * NCk, (sbi + 1) * NCk)
                    p1 = (b2_, b3_)[sbi][0:64, :].rearrange("p (e x) -> p e x", x=128)
                    p2 = (b0_, b1_)[sbi][0:64, 0:256].rearrange("p (e t) -> p e t", t=64)
                    sl = slice(sbi * NCk, (sbi + 1) * NCk)
                    if lvl == 0:
                        for i, e in enumerate(es_):
                            P.mm(p1[:, i, 0:64], PkT[cur][:, e, :], PM[cur][:, e, 0, :], signal=(i == NCk - 1))
                        for i, e in enumerate(es_):
                            P.mm(p2[:, i, :], PM[cur][:, e, 0, :], PkT[cur][:, e, :], signal=(i == NCk - 1))
                        P.copy(PM[nxt][:, sl, 0, :], p1[:, :, 0:64], eng="act")
                        P.copy(PM[nxt][:, sl, 1, :], PM[cur][:, sl, 1, :], eng="dve")
                        P.copy(PkT[nxt][:, sl, :], p2, eng="dve")
                    elif lvl < 5:
                        for i, e in enumerate(es_):
                            P.mm(p1[:, i, :], PkT[cur][:, e, :], PM[cur][:, e, :, :].rearrange("p two t -> p (two t)"), signal=(i == NCk - 1))
                        for i, e in enumerate(es_):
                            P.mm(p2[:, i, :], PM[cur][:, e, 0, :], PkT[cur][:, e, :], signal=(i == NCk - 1))
                        P.copy(PM[nxt][:, sl, 0, :], p1[:, :, 0:64], eng="act")
                        P.tt(PM[nxt][:, sl, 1, :], p1[:, :, 64:128], PM[cur][:, sl, 1, :], ADD)
                        P.copy(PkT[nxt][:, sl, :], p2, eng="act")
                    else:
                        for i, e in enumerate(es_):
                            P.mm(p2[:, i, :], PkT[cur][:, e, :], PM[cur][:, e, 1, :], signal=(i == NCk - 1))
                        P.tt(PM[nxt][:, sl, 1, :], p2, PM[cur][:, sl, 1, :], ADD)
                    yield
                cur = nxt
            Mf = PM[cur]
            psc = [b2_[0:64, 0:64], b3_[0:64, 0:64]]
            psc2 = b0_[0:64, 0:128].rearrange("p (hd t) -> p hd t", hd=2)
            psz = b1_
            psya = [b2_, b3_]
            psyb = b1_
            for c in range(NCk):
                yc = slice(256 + c * 64, 256 + (c + 1) * 64)
                for hd in range(2):
                    pb = hd * 64
                    P.mm(psc[hd], ar[pb:pb + 64, c, 0, :], Zsb[pb:pb + 64, :], signal=(hd == 1))
                for hd in range(2):
                    pb = hd * 64
                    P.mm(psya[hd][pb:pb + 64, yc], Zsb[pb:pb + 64, :], ar[pb:pb + 64, c, 1, :], signal=(hd == 1))
                for hd in range(2):
                    P.tt(Pb[:, hd, :], psc[hd], PV32[:, hd * NCk + c, :], ADD)
                yield
                for hd in range(2):
                    P.mm(psc2[:, hd, :], Mf[:, hd * NCk + c, 1, :], Pb[:, hd, :], signal=(hd == 1))
                P.copy(Ub[:, :, :], psc2, eng="act")
                yield
                for hd in range(2):
                    pb = hd * 64
                    P.mm(psz[pb:pb + 64, 0:64], bT[:, c, pb:pb + 64], Ub[:, hd, :], start=True, stop=False, signal=False)
                    P.mm(psz[pb:pb + 64, 0:64], kT[:, c, pb:pb + 64], vT[:, c, pb:pb + 64], start=False, stop=True, signal=(hd == 1))
                for hd in range(2):
                    pb = hd * 64
                    P.mm(psyb[pb:pb + 64, yc], Ub[:, hd, :], AM[:, hd, c, 1, :], start=True, stop=False, signal=False)
                    P.mm(psyb[pb:pb + 64, yc], vT[:, c, pb:pb + 64], AM[:, hd, c, 3, :], start=False, stop=True, signal=(hd == 1))
                wc = eP[:, c * 64 + 63:c * 64 + 64]
                P.tt(Tt, psz[:, 0:64], Zs32, ADD)
                P.act(Zsb, Tt, AF.Copy, scale=wc)
                P.ts(Zs32, Tt, wc, MUL)
                yield
            for hd in range(2):
                pb = hd * 64
                P.act(Y1[pb:pb + 64, :], psya[hd][pb:pb + 64, 256:512], AF.Copy)
            P.tt(Y32, psyb[:, 256:512], Y1, ADD)
            P.act(ybf, Y32, AF.Copy)
            P.act(ysq, Y32, AF.Square)
            yield
            P.mm(b0_[:, W_], bonesb[:, :], ybf)
            P.mm(b2_[:, W_], bonesb[:, :], ysq)
            P.act(gt1, b0_[:, W_], AF.Copy, scale=1.0 / 64)
            P.tt(gt2, gt1, gt1, MUL)
            P.stt(gt2, b2_[:, W_], 1.0 / 64, gt2, MUL, SUB)
            P.ts(gt2, gt2, 0.0, MAX, GN_EPS, ADD)
            P.act(gt2, gt2, AF.Sqrt)
            P.recip(gt2, gt2)
            yield
            P.tt(gt3, Y32, gt1, SUB)
            P.tt(gt3, gt3, gt2, MUL)
            P.ts(gt3, gt3, vcol(V_LW + c4), MUL, vcol(V_LB + c4), ADD)
            P.tt(gt3, gt3, bonv, ADD)
            P.tt(ybuf[:, h, c4, t0:t0 + TW], gt3, G32, MUL)
            yield

        def run_lockstep(gens):
            live = list(gens)
            while live:
                nxt_live = []
                for g_ in live:
                    try:
                        next(g_)
                        nxt_live.append(g_)
                    except StopIteration:
                        pass
                live = nxt_live

        def mixer(j):
            norm_to_hT(V_G + 16)
            for h in range(NH):
                P.copy(hT[:, h, :, 1:2], hprev[:, h, :].unsqueeze(2), eng="dve")
                P.copy(hprev[:, h, :].unsqueeze(2), hT[:, h, :, 513:514], eng="dve")
            for h in range(NH):
                for part in range(2):
                    pp = ps[part]
                    cs0 = part * 128
                    n = 0
                    for k in range(8):
                        P.mm(pp[:, :], la_cur[:, k * 256 + cs0:k * 256 + cs0 + 128], hT[:, h, k, 2:514], start=(n == 0), stop=False)
                        n += 1
                        P.mm(pp[:, :], la_prev[:, k * 256 + cs0:k * 256 + cs0 + 128], hT[:, h, k, 1:513], start=False, stop=(k == 7))
                    if part == 0:
                        P.act(lora1[0:64, h, 0, :], pp[0:64, :], AF.Tanh)
                        P.act(lora1[64:128, h, 0, :], pp[64:128, :], AF.Copy)
                    else:
                        P.act(lora1[:, h, 1, :], pp[:, :], AF.Sigmoid)
            chk("lora_a")
            for c4 in range(4):
                w = ws_get()
                for sub in range(TT // TW):
                    run_lockstep([wkv_gen(j, h, c4, sub, w) for h in range(NH)])
            chk("wkv")
            for g, wd in enumerate((2, 4, 8, 16)):
                w = ws_get()
                nlev = g + 1
                for h in range(NH):
                    pp = ps[h % 2]
                    for k in range(8):
                        P.mm(pp[:, :], w[:, k * 128:(k + 1) * 128], hT[:, h, k, 2:514], start=(k == 0), stop=(k == 7))
                    P.copy(PB[:, 0:16], poolcar[:, h, g, :], eng="dve")
                    P.act(PB[:, 16:528], pp[:, :], AF.Copy)
                    P.copy(poolcar[:, h, g, :], PB[:, 512:528], eng="dve")
                    src, lo = PB, 0
                    bufs = [PS1, PS2]
                    for lv in range(nlev):
                        sh = 1 << lv
                        dstb = bufs[lv % 2]
                        nlo = lo + sh
                        P.tt(dstb[:, nlo:528], src[:, nlo:528], src[:, nlo - sh:528 - sh], ADD)
                        src, lo = dstb, nlo
                    P.stt(pmix, src[:, 16:528], 1.0 / wd, PB[:, 16:528], MUL, SUB)
                    if j == 0:
                        P.tt(t1[:, 0:16], src[:, 16:32], invc0[:, g * 16:(g + 1) * 16], MUL)
                        P.tt(pmix[:, 0:16], t1[:, 0:16], PB[:, 16:32], SUB)
                    pq = ps[2 + h % 2]
                    P.mm(pq[:, :], poolw[:, g * 128:(g + 1) * 128], pmix)
                    P.act(ypool[:, h, g, :], pq[:, :], AF.Copy, scale=vcol(V_PS + g))
            chk("pool")
            for dch in range(8):
                w = ws_get()
                for h in range(NH):
                    b0 = (h % 2) * 4
                    pg0, pg1, pbr, pbp = ps[b0], ps[b0 + 1], ps[b0 + 2], ps[b0 + 3]
                    for k in range(8):
                        P.mm(pg0[:, :], w[:, k * 128:(k + 1) * 128], hT[:, h, k, 2:514], start=(k == 0), stop=(k == 7))
                    for k in range(8):
                        P.mm(pg1[:, :], w[:, 1024 + k * 128:1024 + (k + 1) * 128], hT[:, h, k, 2:514], start=(k == 0), stop=(k == 7))
                    for k in range(4):
                        P.mm(pbr[:, :], w[:, 2048 + k * 128:2048 + (k + 1) * 128], ybuf[:, h, k, :], start=(k == 0), stop=(k == 3))
                    for k in range(4):
                        P.mm(pbp[:, :], w[:, 2560 + k * 128:2560 + (k + 1) * 128], ypool[:, h, k, :], start=(k == 0), stop=(k == 3))
                    P.act(ms0, pg0[:, :], AF.Sigmoid, bias=vcol(V_GB + dch))
                    P.act(ms1, pg1[:, :], AF.Sigmoid, bias=vcol(V_GB + 8 + dch))
                    P.tt(mm0, pbr[:, :], ms0, MUL)
                    P.tt(mm1, pbp[:, :], ms1, MUL)
                    P.tt(mrg[:, h, dch, :], mm0, mm1, ADD)
            chk("merge")
            out_proj_phase(8, lambda h, k: mrg[:, h, k, :])
            residual_update(V_G + 24)
            chk("mixer")

        def main_loop():
          for j in range(NT):
            for h in range(NH):
                for tb in range(4):
                    si = rot["xio"] % 3
                    rot["xio"] += 1
                    xs = xio[si]
                    P.dma("sp", CH_X[si], xs[:, :], dr["xin"][h, j * TT + tb * 128:j * TT + (tb + 1) * 128, :])
                    for half in range(2):
                        pst = ps[(tb * 2 + half) % 4]
                        for kk in range(4):
                            k = half * 4 + kk
                            P.tr(pst[:, kk * 128:(kk + 1) * 128], xs[:, k * 128:(k + 1) * 128], ident[:, :], signal=(kk == 3))
                        P.copy(xT[:, h, half * 4:half * 4 + 4, tb * 128:(tb + 1) * 128],
                               pst[:, :].rearrange("p (k t) -> p k t", t=128), eng=evac_eng())
            chk("load")
            ffn(V_G + 0, V_HG1)
            if j == 0:
                dbg_dump("x1", xT[:, :, :, :])
            mixer(j)
            if j == 0:
                dbg_dump("ybuf", ybuf)
                dbg_dump("x2", xT[:, :, :, :])
            ffn(V_G + 32, V_HG5)
            for h in range(NH):
                for tb in range(4):
                    si = rot["xio"] % 3
                    rot["xio"] += 1
                    xs = xio[si]
                    for half in range(2):
                        pst = ps[(tb * 2 + half) % 4]
                        for kk in range(4):
                            k = half * 4 + kk
                            P.tr(pst[:, kk * 128:(kk + 1) * 128], xT[:, h, k, tb * 128:(tb + 1) * 128], ident[:, :], signal=(kk == 3))
                        P.copy(xs[:, half * 512:(half + 1) * 512], pst[:, :], eng=evac_eng())
                    P.dma("sp", CH_X[si], dr["out"][h, j * TT + tb * 128:j * TT + (tb + 1) * 128, :], xs[:, :])
        try:
            main_loop()
            assert ws["next"] == len(units), (ws["next"], len(units))
        except StopBuild:
            print("[kernel] build stopped after", stop_after, flush=True)
        P.finish("sp")
        P.emit(block, sems, dsems)
        print(f"[kernel] S={S} ops={P.nops} per-engine={ {e: len(P.ops[e]) for e in ENGS} }", flush=True)
    return nc


_CACHE = {}


def run(inputs, S, ncores, dbg=None, stop_after=None):
    x = np.asarray(inputs["x"], np.float32)
    inp = {k: np.asarray(v, np.float32) for k, v in inputs.items() if k != "x"}
    shared = host_weights(inp)
    shared.update(host_consts())
    key = (S, tuple(sorted(dbg.items())) if dbg else None)
    nc = build(S, dbg, stop_after)
    in_maps = []
    for c in range(ncores):
        m = dict(shared)
        m["xin"] = np.ascontiguousarray(x[c * NH:(c + 1) * NH, :S])
        in_maps.append(m)
    res = run_bass_kernel_spmd(nc, in_maps, core_ids=list(range(ncores)))
    out = np.concatenate([r["out"] for r in res.results], 0)
    return out, res


def kernel(**inputs):
    out, _ = run(inputs, 2048, NCORES)
    return out.astype(np.float32)
```

```python
import numpy as np
from contextlib import ExitStack
import concourse.bass as bass
import concourse.mybir as mybir
from concourse.bass_utils import run_bass_kernel_spmd

F32 = mybir.dt.float32
BF16 = mybir.dt.bfloat16
AF = mybir.ActivationFunctionType
ALU = mybir.AluOpType
ESZ = {F32: 4, BF16: 2}

ENGS = ("pe", "act", "dve", "pool", "sp")
GRAN = 256
NCORES = 8
NH = 2
TT = 512
D = 1024
DFF = 2816
NFU = 22
CH = 64
C0 = float(np.exp(-0.5))
RMS_EPS = 1e-6
GN_EPS = 64e-5


class Prog:
    def __init__(self, nc, n_dma_chan):
        self.nc = nc
        self.ops = {e: [] for e in ENGS}
        self.cnt = {e: 0 for e in ENGS}
        self.pending = {e: False for e in ENGS}
        self.last_w = {}
        self.readers = {}
        self.water = {e: {} for e in ENGS}
        self.dcnt = [0] * n_dma_chan
        self.n_dma_chan = n_dma_chan
        self.tracked = set()
        self.nops = 0

    def keys(self, ap):
        name = ap.tensor.name
        if name not in self.tracked:
            return ()
        esz = ESZ[ap.dtype]
        pat = ap.ap
        ps = pat[0][0]
        off = ap.offset % ps if ps > 0 else ap.offset
        span = 1
        for st, n in pat[1:]:
            span += (n - 1) * abs(st)
        lo = off * esz
        hi = (off + span) * esz
        return [(name, g) for g in range(lo // GRAN, (hi - 1) // GRAN + 1)]

    def _collect(self, eng, rkeys, wkeys):
        deps = {}

        def add(d, raw):
            k, v, pe = d
            if pe == eng and not raw:
                return
            if deps.get(k, 0) < v:
                deps[k] = v

        lw = self.last_w
        for key in rkeys:
            d = lw.get(key)
            if d is not None:
                add(d, True)
        for key in wkeys:
            d = lw.get(key)
            if d is not None:
                add(d, False)
            for d in self.readers.get(key, ()):
                add(d, False)
        out = []
        wm = self.water[eng]
        for k, v in deps.items():
            if wm.get(k, 0) < v:
                wm[k] = v
                out.append((k, v))
        return out

    def _record(self, dep, rkeys, wkeys):
        for key in rkeys:
            lst = self.readers.setdefault(key, [])
            for i, d in enumerate(lst):
                if d[0] == dep[0]:
                    lst[i] = dep
                    break
            else:
                lst.append(dep)
        for key in wkeys:
            self.last_w[key] = dep
            self.readers[key] = []

    def _rw(self, reads, writes):
        rk = []
        for a in reads:
            rk.extend(self.keys(a))
        wk = []
        for a in writes:
            wk.extend(self.keys(a))
        return rk, wk

    def op(self, eng, fn, reads, writes, signal=True):
        rk, wk = self._rw(reads, writes)
        waits = self._collect(eng, rk, wk)
        if signal:
            self.cnt[eng] += 1
            dep = (eng, self.cnt[eng], eng)
            self.pending[eng] = False
        else:
            dep = (eng, self.cnt[eng] + 1, eng)
            self.pending[eng] = True
        self._record(dep, rk, wk)
        self.ops[eng].append((waits, fn, "e" if signal else None))
        self.nops += 1

    def dma(self, eng, chan, out, in_, **kw):
        rk, wk = self._rw([in_], [out])
        waits = self._collect(eng, rk, wk)
        self.dcnt[chan] += 16
        dep = (("dma", chan), self.dcnt[chan], "dma")
        self._record(dep, rk, wk)
        self.ops[eng].append((waits, lambda e: e.dma_start(out=out, in_=in_, **kw), ("dma", chan)))
        self.nops += 1

    def bump(self, chan):
        k = ("dma", chan)
        full = (k, self.dcnt[chan], "dma")
        for key, dep in self.last_w.items():
            if dep[0] == k:
                self.last_w[key] = full

    def finish(self, eng="sp"):
        waits = []
        for e in ENGS:
            if e != eng and self.cnt[e] > 0:
                waits.append((e, self.cnt[e]))
        for c in range(self.n_dma_chan):
            if self.dcnt[c] > 0:
                waits.append((("dma", c), self.dcnt[c]))
        self.ops[eng].append((waits, None, None))

    def emit(self, block, sems, dsems):
        for e in ENGS:
            assert not self.pending[e], f"unsignalled tail on {e}"

        def semof(k):
            return dsems[k[1]] if isinstance(k, tuple) else sems[k]

        def run(name, engine):
            sem = sems[name]
            for waits, fn, inc in self.ops[name]:
                for k, v in waits:
                    engine.wait_ge(semof(k), v)
                if fn is None:
                    continue
                ins = fn(engine)
                if inc is None:
                    continue
                if inc == "e":
                    ins.then_inc(sem, 1)
                else:
                    ins.then_inc(dsems[inc[1]], 16)

        block.tensor(lambda e: run("pe", e))
        block.scalar(lambda e: run("act", e))
        block.vector(lambda e: run("dve", e))
        block.gpsimd(lambda e: run("pool", e))
        block.sync(lambda e: run("sp", e))

    def mm(self, out, lhsT, rhs, start=True, stop=True, signal=True):
        self.op("pe", lambda e: e.matmul(out, lhsT=lhsT, rhs=rhs, start=start, stop=stop),
                [lhsT, rhs], [out], signal)

    def tr(self, out, in_, ident, signal=True):
        self.op("pe", lambda e: e.transpose(out, in_, ident), [in_, ident], [out], signal)

    def act(self, out, in_, func, bias=None, scale=None, eng="act"):
        reads = [in_]
        kw = {}
        if bias is not None:
            kw["bias"] = bias
            if not isinstance(bias, (int, float)):
                reads.append(bias)
        if scale is not None:
            kw["scale"] = scale
            if not isinstance(scale, (int, float)):
                reads.append(scale)
        self.op(eng, lambda e: e.activation(out=out, in_=in_, func=func, **kw), reads, [out])

    def tt(self, out, in0, in1, op, eng="dve"):
        self.op(eng, lambda e: e.tensor_tensor(out=out, in0=in0, in1=in1, op=op), [in0, in1], [out])

    def ts(self, out, in0, s1, op0, s2=None, op1=None, eng="dve"):
        reads = [in0]
        for s in (s1, s2):
            if s is not None and not isinstance(s, (int, float)):
                reads.append(s)
        if op1 is None:
            fn = lambda e: e.tensor_scalar(out=out, in0=in0, scalar1=s1, scalar2=None, op0=op0)
        else:
            fn = lambda e: e.tensor_scalar(out=out, in0=in0, scalar1=s1, scalar2=s2, op0=op0, op1=op1)
        self.op(eng, fn, reads, [out])

    def stt(self, out, in0, scalar, in1, op0, op1):
        reads = [in0, in1]
        if not isinstance(scalar, (int, float)):
            reads.append(scalar)
        self.op("dve", lambda e: e.scalar_tensor_tensor(out=out, in0=in0, scalar=scalar, in1=in1, op0=op0, op1=op1),
                reads, [out])

    def copy(self, out, in_, eng="dve"):
        if eng == "act":
            self.act(out, in_, AF.Copy)
        else:
            self.op(eng, lambda e: e.tensor_copy(out=out, in_=in_), [in_], [out])

    def scan(self, out, d0, d1, init, op0, op1):
        self.op("dve", lambda e: e.tensor_tensor_scan(out=out, data0=d0, data1=d1, initial=init, op0=op0, op1=op1),
                [d0, d1], [out])

    def recip(self, out, in_):
        self.op("dve", lambda e: e.reciprocal(out=out, in_=in_), [in_], [out])

    def memset(self, ap, val, eng="dve"):
        self.op(eng, lambda e: e.memset(ap, val), [], [ap])


V_G = 0
V_GB = 48
V_MU = 64
V_W0 = 76
V_A0 = 80
V_KK = 84
V_KA = 88
V_RK = 92
V_LW = 96
V_LB = 100
V_PS = 104
V_OMU = 108
V_HG1 = 120
V_HG5 = 128
NV = 136
NV_IN = 108


def host_consts():
    c = {}
    c["ident"] = np.eye(128, dtype=np.float32)
    bo = np.zeros((128, 128), np.float32)
    bo[:64, :64] = 1.0
    bo[64:, 64:] = 1.0
    c["blockones"] = bo
    c["ones"] = np.ones((128, 128), np.float32)
    s = np.arange(64)[:, None]
    t = np.arange(64)[None, :]
    strict = (s < t).astype(np.float32)
    incl = (s <= t).astype(np.float32)
    m4 = np.concatenate([strict, incl, strict, incl], 1)
    c["maskA"] = np.tile(m4, (1, 2)).copy()
    c["maskNT"] = np.tile(strict.T, (1, 8)).copy()
    c["identM"] = np.tile(np.eye(64, dtype=np.float32), (1, 16)).copy()
    rm = np.ones((128, TT), np.float32)
    rm[:, ::CH] = 0.0
    c["resetmask"] = rm
    ic = np.zeros((128, 4, 16), np.float32)
    for g, w in enumerate((2, 4, 8, 16)):
        ic[:, g, :] = 1.0 / np.minimum(np.arange(1, 17), w)
    c["invc0"] = ic.reshape(128, 64)
    return c


def host_weights(inp):
    L = 0
    w = {}

    def A(wg, wu):
        g = wg.reshape(8, 128, NFU, 128).transpose(2, 1, 0, 3)
        u = wu.reshape(8, 128, NFU, 128).transpose(2, 1, 0, 3)
        return np.ascontiguousarray(np.stack([g, u], 2).reshape(NFU, 128, 2048))

    def B(wd):
        return np.ascontiguousarray(wd.reshape(NFU, 128, 8, 128).transpose(2, 1, 0, 3).reshape(8, 128, DFF))

    w["wA1"] = A(inp["ffn1_gate"][L], inp["ffn1_up"][L])
    w["wB1"] = B(inp["ffn1_down"][L])
    w["wA2"] = A(inp["ffn2_gate"][L], inp["ffn2_up"][L])
    w["wB2"] = B(inp["ffn2_down"][L])
    w["win"] = np.ascontiguousarray(inp["w_in"][L].reshape(8, 128, 32, 128).transpose(2, 1, 0, 3).reshape(32, 128, 1024))
    cat = np.concatenate([inp["decay_a"][L], inp["aaa_a"][L], inp["gate_a"][L]], 1)
    w["la"] = np.ascontiguousarray(cat.reshape(8, 128, 256).transpose(1, 0, 2).reshape(128, 2048))
    mw = inp["mu_wag"][L]
    mucat = np.concatenate([np.broadcast_to(mw[0][:, None], (1024, 64)), np.broadcast_to(mw[1][:, None], (1024, 64)),
                            np.broadcast_to(mw[2][:, None], (1024, 128))], 1)
    w["mula"] = np.ascontiguousarray(mucat.reshape(8, 128, 256).transpose(1, 0, 2).reshape(128, 2048))
    lb1 = np.concatenate([inp["decay_b"][L], inp["aaa_b"][L]], 0)
    w["lb"] = np.ascontiguousarray(np.concatenate([lb1, inp["gate_b"][L]], 1))
    w["poolw"] = np.ascontiguousarray(inp["pool_w"][L].transpose(1, 0, 2).reshape(128, 512))
    br = inp["w_branch_rwkv"][L].reshape(4, 128, 8, 128).transpose(2, 1, 0, 3)
    bp = inp["w_branch_pool"][L].reshape(4, 128, 8, 128).transpose(2, 1, 0, 3)
    w["wbr"] = np.ascontiguousarray(np.concatenate([br, bp], 2).reshape(8, 128, 1024))
    w["wo"] = np.ascontiguousarray(inp["w_out"][L].reshape(8, 128, 8, 128).transpose(2, 1, 0, 3).reshape(8, 128, 1024))
    v = np.zeros((128, NV_IN), np.float32)

    def put(col, vec):
        n = vec.shape[0] // 128
        v[:, col:col + n] = vec.reshape(n, 128).T

    for i in range(6):
        put(V_G + i * 8, inp["norm_gains"][L][i])
    for b in range(2):
        put(V_GB + b * 8, inp["gate_bias"][L][b])
    for i in range(3):
        put(V_MU + i * 4, inp["mu_rkv"][L][i])
    put(V_W0, inp["w0"][L])
    put(V_A0, inp["a0"][L])
    put(V_KK, inp["k_k"][L])
    put(V_KA, inp["k_a"][L])
    put(V_RK, inp["r_k"][L].reshape(512))
    put(V_LW, inp["ln_x_w"][L])
    put(V_LB, inp["ln_x_b"][L])
    put(V_PS, inp["pool_scale"][L])
    w["vecs"] = v
    return w


DRAM_IN = {
    "wA1": [NFU, 128, 2048], "wB1": [8, 128, DFF], "wA2": [NFU, 128, 2048], "wB2": [8, 128, DFF],
    "win": [32, 128, 1024], "la": [128, 2048], "mula": [128, 2048], "lb": [128, 1024], "poolw": [128, 512],
    "wbr": [8, 128, 1024], "wo": [8, 128, 1024], "vecs": [128, NV_IN],
    "ident": [128, 128], "blockones": [128, 128], "ones": [128, 128], "maskA": [64, 512], "maskNT": [64, 512],
    "identM": [64, 1024], "resetmask": [128, TT], "invc0": [128, 64],
}


class StopBuild(Exception):
    pass


def build(S, dbg=None, stop_after=None):
    NT = S // TT

    def chk(name):
        if stop_after == name:
            raise StopBuild()

    nc = bass.Bass("TRN2", target_bir_lowering=False)
    dr = {}
    dr["xin"] = nc.dram_tensor("xin", [NH, S, D], F32, kind="ExternalInput").ap()
    for name, shp in DRAM_IN.items():
        dr[name] = nc.dram_tensor(name, shp, F32, kind="ExternalInput").ap()
    dr["out"] = nc.dram_tensor("out", [NH, S, D], F32, kind="ExternalOutput").ap()
    dbg_out = {}
    if dbg:
        for name, shp in dbg.items():
            dbg_out[name] = nc.dram_tensor("dbg_" + name, shp, F32, kind="ExternalOutput").ap()

    NCHAN = 24
    with ExitStack() as es:
        P = Prog(nc, NCHAN)

        def sb(name, shape, dt):
            t = es.enter_context(nc.sbuf_tensor("sb_" + name, shape, dt))
            P.tracked.add("sb_" + name)
            return t

        xT = sb("xT", [128, NH, 8, TT], F32)
        hT = sb("hT", [128, NH, 8, 514], BF16)
        SCR = sb("SCR", [128, 76 * 256], F32)
        NSLOT = 4
        wring = [sb(f"wr{i}", [128, 3072], BF16) for i in range(NSLOT)]
        la_cur = sb("la_cur", [128, 2048], BF16)
        la_prev = sb("la_prev", [128, 2048], BF16)
        lb = sb("lb", [128, 1024], BF16)
        poolw = sb("poolw", [128, 512], BF16)
        ident = sb("ident", [128, 128], F32)
        identb = sb("identb", [128, 128], BF16)
        onesb = sb("onesb", [128, 128], BF16)
        bonesb = sb("bonesb", [128, 128], BF16)
        maskA = sb("maskA", [64, 512], F32)
        maskNT = sb("maskNT", [64, 512], F32)
        identM = sb("identM", [64, 1024], BF16)
        resetm = sb("resetm", [128, TT], F32)
        invc0 = sb("invc0", [128, 64], F32)
        vecs = sb("vecs", [128, NV], F32)
        xio = [sb(f"xio{i}", [128, D], F32) for i in range(3)]
        sq = [sb(f"sq{i}", [128, TT], BF16) for i in range(2)]
        sil = [sb(f"sil{i}", [128, TT], BF16) for i in range(2)]
        rstd = sb("rstd", [128, NH, TT], F32)
        rtmp = sb("rtmp", [128, TT], F32)
        lora1 = sb("lora1", [128, NH, 2, TT], BF16)
        hprev = sb("hprev", [128, NH, 8], BF16)
        pcar = sb("pcar", [128, NH, 12], F32)
        poolcar = sb("poolcar", [128, NH, 4, 16], F32)
        Z32 = sb("Z32", [128, NH, 4, 64], F32)
        Zb = sb("Zb", [128, NH, 4, 64], BF16)

        def scr(off_b, nbytes, dt, pattern=None, parts=128, **kw):
            assert off_b % 4 == 0 and nbytes % 4 == 0 and off_b + nbytes <= 76 * 1024
            a = SCR[0:parts, off_b // 4:(off_b + nbytes) // 4]
            if dt != F32:
                a = a.bitcast(dt)
            if pattern:
                a = a.rearrange(pattern, **kw)
            return a

        K = 1024
        hid = scr(0, 44 * K, BF16, "p (h u t) -> p h u t", h=NH, u=NFU)
        f32b = scr(44 * K, 32 * K, F32, "p (h k t) -> p h k t", h=NH, k=8)
        ybuf = scr(0, 8 * K, BF16, "p (h c t) -> p h c t", h=NH, c=4)
        ypool = scr(8 * K, 8 * K, BF16, "p (h c t) -> p h c t", h=NH, c=4)
        eP = scr(16 * K, 2 * K, F32)
        G32 = scr(18 * K, 2 * K, F32)
        bonv = scr(20 * K, 2 * K, F32)
        bk = scr(22 * K, 2 * K, BF16, "p (c two t) -> p c two t", two=2, t=64)
        ar = scr(24 * K, 2 * K, BF16, "p (c two t) -> p c two t", two=2, t=64)
        vT = scr(26 * K, 2 * K, BF16, "p (c x) -> p c x", x=128, parts=64)
        bT = scr(28 * K, 2 * K, BF16, "p (c x) -> p c x", x=128, parts=64)
        kT = scr(30 * K, 2 * K, BF16, "p (c x) -> p c x", x=128, parts=64)
        AM = scr(32 * K, 8 * K, BF16, "p (hd c q t) -> p hd c q t", hd=2, q=4, t=64, parts=64)
        Pst = [scr(40 * K + i * 2304, 2056, F32) for i in range(3)]
        r32 = scr(47 * K, 2 * K, F32)
        k32 = scr(49 * K, 2 * K, F32)
        sw = scr(51 * K, 2 * K, F32)
        a32 = scr(53 * K, 2 * K, F32)
        Lp = scr(55 * K, 2 * K, F32)
        eN = scr(57 * K, 2 * K, F32)
        ePm = scr(59 * K, 2 * K, F32)
        kkn = scr(61 * K, 2 * K, F32)
        kmod = scr(63 * K, 2 * K, F32)
        t1 = scr(65 * K, 2 * K, F32)
        t2 = scr(67 * K, 2 * K, F32)
        vb = scr(69 * K, 1 * K, BF16)
        sqk = scr(70 * K, 1 * K, BF16)
        rbb = scr(71 * K, 1 * K, BF16)
        PM = [scr(40 * K + i * 4 * K, 4 * K, BF16, "p (e two t) -> p e two t", two=2, t=64, parts=64) for i in range(2)]
        PkT = [scr(48 * K + i * 2 * K, 2 * K, BF16, "p (e t) -> p e t", t=64, parts=64) for i in range(2)]
        PV32 = scr(52 * K, 4 * K, F32, "p (e t) -> p e t", t=64, parts=64)
        Pb = scr(56 * K, 256, BF16, "p (hd t) -> p hd t", hd=2, parts=64)
        Ub = scr(56 * K + 256, 256, BF16, "p (hd t) -> p hd t", hd=2, parts=64)
        Tt = scr(56 * K + 512, 256, F32)
        Y1 = scr(57 * K, 2 * K, F32)
        Y32 = scr(59 * K, 2 * K, F32)
        gt1 = scr(61 * K, 2 * K, F32)
        gt2 = scr(63 * K, 2 * K, F32)
        gt3 = scr(65 * K, 2 * K, F32)
        ybf = scr(67 * K, 1 * K, BF16)
        ysq = scr(68 * K, 1 * K, BF16)
        PBs = [scr(40 * K + i * 10 * K, 2112, F32) for i in range(2)]
        PS1s = [scr(43 * K + i * 10 * K, 2112, F32) for i in range(2)]
        PS2s = [scr(46 * K + i * 10 * K, 2112, F32) for i in range(2)]
        pmixs = [scr(49 * K + i * 10 * K, 1 * K, BF16) for i in range(2)]
        mrg = scr(16 * K, 16 * K, BF16, "p (h k t) -> p h k t", h=NH, k=8)
        ms0 = scr(40 * K, 2 * K, F32)
        ms1 = scr(42 * K, 2 * K, F32)
        mm0 = scr(44 * K, 2 * K, F32)
        mm1 = scr(46 * K, 2 * K, F32)
        la_st = scr(0, 8 * K, F32)
        mula_st = scr(8 * K, 8 * K, F32)
        la_t = scr(16 * K, 8 * K, F32)

        ps = []
        for i in range(8):
            t = es.enter_context(nc.psum_tensor(f"ps{i}", [128, 512], F32))
            P.tracked.add(f"ps{i}")
            ps.append(t)

        sems = {e: es.enter_context(nc.semaphore("s_" + e)) for e in ENGS}
        dsems = [es.enter_context(nc.semaphore(f"d{i}")) for i in range(NCHAN)]
        block = es.enter_context(nc.Block())

        MUL, ADD, SUB, MAX = ALU.mult, ALU.add, ALU.subtract, ALU.max
        CH_W = list(range(0, NSLOT))
        CH_X = [NSLOT + i for i in range(3)]
        CH_MISC = NSLOT + 3
        CH_DBG = NSLOT + 4
        rot = {"evac": 0, "xio": 0}

        def vcol(c, n=1):
            return vecs[:, c:c + n]

        def evac_eng():
            rot["evac"] += 1
            return "act" if rot["evac"] % 2 else "dve"

        P.dma("sp", CH_MISC, ident[:, :], dr["ident"][:, :])
        P.dma("sp", CH_MISC, maskA[:, :], dr["maskA"][:, :])
        P.dma("sp", CH_MISC, maskNT[:, :], dr["maskNT"][:, :])
        P.dma("sp", CH_MISC, resetm[:, :], dr["resetmask"][:, :])
        P.dma("sp", CH_MISC, invc0[:, :], dr["invc0"][:, :])
        P.dma("sp", CH_MISC, vecs[:, 0:NV_IN], dr["vecs"][:, :])
        P.dma("sp", CH_MISC, la_st, dr["la"][:, :])
        P.dma("sp", CH_MISC, mula_st, dr["mula"][:, :])
        P.dma("pool", CH_MISC + 2, identb[:, :], dr["ident"][:, :], max_dma_last_dim=4096)
        P.dma("pool", CH_MISC + 2, onesb[:, :], dr["ones"][:, :], max_dma_last_dim=4096)
        P.dma("pool", CH_MISC + 2, bonesb[:, :], dr["blockones"][:, :], max_dma_last_dim=4096)
        P.dma("pool", CH_MISC + 2, identM[:, :], dr["identM"][:, :], max_dma_last_dim=4096)
        P.dma("pool", CH_MISC + 2, lb[:, :], dr["lb"][:, :], max_dma_last_dim=4096)
        P.dma("pool", CH_MISC + 2, poolw[:, :], dr["poolw"][:, :], max_dma_last_dim=4096)
        P.bump(CH_MISC)
        P.bump(CH_MISC + 2)
        P.ts(vcol(V_OMU, 12), vcol(V_MU, 12), -1.0, MUL, 1.0, ADD)
        P.ts(vcol(V_HG1, 8), vcol(V_G + 8, 8), 0.5, MUL)
        P.ts(vcol(V_HG5, 8), vcol(V_G + 40, 8), 0.5, MUL)
        P.tt(la_t, la_st, mula_st, MUL)
        P.copy(la_prev[:, :], la_t)
        P.tt(la_cur[:, :], la_st, la_t, SUB)
        P.memset(hprev[:, :, :], 0.0)
        P.memset(pcar[:, :, :], 0.0)
        P.memset(poolcar[:, :, :, :], 0.0)
        P.memset(Z32[:, :, :, :], 0.0)
        P.memset(Zb[:, :, :, :], 0.0)

        units = []
        for j in range(NT):
            for u in range(NFU):
                units.append((dr["wA1"][u, :, :], 2048))
            for d_ in range(8):
                units.append((dr["wB1"][d_, :, :], DFF))
            for c4 in range(4):
                units.append(("R", c4))
            for g in range(4):
                units.append((dr["win"][12 + g, :, :], 1024))
            for d_ in range(8):
                units.append(("M", d_))
            for d_ in range(8):
                units.append((dr["wo"][d_, :, :], 1024))
            for u in range(NFU):
                units.append((dr["wA2"][u, :, :], 2048))
            for d_ in range(8):
                units.append((dr["wB2"][d_, :, :], DFF))
        ws = {"issued": 0, "next": 0}

        def ws_issue(i):
            slot = wring[i % NSLOT]
            ch = CH_W[i % NSLOT]
            u = units[i]
            if u[0] == "R":
                c4 = u[1]
                for q in range(3):
                    P.dma("pool", ch, slot[:, q * 1024:(q + 1) * 1024], dr["win"][q * 4 + c4, :, :], max_dma_last_dim=4096)
                P.bump(ch)
            elif u[0] == "M":
                d_ = u[1]
                P.dma("pool", ch, slot[:, 0:1024], dr["win"][16 + d_, :, :], max_dma_last_dim=4096)
                P.dma("pool", ch, slot[:, 1024:2048], dr["win"][24 + d_, :, :], max_dma_last_dim=4096)
                P.dma("pool", ch, slot[:, 2048:3072], dr["wbr"][d_, :, :], max_dma_last_dim=4096)
                P.bump(ch)
            else:
                src, n = u
                P.dma("pool", ch, slot[:, 0:n], src, max_dma_last_dim=4096)

        def ws_get():
            i = ws["next"]
            ws["next"] += 1
            while ws["issued"] <= min(len(units) - 1, i + NSLOT - 1):
                ws_issue(ws["issued"])
                ws["issued"] += 1
            return wring[i % NSLOT]

        def dbg_dump(name, src_ap):
            if name in dbg_out:
                P.dma("sp", CH_DBG, dbg_out[name], src_ap)

        def rms_stats(src, h, psn):
            for k in range(8):
                s = sq[k % 2]
                P.act(s[:, :], src[:, h, k, :], AF.Square)
                P.mm(psn[:, :], onesb[:, :], s[:, :], start=(k == 0), stop=(k == 7))

        def rstd_from(psn, h, n, eps):
            P.act(rtmp[:, :], psn[:, :], AF.Sqrt, scale=1.0 / n, bias=eps)
            P.recip(rstd[:, h, :], rtmp[:, :])

        def norm_to_hT(gcol):
            for h in range(NH):
                psn = ps[6 + h]
                rms_stats(xT, h, psn)
                rstd_from(psn, h, D, RMS_EPS)
                for k in range(8):
                    P.stt(hT[:, h, k, 2:514], xT[:, h, k, :], vcol(gcol + k), rstd[:, h, :], MUL, MUL)

        def residual_update(hgcol):
            for h in range(NH):
                rstd_from(ps[6 + h], h, D, RMS_EPS)
            for h in range(NH):
                for k in range(8):
                    P.tt(f32b[:, h, k, :], f32b[:, h, k, :], rstd[:, h, :], MUL)
                    P.stt(xT[:, h, k, :], f32b[:, h, k, :], vcol(hgcol + k), xT[:, h, k, :], MUL, ADD)

        def out_proj_phase(nk, rhs_of):
            for dch in range(8):
                w = ws_get()
                for h in range(NH):
                    pso = ps[4 + (dch * NH + h) % 2]
                    for k in range(nk):
                        P.mm(pso[:, :], w[:, k * 128:(k + 1) * 128], rhs_of(h, k), start=(k == 0), stop=(k == nk - 1))
                    P.act(f32b[:, h, dch, :], pso[:, :], AF.Copy)
            for h in range(NH):
                rms_stats(f32b, h, ps[6 + h])

        def ffn(gcol_in, hgcol_out):
            norm_to_hT(gcol_in)
            chk("ffn_norm")
            for u in range(NFU):
                if u == 1:
                    chk("ffnA0")
                w = ws_get()
                for h in range(NH):
                    i = (u * NH + h) % 2
                    psg, psu = ps[2 * i], ps[2 * i + 1]
                    for k in range(8):
                        P.mm(psg[:, :], w[:, k * 128:(k + 1) * 128], hT[:, h, k, 2:514], start=(k == 0), stop=(k == 7))
                    for k in range(8):
                        P.mm(psu[:, :], w[:, 1024 + k * 128:1024 + (k + 1) * 128], hT[:, h, k, 2:514], start=(k == 0), stop=(k == 7))
                    P.act(sil[i][:, :], psg[:, :], AF.Silu)
                    P.tt(hid[:, h, u, :], psu[:, :], sil[i][:, :], MUL)
            chk("ffnA")
            out_proj_phase(NFU, lambda h, k: hid[:, h, k, :])
            chk("ffnB")
            residual_update(hgcol_out)
            chk("ffn")

        NCk = 4
        TW = NCk * CH
        NE = 2 * NCk

        def half_bufs(hh):
            b0 = 16 * K + hh * 30 * K
            Bf = {}
            Bf["eP"] = scr(b0, 1 * K, F32)
            Bf["G32"] = scr(b0 + 1 * K, 1 * K, F32)
            Bf["bonv"] = scr(b0 + 2 * K, 1 * K, F32)
            Bf["bk"] = scr(b0 + 3 * K, 1 * K, BF16, "p (c two t) -> p c two t", two=2, t=64)
            Bf["ar"] = scr(b0 + 4 * K, 1 * K, BF16, "p (c two t) -> p c two t", two=2, t=64)
            Bf["vT"] = scr(b0 + 5 * K, 1 * K, BF16, "p (c x) -> p c x", x=128, parts=64)
            Bf["bT"] = scr(b0 + 6 * K, 1 * K, BF16, "p (c x) -> p c x", x=128, parts=64)
            Bf["kT"] = scr(b0 + 7 * K, 1 * K, BF16, "p (c x) -> p c x", x=128, parts=64)
            Bf["AM"] = scr(b0 + 8 * K, 4 * K, BF16, "p (hd c q t) -> p hd c q t", hd=2, q=4, t=64, parts=64)
            p0 = b0 + 12 * K
            Bf["Pst"] = [scr(p0 + i_ * 1152, 1032, F32) for i_ in range(3)]
            q0 = p0 + 3456
            names = ["r32", "k32", "sw", "a32", "Lp", "eN", "ePm", "kkn", "kmod", "t1", "t2"]
            for n_, nm in enumerate(names):
                Bf[nm] = scr(q0 + n_ * K, 1 * K, F32)
            q1 = q0 + len(names) * K
            Bf["vb"] = scr(q1, 512, BF16)
            Bf["sqk"] = scr(q1 + 512, 512, BF16)
            Bf["rbb"] = scr(q1 + 1024, 512, BF16)
            assert q1 + 1536 <= b0 + 30 * K
            Bf["PM"] = [scr(p0 + i_ * 2 * K, 2 * K, BF16, "p (e two t) -> p e two t", two=2, t=64, parts=64) for i_ in range(2)]
            Bf["PkT"] = [scr(p0 + 4 * K + i_ * K, 1 * K, BF16, "p (e t) -> p e t", t=64, parts=64) for i_ in range(2)]
            Bf["PV32"] = scr(p0 + 6 * K, 2 * K, F32, "p (e t) -> p e t", t=64, parts=64)
            Bf["Pb"] = scr(p0 + 8 * K, 256, BF16, "p (hd t) -> p hd t", hd=2, parts=64)
            Bf["Ub"] = scr(p0 + 8 * K + 256, 256, BF16, "p (hd t) -> p hd t", hd=2, parts=64)
            Bf["Tt"] = scr(p0 + 8 * K + 512, 256, F32)
            Bf["Y1"] = scr(p0 + 9 * K, 1 * K, F32)
            Bf["Y32"] = scr(p0 + 10 * K, 1 * K, F32)
            Bf["gt1"] = scr(p0 + 11 * K, 1 * K, F32)
            Bf["gt2"] = scr(p0 + 12 * K, 1 * K, F32)
            Bf["gt3"] = scr(p0 + 13 * K, 1 * K, F32)
            Bf["ybf"] = scr(p0 + 14 * K, 512, BF16)
            Bf["ysq"] = scr(p0 + 14 * K + 512, 512, BF16)
            Bf["ps"] = [ps[4 * hh + i_] for i_ in range(4)]
            return Bf

        HB = [half_bufs(0), half_bufs(1)]

        def wkv_gen(j, h, c4, sub, w):
            Bf = HB[h]
            eP, G32, bonv, bk, ar, vT, bT, kT, AM = (Bf[n_] for n_ in ("eP", "G32", "bonv", "bk", "ar", "vT", "bT", "kT", "AM"))
            Pst, r32, k32, sw, a32, Lp, eN, ePm, kkn, kmod, t1, t2, vb, sqk, rbb = (Bf[n_] for n_ in (
                "Pst", "r32", "k32", "sw", "a32", "Lp", "eN", "ePm", "kkn", "kmod", "t1", "t2", "vb", "sqk", "rbb"))
            PM, PkT, PV32, Pb, Ub, Tt, Y1, Y32, gt1, gt2, gt3, ybf, ysq = (Bf[n_] for n_ in (
                "PM", "PkT", "PV32", "Pb", "Ub", "Tt", "Y1", "Y32", "gt1", "gt2", "gt3", "ybf", "ysq"))
            b0_, b1_, b2_, b3_ = Bf["ps"]
            t0 = sub * TW
            W_ = slice(0, TW)
            Zs32 = Z32[:, h, c4, :]
            Zsb = Zb[:, h, c4, :]
            dst = [r32, k32, vb]
            for i in range(3):
                pp = (b0_, b1_)[i % 2]
                for k in range(8):
                    P.mm(pp[:, W_], w[:, i * 1024 + k * 128:i * 1024 + (k + 1) * 128], hT[:, h, k, 2 + t0:2 + t0 + TW], start=(k == 0), stop=(k == 7))
                st = Pst[i]
                P.copy(st[:, 0:1], pcar[:, h, c4 * 3 + i:c4 * 3 + i + 1], eng="dve")
                P.act(st[:, 1:TW + 1], pp[:, W_], AF.Copy)
                P.copy(pcar[:, h, c4 * 3 + i:c4 * 3 + i + 1], st[:, TW:TW + 1], eng="dve")
                P.ts(t1, st[:, 0:TW], vcol(V_MU + i * 4 + c4), MUL)
                P.stt(dst[i], st[:, 1:TW + 1], vcol(V_OMU + i * 4 + c4), t1, MUL, ADD)
                yield
            cs = slice(c4 * 128, (c4 + 1) * 128)
            P.mm(b0_[:, W_], lb[0:64, cs], lora1[0:64, h, 0, t0:t0 + TW])
            P.act(sw, b0_[:, W_], AF.Sigmoid, bias=vcol(V_W0 + c4))
            P.mm(b1_[:, W_], lb[64:128, cs], lora1[64:128, h, 0, t0:t0 + TW])
            P.act(a32, b1_[:, W_], AF.Sigmoid, bias=vcol(V_A0 + c4))
            P.mm(b2_[:, W_], lb[:, 512 + c4 * 128:512 + (c4 + 1) * 128], lora1[:, h, 1, t0:t0 + TW])
            P.act(G32, b2_[:, W_], AF.Copy)
            yield
            P.scan(Lp, resetm[:, W_], sw, 0.0, MUL, ADD)
            P.tt(t2, Lp, sw, SUB)
            P.act(eP, Lp, AF.Exp, scale=-C0)
            P.act(eN, Lp, AF.Exp, scale=C0)
            P.act(ePm, t2, AF.Exp, scale=-C0)
            P.act(sqk, k32, AF.Square, scale=vcol(V_KK + c4))
            P.mm(b1_[:, W_], bonesb[:, :], sqk)
            yield
            P.act(t1, b1_[:, W_], AF.Sqrt)
            P.ts(t1, t1, 1e-12, MAX)
            P.recip(t1, t1)
            P.stt(kkn, k32, vcol(V_KK + c4), t1, MUL, MUL)
            P.ts(t2, a32, -1.0, ADD, vcol(V_KA + c4), MUL)
            P.stt(kmod, t2, 1.0, k32, ADD, MUL)
            yield
            v3 = lambda a: a.rearrange("p (c t) -> p c t", t=64)
            P.tt(bk[:, :, 1, :], v3(kmod), v3(eN), MUL)
            P.tt(t2, kkn, a32, MUL)
            P.tt(bk[:, :, 0, :], v3(t2), v3(eN), MUL)
            P.stt(ar[:, :, 0, :], v3(kkn), -1.0, v3(ePm), MUL, MUL)
            P.tt(ar[:, :, 1, :], v3(r32), v3(eP), MUL)
            P.stt(rbb, r32, vcol(V_RK + c4), kmod, MUL, MUL)
            P.mm(b0_[:, W_], bonesb[:, :], rbb)
            P.tt(bonv, b0_[:, W_], vb, MUL)
            yield
            for (src_of, dstT, pst) in ((lambda c: vb[:, c * 64:(c + 1) * 64], vT, b2_),
                                        (lambda c: bk[:, c, 0, :], bT, b3_),
                                        (lambda c: bk[:, c, 1, :], kT, b1_)):
                pv = pst[0:64, 0:256].bitcast(BF16).rearrange("p (c x) -> p c x", x=128)
                for c in range(NCk):
                    P.tr(pv[:, c, :], src_of(c), identb[:, :], signal=(c == NCk - 1))
                P.copy(dstT[:, :, :], pv[:, :, :], eng=evac_eng())
            yield
            for cp in range(NCk // 2):
                for hd in range(2):
                    pb = hd * 64
                    bank = (b2_, b3_)[hd]
                    pa = bank[0:64, :].rearrange("p (cc x) -> p cc x", cc=2)
                    for cc in range(2):
                        c = cp * 2 + cc
                        rhs = ar[pb:pb + 64, c, :, :].rearrange("p two t -> p (two t)")
                        P.mm(pa[:, cc, 0:128], bk[pb:pb + 64, c, 0, :], rhs, signal=False)
                        P.mm(pa[:, cc, 128:256], bk[pb:pb + 64, c, 1, :], rhs, signal=(cc == 1))
                    P.tt(AM[:, hd, cp * 2:cp * 2 + 2, :, :].rearrange("p c q t -> p (c q t)"), bank[0:64, :], maskA[:, :], MUL)
                yield
            pnt = [b0_[0:64, 0:256].rearrange("p (e t) -> p e t", t=64), b1_[0:64, 0:256].rearrange("p (e t) -> p e t", t=64)]
            for c in range(NCk):
                for hd in range(2):
                    pb = hd * 64
                    P.mm(pnt[hd][:, c, :], ar[pb:pb + 64, c, 0, :], bk[pb:pb + 64, c, 0, :], signal=(c == NCk - 1))
            for hd in range(2):
                P.tt(PkT[0][:, hd * NCk:(hd + 1) * NCk, :].rearrange("p e t -> p (e t)"), (b0_, b1_)[hd][0:64, 0:256], maskNT[:, 0:256], MUL)
            Nview = AM[:, :, :, 0, :].rearrange("p hd c t -> p (hd c) t")
            P.copy(PM[0][:, :, 0, :], Nview, eng="act")
            P.tt(PM[0][:, :, 1, :], Nview, identM[:, 0:NE * 64].rearrange("p (e t) -> p e t", t=64), ADD)
            ppv = b2_[0:64, :].rearrange("p (e t) -> p e t", t=64)
            for hd in range(2):
                for c in range(NCk):
                    P.mm(ppv[:, hd * NCk + c, :], AM[:, hd, c, 2, :], vT[:, c, hd * 64:(hd + 1) * 64], signal=(c == NCk - 1))
            P.copy(PV32[:, :, :], ppv, eng=evac_eng())
            yield
            cur = 0
            for lvl in range(6):
                nxt = 1 - cur
                for sbi in range(2):
                    es_ = range(sbi * NCk, (sbi + 1) * NCk)
                    p1 = (b2_, b3_)[sbi][0:64, :].rearrange("p (e x) -> p e x", x=128)
                    p2 = (b0_, b1_)[sbi][0:64, 0:256].rearrange("p (e t) -> p e t", t=64)
                    sl = slice(sbi * NCk, (sbi + 1) * NCk)
                    if lvl == 0:
                        for i, e in enumerate(es_):
                            P.mm(p1[:, i, 0:64], PkT[cur][:, e, :], PM[cur][:, e, 0, :], signal=(i == NCk - 1))
                        for i, e in enumerate(es_):
                            P.mm(p2[:, i, :], PM[cur][:, e, 0, :], PkT[cur][:, e, :], signal=(i == NCk - 1))
                        P.copy(PM[nxt][:, sl, 0, :], p1[:, :, 0:64], eng="act")
                        P.copy(PM[nxt][:, sl, 1, :], PM[cur][:, sl, 1, :], eng="dve")
                        P.copy(PkT[nxt][:, sl, :], p2, eng="dve")
                    elif lvl < 5:
                        for i, e in enumerate(es_):
                            P.mm(p1[:, i, :], PkT[cur][:, e, :], PM[cur][:, e, :, :].rearrange("p two t -> p (two t)"), signal=(i == NCk - 1))
                        for i, e in enumerate(es_):
                            P.mm(p2[:, i, :], PM[cur][:, e, 0, :], PkT[cur][:, e, :], signal=(i == NCk - 1))
                        P.copy(PM[nxt][:, sl, 0, :], p1[:, :, 0:64], eng="act")
                        P.tt(PM[nxt][:, sl, 1, :], p1[:, :, 64:128], PM[cur][:, sl, 1, :], ADD)
                        P.copy(PkT[nxt][:, sl, :], p2, eng="act")
                    else:
                        for i, e in enumerate(es_):
                            P.mm(p2[:, i, :], PkT[cur][:, e, :], PM[cur][:, e, 1, :], signal=(i == NCk - 1))
                        P.tt(PM[nxt][:, sl, 1, :], p2, PM[cur][:, sl, 1, :], ADD)
                    yield
                cur = nxt
            Mf = PM[cur]
            psc = [b2_[0:64, 0:64], b3_[0:64, 0:64]]
            psc2 = b0_[0:64, 0:128].rearrange("p (hd t) -> p hd t", hd=2)
            psz = b1_
            psya = [b2_, b3_]
            psyb = b1_
            for c in range(NCk):
                yc = slice(256 + c * 64, 256 + (c + 1) * 64)
                for hd in range(2):
                    pb = hd * 64
                    P.mm(psc[hd], ar[pb:pb + 64, c, 0, :], Zsb[pb:pb + 64, :], signal=(hd == 1))
                for hd in range(2):
                    pb = hd * 64
                    P.mm(psya[hd][pb:pb + 64, yc], Zsb[pb:pb + 64, :], ar[pb:pb + 64, c, 1, :], signal=(hd == 1))
                for hd in range(2):
                    P.tt(Pb[:, hd, :], psc[hd], PV32[:, hd * NCk + c, :], ADD)
                yield
                for hd in range(2):
                    P.mm(psc2[:, hd, :], Mf[:, hd * NCk + c, 1, :], Pb[:, hd, :], signal=(hd == 1))
                P.copy(Ub[:, :, :], psc2, eng="act")
                yield
                for hd in range(2):
                    pb = hd * 64
                    P.mm(psz[pb:pb + 64, 0:64], bT[:, c, pb:pb + 64], Ub[:, hd, :], start=True, stop=False, signal=False)
                    P.mm(psz[pb:pb + 64, 0:64], kT[:, c, pb:pb + 64], vT[:, c, pb:pb + 64], start=False, stop=True, signal=(hd == 1))
                for hd in range(2):
                    pb = hd * 64
                    P.mm(psyb[pb:pb + 64, yc], Ub[:, hd, :], AM[:, hd, c, 1, :], start=True, stop=False, signal=False)
                    P.mm(psyb[pb:pb + 64, yc], vT[:, c, pb:pb + 64], AM[:, hd, c, 3, :], start=False, stop=True, signal=(hd == 1))
                wc = eP[:, c * 64 + 63:c * 64 + 64]
                P.tt(Tt, psz[:, 0:64], Zs32, ADD)
                P.act(Zsb, Tt, AF.Copy, scale=wc)
                P.ts(Zs32, Tt, wc, MUL)
                yield
            for hd in range(2):
                pb = hd * 64
                P.act(Y1[pb:pb + 64, :], psya[hd][pb:pb + 64, 256:512], AF.Copy)
            P.tt(Y32, psyb[:, 256:512], Y1, ADD)
            P.act(ybf, Y32, AF.Copy)
            P.act(ysq, Y32, AF.Square)
            yield
            P.mm(b0_[:, W_], bonesb[:, :], ybf)
            P.mm(b2_[:, W_], bonesb[:, :], ysq)
            P.act(gt1, b0_[:, W_], AF.Copy, scale=1.0 / 64)
            P.tt(gt2, gt1, gt1, MUL)
            P.stt(gt2, b2_[:, W_], 1.0 / 64, gt2, MUL, SUB)
            P.ts(gt2, gt2, 0.0, MAX, GN_EPS, ADD)
            P.act(gt2, gt2, AF.Sqrt)
            P.recip(gt2, gt2)
            yield
            P.tt(gt3, Y32, gt1, SUB)
            P.tt(gt3, gt3, gt2, MUL)
            P.ts(gt3, gt3, vcol(V_LW + c4), MUL, vcol(V_LB + c4), ADD)
            P.tt(gt3, gt3, bonv, ADD)
            P.tt(ybuf[:, h, c4, t0:t0 + TW], gt3, G32, MUL)
            yield

        def run_lockstep(gens):
            live = list(gens)
            while live:
                nxt_live = []
                for g_ in live:
                    try:
                        next(g_)
                        nxt_live.append(g_)
                    except StopIteration:
                        pass
                live = nxt_live

        def mixer(j):
            norm_to_hT(V_G + 16)
            for h in range(NH):
                P.copy(hT[:, h, :, 1:2], hprev[:, h, :].unsqueeze(2), eng="dve")
                P.copy(hprev[:, h, :].unsqueeze(2), hT[:, h, :, 513:514], eng="dve")
            for h in range(NH):
                for part in range(2):
                    pp = ps[part]
                    cs0 = part * 128
                    n = 0
                    for k in range(8):
                        P.mm(pp[:, :], la_cur[:, k * 256 + cs0:k * 256 + cs0 + 128], hT[:, h, k, 2:514], start=(n == 0), stop=False)
                        n += 1
                        P.mm(pp[:, :], la_prev[:, k * 256 + cs0:k * 256 + cs0 + 128], hT[:, h, k, 1:513], start=False, stop=(k == 7))
                    if part == 0:
                        P.act(lora1[0:64, h, 0, :], pp[0:64, :], AF.Tanh)
                        P.act(lora1[64:128, h, 0, :], pp[64:128, :], AF.Copy)
                    else:
                        P.act(lora1[:, h, 1, :], pp[:, :], AF.Sigmoid)
            chk("lora_a")
            for c4 in range(4):
                w = ws_get()
                for sub in range(TT // TW):
                    run_lockstep([wkv_gen(j, h, c4, sub, w) for h in range(NH)])
            chk("wkv")
            for g, wd in enumerate((2, 4, 8, 16)):
                w = ws_get()
                nlev = g + 1
                for h in range(NH):
                    PB, PS1, PS2, pmix = PBs[h], PS1s[h], PS2s[h], pmixs[h]
                    pp = ps[h % 2]
                    for k in range(8):
                        P.mm(pp[:, :], w[:, k * 128:(k + 1) * 128], hT[:, h, k, 2:514], start=(k == 0), stop=(k == 7))
                    P.copy(PB[:, 0:16], poolcar[:, h, g, :], eng="dve")
                    P.act(PB[:, 16:528], pp[:, :], AF.Copy)
                    P.copy(poolcar[:, h, g, :], PB[:, 512:528], eng="dve")
                    src, lo = PB, 0
                    bufs = [PS1, PS2]
                    for lv in range(nlev):
                        sh = 1 << lv
                        dstb = bufs[lv % 2]
                        nlo = lo + sh
                        P.tt(dstb[:, nlo:528], src[:, nlo:528], src[:, nlo - sh:528 - sh], ADD)
                        src, lo = dstb, nlo
                    P.stt(pmix, src[:, 16:528], 1.0 / wd, PB[:, 16:528], MUL, SUB)
                    if j == 0:
                        P.tt(t1[:, 0:16], src[:, 16:32], invc0[:, g * 16:(g + 1) * 16], MUL)
                        P.tt(pmix[:, 0:16], t1[:, 0:16], PB[:, 16:32], SUB)
                    pq = ps[2 + h % 2]
                    P.mm(pq[:, :], poolw[:, g * 128:(g + 1) * 128], pmix)
                    P.act(ypool[:, h, g, :], pq[:, :], AF.Copy, scale=vcol(V_PS + g))
            chk("pool")
            for dch in range(8):
                w = ws_get()
                for h in range(NH):
                    b0 = (h % 2) * 4
                    pg0, pg1, pbr, pbp = ps[b0], ps[b0 + 1], ps[b0 + 2], ps[b0 + 3]
                    for k in range(8):
                        P.mm(pg0[:, :], w[:, k * 128:(k + 1) * 128], hT[:, h, k, 2:514], start=(k == 0), stop=(k == 7))
                    for k in range(8):
                        P.mm(pg1[:, :], w[:, 1024 + k * 128:1024 + (k + 1) * 128], hT[:, h, k, 2:514], start=(k == 0), stop=(k == 7))
                    for k in range(4):
                        P.mm(pbr[:, :], w[:, 2048 + k * 128:2048 + (k + 1) * 128], ybuf[:, h, k, :], start=(k == 0), stop=(k == 3))
                    for k in range(4):
                        P.mm(pbp[:, :], w[:, 2560 + k * 128:2560 + (k + 1) * 128], ypool[:, h, k, :], start=(k == 0), stop=(k == 3))
                    P.act(ms0, pg0[:, :], AF.Sigmoid, bias=vcol(V_GB + dch))
                    P.act(ms1, pg1[:, :], AF.Sigmoid, bias=vcol(V_GB + 8 + dch))
                    P.tt(mm0, pbr[:, :], ms0, MUL)
                    P.tt(mm1, pbp[:, :], ms1, MUL)
                    P.tt(mrg[:, h, dch, :], mm0, mm1, ADD)
            chk("merge")
            out_proj_phase(8, lambda h, k: mrg[:, h, k, :])
            residual_update(V_G + 24)
            chk("mixer")

        def main_loop():
          for j in range(NT):
            for h in range(NH):
                for tb in range(4):
                    si = rot["xio"] % 3
                    rot["xio"] += 1
                    xs = xio[si]
                    P.dma("sp", CH_X[si], xs[:, :], dr["xin"][h, j * TT + tb * 128:j * TT + (tb + 1) * 128, :])
                    for half in range(2):
                        pst = ps[(tb * 2 + half) % 4]
                        for kk in range(4):
                            k = half * 4 + kk
                            P.tr(pst[:, kk * 128:(kk + 1) * 128], xs[:, k * 128:(k + 1) * 128], ident[:, :], signal=(kk == 3))
                        P.copy(xT[:, h, half * 4:half * 4 + 4, tb * 128:(tb + 1) * 128],
                               pst[:, :].rearrange("p (k t) -> p k t", t=128), eng=evac_eng())
            chk("load")
            ffn(V_G + 0, V_HG1)
            if j == 0:
                dbg_dump("x1", xT[:, :, :, :])
            mixer(j)
            if j == 0:
                dbg_dump("ybuf", ybuf)
                dbg_dump("x2", xT[:, :, :, :])
            ffn(V_G + 32, V_HG5)
            for h in range(NH):
                for tb in range(4):
                    si = rot["xio"] % 3
                    rot["xio"] += 1
                    xs = xio[si]
                    for half in range(2):
                        pst = ps[(tb * 2 + half) % 4]
                        for kk in range(4):
                            k = half * 4 + kk
                            P.tr(pst[:, kk * 128:(kk + 1) * 128], xT[:, h, k, tb * 128:(tb + 1) * 128], ident[:, :], signal=(kk == 3))
                        P.copy(xs[:, half * 512:(half + 1) * 512], pst[:, :], eng=evac_eng())
                    P.dma("sp", CH_X[si], dr["out"][h, j * TT + tb * 128:j * TT + (tb + 1) * 128, :], xs[:, :])
        try:
            main_loop()
            assert ws["next"] == len(units), (ws["next"], len(units))
        except StopBuild:
            print("[kernel] build stopped after", stop_after, flush=True)
        P.finish("sp")
        P.emit(block, sems, dsems)
        print(f"[kernel] S={S} ops={P.nops} per-engine={ {e: len(P.ops[e]) for e in ENGS} }", flush=True)
    return nc


_CACHE = {}


def run(inputs, S, ncores, dbg=None, stop_after=None):
    x = np.asarray(inputs["x"], np.float32)
    inp = {k: np.asarray(v, np.float32) for k, v in inputs.items() if k != "x"}
    shared = host_weights(inp)
    shared.update(host_consts())
    key = (S, tuple(sorted(dbg.items())) if dbg else None)
    nc = build(S, dbg, stop_after)
    in_maps = []
    for c in range(ncores):
        m = dict(shared)
        m["xin"] = np.ascontiguousarray(x[c * NH:(c + 1) * NH, :S])
        in_maps.append(m)
    res = run_bass_kernel_spmd(nc, in_maps, core_ids=list(range(ncores)))
    out = np.concatenate([r["out"] for r in res.results], 0)
    return out, res


def kernel(**inputs):
    out, _ = run(inputs, 2048, NCORES)
    return out.astype(np.float32)
```

```python
import numpy as np
from contextlib import ExitStack
import concourse.bass as bass
import concourse.mybir as mybir
from concourse.bass_utils import run_bass_kernel_spmd

F32 = mybir.dt.float32
BF16 = mybir.dt.bfloat16
AF = mybir.ActivationFunctionType
ALU = mybir.AluOpType
ESZ = {F32: 4, BF16: 2}

ENGS = ("pe", "act", "dve", "pool", "sp")
GRAN = 256
NCORES = 8
NH = 2
TT = 512
D = 1024
DFF = 2816
NFU = 22
CH = 64
C0 = float(np.exp(-0.5))
RMS_EPS = 1e-6
GN_EPS = 64e-5


class Prog:
    def __init__(self, nc, n_dma_chan):
        self.nc = nc
        self.ops = {e: [] for e in ENGS}
        self.cnt = {e: 0 for e in ENGS}
        self.pending = {e: False for e in ENGS}
        self.last_w = {}
        self.readers = {}
        self.water = {e: {} for e in ENGS}
        self.dcnt = [0] * n_dma_chan
        self.n_dma_chan = n_dma_chan
        self.tracked = set()
        self.nops = 0

    def keys(self, ap):
        name = ap.tensor.name
        if name not in self.tracked:
            return ()
        esz = ESZ[ap.dtype]
        pat = ap.ap
        ps = pat[0][0]
        off = ap.offset % ps if ps > 0 else ap.offset
        span = 1
        for st, n in pat[1:]:
            span += (n - 1) * abs(st)
        lo = off * esz
        hi = (off + span) * esz
        return [(name, g) for g in range(lo // GRAN, (hi - 1) // GRAN + 1)]

    def _collect(self, eng, rkeys, wkeys):
        deps = {}

        def add(d, raw):
            k, v, pe = d
            if pe == eng and not raw:
                return
            if deps.get(k, 0) < v:
                deps[k] = v

        lw = self.last_w
        for key in rkeys:
            d = lw.get(key)
            if d is not None:
                add(d, True)
        for key in wkeys:
            d = lw.get(key)
            if d is not None:
                add(d, False)
            for d in self.readers.get(key, ()):
                add(d, False)
        out = []
        wm = self.water[eng]
        for k, v in deps.items():
            if wm.get(k, 0) < v:
                wm[k] = v
                out.append((k, v))
        return out

    def _record(self, dep, rkeys, wkeys):
        for key in rkeys:
            lst = self.readers.setdefault(key, [])
            for i, d in enumerate(lst):
                if d[0] == dep[0]:
                    lst[i] = dep
                    break
            else:
                lst.append(dep)
        for key in wkeys:
            self.last_w[key] = dep
            self.readers[key] = []

    def _rw(self, reads, writes):
        rk = []
        for a in reads:
            rk.extend(self.keys(a))
        wk = []
        for a in writes:
            wk.extend(self.keys(a))
        return rk, wk

    def op(self, eng, fn, reads, writes, signal=True):
        signal = True
        rk, wk = self._rw(reads, writes)
        waits = self._collect(eng, rk, wk)
        if signal:
            self.cnt[eng] += 1
            dep = (eng, self.cnt[eng], eng)
            self.pending[eng] = False
        else:
            dep = (eng, self.cnt[eng] + 1, eng)
            self.pending[eng] = True
        self._record(dep, rk, wk)
        self.ops[eng].append((waits, fn, "e" if signal else None))
        self.nops += 1

    def dma(self, eng, chan, out, in_, **kw):
        rk, wk = self._rw([in_], [out])
        waits = self._collect(eng, rk, wk)
        self.dcnt[chan] += 16
        dep = (("dma", chan), self.dcnt[chan], "dma")
        self._record(dep, rk, wk)
        self.ops[eng].append((waits, lambda e: e.dma_start(out=out, in_=in_, **kw), ("dma", chan)))
        self.nops += 1

    def bump(self, chan):
        k = ("dma", chan)
        full = (k, self.dcnt[chan], "dma")
        for key, dep in self.last_w.items():
            if dep[0] == k:
                self.last_w[key] = full

    def finish(self, eng="sp"):
        waits = []
        for e in ENGS:
            if e != eng and self.cnt[e] > 0:
                waits.append((e, self.cnt[e]))
        for c in range(self.n_dma_chan):
            if self.dcnt[c] > 0:
                waits.append((("dma", c), self.dcnt[c]))
        self.ops[eng].append((waits, None, None))

    def emit(self, block, sems, dsems):
        for e in ENGS:
            assert not self.pending[e], f"unsignalled tail on {e}"

        def semof(k):
            return dsems[k[1]] if isinstance(k, tuple) else sems[k]

        def run(name, engine):
            sem = sems[name]
            for waits, fn, inc in self.ops[name]:
                for k, v in waits:
                    engine.wait_ge(semof(k), v)
                if fn is None:
                    continue
                ins = fn(engine)
                if inc is None:
                    continue
                if inc == "e":
                    ins.then_inc(sem, 1)
                else:
                    ins.then_inc(dsems[inc[1]], 16)

        block.tensor(lambda e: run("pe", e))
        block.scalar(lambda e: run("act", e))
        block.vector(lambda e: run("dve", e))
        block.gpsimd(lambda e: run("pool", e))
        block.sync(lambda e: run("sp", e))

    def mm(self, out, lhsT, rhs, start=True, stop=True, signal=True):
        self.op("pe", lambda e: e.matmul(out, lhsT=lhsT, rhs=rhs, start=start, stop=stop),
                [lhsT, rhs], [out], signal)

    def tr(self, out, in_, ident, signal=True):
        self.op("pe", lambda e: e.transpose(out, in_, ident), [in_, ident], [out], signal)

    def act(self, out, in_, func, bias=None, scale=None, eng="act"):
        reads = [in_]
        kw = {}
        if bias is not None:
            kw["bias"] = bias
            if not isinstance(bias, (int, float)):
                reads.append(bias)
        if scale is not None:
            kw["scale"] = scale
            if not isinstance(scale, (int, float)):
                reads.append(scale)
        self.op(eng, lambda e: e.activation(out=out, in_=in_, func=func, **kw), reads, [out])

    def tt(self, out, in0, in1, op, eng="dve"):
        self.op(eng, lambda e: e.tensor_tensor(out=out, in0=in0, in1=in1, op=op), [in0, in1], [out])

    def ts(self, out, in0, s1, op0, s2=None, op1=None, eng="dve"):
        reads = [in0]
        for s in (s1, s2):
            if s is not None and not isinstance(s, (int, float)):
                reads.append(s)
        if op1 is None:
            fn = lambda e: e.tensor_scalar(out=out, in0=in0, scalar1=s1, scalar2=None, op0=op0)
        else:
            fn = lambda e: e.tensor_scalar(out=out, in0=in0, scalar1=s1, scalar2=s2, op0=op0, op1=op1)
        self.op(eng, fn, reads, [out])

    def stt(self, out, in0, scalar, in1, op0, op1):
        reads = [in0, in1]
        if not isinstance(scalar, (int, float)):
            reads.append(scalar)
        self.op("dve", lambda e: e.scalar_tensor_tensor(out=out, in0=in0, scalar=scalar, in1=in1, op0=op0, op1=op1),
                reads, [out])

    def copy(self, out, in_, eng="dve"):
        if eng == "act":
            self.act(out, in_, AF.Copy)
        else:
            self.op(eng, lambda e: e.tensor_copy(out=out, in_=in_), [in_], [out])

    def scan(self, out, d0, d1, init, op0, op1):
        self.op("dve", lambda e: e.tensor_tensor_scan(out=out, data0=d0, data1=d1, initial=init, op0=op0, op1=op1),
                [d0, d1], [out])

    def recip(self, out, in_):
        self.op("dve", lambda e: e.reciprocal(out=out, in_=in_), [in_], [out])

    def memset(self, ap, val, eng="dve"):
        self.op(eng, lambda e: e.memset(ap, val), [], [ap])


V_G = 0
V_GB = 48
V_MU = 64
V_W0 = 76
V_A0 = 80
V_KK = 84
V_KA = 88
V_RK = 92
V_LW = 96
V_LB = 100
V_PS = 104
V_OMU = 108
V_HG1 = 120
V_HG5 = 128
NV = 136
NV_IN = 108


def host_consts():
    c = {}
    c["ident"] = np.eye(128, dtype=np.float32)
    bo = np.zeros((128, 128), np.float32)
    bo[:64, :64] = 1.0
    bo[64:, 64:] = 1.0
    c["blockones"] = bo
    c["ones"] = np.ones((128, 128), np.float32)
    s = np.arange(64)[:, None]
    t = np.arange(64)[None, :]
    strict = (s < t).astype(np.float32)
    incl = (s <= t).astype(np.float32)
    m4 = np.concatenate([strict, incl, strict, incl], 1)
    c["maskA"] = np.tile(m4, (1, 2)).copy()
    c["maskNT"] = np.tile(strict.T, (1, 8)).copy()
    c["identM"] = np.tile(np.eye(64, dtype=np.float32), (1, 16)).copy()
    rm = np.ones((128, TT), np.float32)
    rm[:, ::CH] = 0.0
    c["resetmask"] = rm
    ic = np.zeros((128, 4, 16), np.float32)
    for g, w in enumerate((2, 4, 8, 16)):
        ic[:, g, :] = 1.0 / np.minimum(np.arange(1, 17), w)
    c["invc0"] = ic.reshape(128, 64)
    return c


def host_weights(inp):
    L = 0
    w = {}

    def A(wg, wu):
        g = wg.reshape(8, 128, NFU, 128).transpose(2, 1, 0, 3)
        u = wu.reshape(8, 128, NFU, 128).transpose(2, 1, 0, 3)
        return np.ascontiguousarray(np.stack([g, u], 2).reshape(NFU, 128, 2048))

    def B(wd):
        return np.ascontiguousarray(wd.reshape(NFU, 128, 8, 128).transpose(2, 1, 0, 3).reshape(8, 128, DFF))

    w["wA1"] = A(inp["ffn1_gate"][L], inp["ffn1_up"][L])
    w["wB1"] = B(inp["ffn1_down"][L])
    w["wA2"] = A(inp["ffn2_gate"][L], inp["ffn2_up"][L])
    w["wB2"] = B(inp["ffn2_down"][L])
    w["win"] = np.ascontiguousarray(inp["w_in"][L].reshape(8, 128, 32, 128).transpose(2, 1, 0, 3).reshape(32, 128, 1024))
    cat = np.concatenate([inp["decay_a"][L], inp["aaa_a"][L], inp["gate_a"][L]], 1)
    w["la"] = np.ascontiguousarray(cat.reshape(8, 128, 256).transpose(1, 0, 2).reshape(128, 2048))
    mw = inp["mu_wag"][L]
    mucat = np.concatenate([np.broadcast_to(mw[0][:, None], (1024, 64)), np.broadcast_to(mw[1][:, None], (1024, 64)),
                            np.broadcast_to(mw[2][:, None], (1024, 128))], 1)
    w["mula"] = np.ascontiguousarray(mucat.reshape(8, 128, 256).transpose(1, 0, 2).reshape(128, 2048))
    lb1 = np.concatenate([inp["decay_b"][L], inp["aaa_b"][L]], 0)
    w["lb"] = np.ascontiguousarray(np.concatenate([lb1, inp["gate_b"][L]], 1))
    w["poolw"] = np.ascontiguousarray(inp["pool_w"][L].transpose(1, 0, 2).reshape(128, 512))
    br = inp["w_branch_rwkv"][L].reshape(4, 128, 8, 128).transpose(2, 1, 0, 3)
    bp = inp["w_branch_pool"][L].reshape(4, 128, 8, 128).transpose(2, 1, 0, 3)
    w["wbr"] = np.ascontiguousarray(np.concatenate([br, bp], 2).reshape(8, 128, 1024))
    w["wo"] = np.ascontiguousarray(inp["w_out"][L].reshape(8, 128, 8, 128).transpose(2, 1, 0, 3).reshape(8, 128, 1024))
    v = np.zeros((128, NV_IN), np.float32)

    def put(col, vec):
        n = vec.shape[0] // 128
        v[:, col:col + n] = vec.reshape(n, 128).T

    for i in range(6):
        put(V_G + i * 8, inp["norm_gains"][L][i])
    for b in range(2):
        put(V_GB + b * 8, inp["gate_bias"][L][b])
    for i in range(3):
        put(V_MU + i * 4, inp["mu_rkv"][L][i])
    put(V_W0, inp["w0"][L])
    put(V_A0, inp["a0"][L])
    put(V_KK, inp["k_k"][L])
    put(V_KA, inp["k_a"][L])
    put(V_RK, inp["r_k"][L].reshape(512))
    put(V_LW, inp["ln_x_w"][L])
    put(V_LB, inp["ln_x_b"][L])
    put(V_PS, inp["pool_scale"][L])
    w["vecs"] = v
    return w


DRAM_IN = {
    "wA1": [NFU, 128, 2048], "wB1": [8, 128, DFF], "wA2": [NFU, 128, 2048], "wB2": [8, 128, DFF],
    "win": [32, 128, 1024], "la": [128, 2048], "mula": [128, 2048], "lb": [128, 1024], "poolw": [128, 512],
    "wbr": [8, 128, 1024], "wo": [8, 128, 1024], "vecs": [128, NV_IN],
    "ident": [128, 128], "blockones": [128, 128], "ones": [128, 128], "maskA": [64, 512], "maskNT": [64, 512],
    "identM": [64, 1024], "resetmask": [128, TT], "invc0": [128, 64],
}


class StopBuild(Exception):
    pass


def build(S, dbg=None, stop_after=None):
    NT = S // TT

    def chk(name):
        if stop_after == name:
            raise StopBuild()

    nc = bass.Bass("TRN2", target_bir_lowering=False)
    dr = {}
    dr["xin"] = nc.dram_tensor("xin", [NH, S, D], F32, kind="ExternalInput").ap()
    for name, shp in DRAM_IN.items():
        dr[name] = nc.dram_tensor(name, shp, F32, kind="ExternalInput").ap()
    dr["out"] = nc.dram_tensor("out", [NH, S, D], F32, kind="ExternalOutput").ap()
    dbg_out = {}
    if dbg:
        for name, shp in dbg.items():
            dbg_out[name] = nc.dram_tensor("dbg_" + name, shp, F32, kind="ExternalOutput").ap()

    NCHAN = 24
    with ExitStack() as es:
        P = Prog(nc, NCHAN)

        def sb(name, shape, dt):
            t = es.enter_context(nc.sbuf_tensor("sb_" + name, shape, dt))
            P.tracked.add("sb_" + name)
            return t

        xT = sb("xT", [128, NH, 8, TT], F32)
        hT = sb("hT", [128, NH, 8, 514], BF16)
        SCR = sb("SCR", [128, 76 * 256], F32)
        NSLOT = 4
        wring = [sb(f"wr{i}", [128, 3072], BF16) for i in range(NSLOT)]
        la_cur = sb("la_cur", [128, 2048], BF16)
        la_prev = sb("la_prev", [128, 2048], BF16)
        lb = sb("lb", [128, 1024], BF16)
        poolw = sb("poolw", [128, 512], BF16)
        ident = sb("ident", [128, 128], F32)
        identb = sb("identb", [128, 128], BF16)
        onesb = sb("onesb", [128, 128], BF16)
        bonesb = sb("bonesb", [128, 128], BF16)
        maskA = sb("maskA", [64, 512], F32)
        maskNT = sb("maskNT", [64, 512], F32)
        identM = sb("identM", [64, 1024], BF16)
        resetm = sb("resetm", [128, TT], F32)
        invc0 = sb("invc0", [128, 64], F32)
        vecs = sb("vecs", [128, NV], F32)
        xio = [sb(f"xio{i}", [128, D], F32) for i in range(3)]
        sq = [sb(f"sq{i}", [128, TT], BF16) for i in range(2)]
        sil = [sb(f"sil{i}", [128, TT], BF16) for i in range(2)]
        rstd = sb("rstd", [128, NH, TT], F32)
        rtmp = sb("rtmp", [128, TT], F32)
        lora1 = sb("lora1", [128, NH, 2, TT], BF16)
        hprev = sb("hprev", [128, NH, 8], BF16)
        pcar = sb("pcar", [128, NH, 12], F32)
        poolcar = sb("poolcar", [128, NH, 4, 16], F32)
        Z32 = sb("Z32", [128, NH, 4, 64], F32)
        Zb = sb("Zb", [128, NH, 4, 64], BF16)

        def scr(off_b, nbytes, dt, pattern=None, parts=128, **kw):
            assert off_b % 4 == 0 and nbytes % 4 == 0 and off_b + nbytes <= 76 * 1024
            a = SCR[0:parts, off_b // 4:(off_b + nbytes) // 4]
            if dt != F32:
                a = a.bitcast(dt)
            if pattern:
                a = a.rearrange(pattern, **kw)
            return a

        K = 1024
        hid = scr(0, 44 * K, BF16, "p (h u t) -> p h u t", h=NH, u=NFU)
        f32b = scr(44 * K, 32 * K, F32, "p (h k t) -> p h k t", h=NH, k=8)
        ybuf = scr(0, 8 * K, BF16, "p (h c t) -> p h c t", h=NH, c=4)
        ypool = scr(8 * K, 8 * K, BF16, "p (h c t) -> p h c t", h=NH, c=4)
        eP = scr(16 * K, 2 * K, F32)
        G32 = scr(18 * K, 2 * K, F32)
        bonv = scr(20 * K, 2 * K, F32)
        bk = scr(22 * K, 2 * K, BF16, "p (c two t) -> p c two t", two=2, t=64)
        ar = scr(24 * K, 2 * K, BF16, "p (c two t) -> p c two t", two=2, t=64)
        vT = scr(26 * K, 2 * K, BF16, "p (c x) -> p c x", x=128, parts=64)
        bT = scr(28 * K, 2 * K, BF16, "p (c x) -> p c x", x=128, parts=64)
        kT = scr(30 * K, 2 * K, BF16, "p (c x) -> p c x", x=128, parts=64)
        AM = scr(32 * K, 8 * K, BF16, "p (hd c q t) -> p hd c q t", hd=2, q=4, t=64, parts=64)
        Pst = [scr(40 * K + i * 2304, 2056, F32) for i in range(3)]
        r32 = scr(47 * K, 2 * K, F32)
        k32 = scr(49 * K, 2 * K, F32)
        sw = scr(51 * K, 2 * K, F32)
        a32 = scr(53 * K, 2 * K, F32)
        Lp = scr(55 * K, 2 * K, F32)
        eN = scr(57 * K, 2 * K, F32)
        ePm = scr(59 * K, 2 * K, F32)
        kkn = scr(61 * K, 2 * K, F32)
        kmod = scr(63 * K, 2 * K, F32)
        t1 = scr(65 * K, 2 * K, F32)
        t2 = scr(67 * K, 2 * K, F32)
        vb = scr(69 * K, 1 * K, BF16)
        sqk = scr(70 * K, 1 * K, BF16)
        rbb = scr(71 * K, 1 * K, BF16)
        PM = [scr(40 * K + i * 4 * K, 4 * K, BF16, "p (e two t) -> p e two t", two=2, t=64, parts=64) for i in range(2)]
        PkT = [scr(48 * K + i * 2 * K, 2 * K, BF16, "p (e t) -> p e t", t=64, parts=64) for i in range(2)]
        PV32 = scr(52 * K, 4 * K, F32, "p (e t) -> p e t", t=64, parts=64)
        Pb = scr(56 * K, 256, BF16, "p (hd t) -> p hd t", hd=2, parts=64)
        Ub = scr(56 * K + 256, 256, BF16, "p (hd t) -> p hd t", hd=2, parts=64)
        Tt = scr(56 * K + 512, 256, F32)
        Y1 = scr(57 * K, 2 * K, F32)
        Y32 = scr(59 * K, 2 * K, F32)
        gt1 = scr(61 * K, 2 * K, F32)
        gt2 = scr(63 * K, 2 * K, F32)
        gt3 = scr(65 * K, 2 * K, F32)
        ybf = scr(67 * K, 1 * K, BF16)
        ysq = scr(68 * K, 1 * K, BF16)
        PBs = [scr(40 * K + i * 10 * K, 2112, F32) for i in range(2)]
        PS1s = [scr(43 * K + i * 10 * K, 2112, F32) for i in range(2)]
        PS2s = [scr(46 * K + i * 10 * K, 2112, F32) for i in range(2)]
        pmixs = [scr(49 * K + i * 10 * K, 1 * K, BF16) for i in range(2)]
        mrg = scr(16 * K, 16 * K, BF16, "p (h k t) -> p h k t", h=NH, k=8)
        ms0 = scr(40 * K, 2 * K, F32)
        ms1 = scr(42 * K, 2 * K, F32)
        mm0 = scr(44 * K, 2 * K, F32)
        mm1 = scr(46 * K, 2 * K, F32)
        la_st = scr(0, 8 * K, F32)
        mula_st = scr(8 * K, 8 * K, F32)
        la_t = scr(16 * K, 8 * K, F32)

        ps = []
        for i in range(8):
            t = es.enter_context(nc.psum_tensor(f"ps{i}", [128, 512], F32))
            P.tracked.add(f"ps{i}")
            ps.append(t)

        sems = {e: es.enter_context(nc.semaphore("s_" + e)) for e in ENGS}
        dsems = [es.enter_context(nc.semaphore(f"d{i}")) for i in range(NCHAN)]
        block = es.enter_context(nc.Block())

        MUL, ADD, SUB, MAX = ALU.mult, ALU.add, ALU.subtract, ALU.max
        CH_W = list(range(0, NSLOT))
        CH_X = [NSLOT + i for i in range(3)]
        CH_MISC = NSLOT + 3
        CH_DBG = NSLOT + 4
        rot = {"evac": 0, "xio": 0}

        def vcol(c, n=1):
            return vecs[:, c:c + n]

        def evac_eng():
            rot["evac"] += 1
            return "act" if rot["evac"] % 2 else "dve"

        P.dma("sp", CH_MISC, ident[:, :], dr["ident"][:, :])
        P.dma("sp", CH_MISC, maskA[:, :], dr["maskA"][:, :])
        P.dma("sp", CH_MISC, maskNT[:, :], dr["maskNT"][:, :])
        P.dma("sp", CH_MISC, resetm[:, :], dr["resetmask"][:, :])
        P.dma("sp", CH_MISC, invc0[:, :], dr["invc0"][:, :])
        P.dma("sp", CH_MISC, vecs[:, 0:NV_IN], dr["vecs"][:, :])
        P.dma("sp", CH_MISC, la_st, dr["la"][:, :])
        P.dma("sp", CH_MISC, mula_st, dr["mula"][:, :])
        P.dma("pool", CH_MISC + 2, identb[:, :], dr["ident"][:, :], max_dma_last_dim=4096)
        P.dma("pool", CH_MISC + 2, onesb[:, :], dr["ones"][:, :], max_dma_last_dim=4096)
        P.dma("pool", CH_MISC + 2, bonesb[:, :], dr["blockones"][:, :], max_dma_last_dim=4096)
        P.dma("pool", CH_MISC + 2, identM[:, :], dr["identM"][:, :], max_dma_last_dim=4096)
        P.dma("pool", CH_MISC + 2, lb[:, :], dr["lb"][:, :], max_dma_last_dim=4096)
        P.dma("pool", CH_MISC + 2, poolw[:, :], dr["poolw"][:, :], max_dma_last_dim=4096)
        P.bump(CH_MISC)
        P.bump(CH_MISC + 2)
        P.ts(vcol(V_OMU, 12), vcol(V_MU, 12), -1.0, MUL, 1.0, ADD)
        P.ts(vcol(V_HG1, 8), vcol(V_G + 8, 8), 0.5, MUL)
        P.ts(vcol(V_HG5, 8), vcol(V_G + 40, 8), 0.5, MUL)
        P.tt(la_t, la_st, mula_st, MUL)
        P.copy(la_prev[:, :], la_t)
        P.tt(la_cur[:, :], la_st, la_t, SUB)
        P.memset(hprev[:, :, :], 0.0)
        P.memset(pcar[:, :, :], 0.0)
        P.memset(poolcar[:, :, :, :], 0.0)
        P.memset(Z32[:, :, :, :], 0.0)
        P.memset(Zb[:, :, :, :], 0.0)

        units = []
        for j in range(NT):
            for u in range(NFU):
                units.append((dr["wA1"][u, :, :], 2048))
            for d_ in range(8):
                units.append((dr["wB1"][d_, :, :], DFF))
            for c4 in range(4):
                units.append(("R", c4))
            for g in range(4):
                units.append((dr["win"][12 + g, :, :], 1024))
            for d_ in range(8):
                units.append(("M", d_))
            for d_ in range(8):
                units.append((dr["wo"][d_, :, :], 1024))
            for u in range(NFU):
                units.append((dr["wA2"][u, :, :], 2048))
            for d_ in range(8):
                units.append((dr["wB2"][d_, :, :], DFF))
        ws = {"issued": 0, "next": 0}

        def ws_issue(i):
            slot = wring[i % NSLOT]
            ch = CH_W[i % NSLOT]
            u = units[i]
            if u[0] == "R":
                c4 = u[1]
                for q in range(3):
                    P.dma("pool", ch, slot[:, q * 1024:(q + 1) * 1024], dr["win"][q * 4 + c4, :, :], max_dma_last_dim=4096)
                P.bump(ch)
            elif u[0] == "M":
                d_ = u[1]
                P.dma("pool", ch, slot[:, 0:1024], dr["win"][16 + d_, :, :], max_dma_last_dim=4096)
                P.dma("pool", ch, slot[:, 1024:2048], dr["win"][24 + d_, :, :], max_dma_last_dim=4096)
                P.dma("pool", ch, slot[:, 2048:3072], dr["wbr"][d_, :, :], max_dma_last_dim=4096)
                P.bump(ch)
            else:
                src, n = u
                P.dma("pool", ch, slot[:, 0:n], src, max_dma_last_dim=4096)

        def ws_get():
            i = ws["next"]
            ws["next"] += 1
            while ws["issued"] <= min(len(units) - 1, i + NSLOT - 1):
                ws_issue(ws["issued"])
                ws["issued"] += 1
            return wring[i % NSLOT]

        def dbg_dump(name, src_ap):
            if name in dbg_out:
                P.dma("sp", CH_DBG, dbg_out[name], src_ap)

        def rms_stats(src, h, psn):
            for k in range(8):
                s = sq[k % 2]
                P.act(s[:, :], src[:, h, k, :], AF.Square)
                P.mm(psn[:, :], onesb[:, :], s[:, :], start=(k == 0), stop=(k == 7))

        def rstd_from(psn, h, n, eps):
            P.act(rtmp[:, :], psn[:, :], AF.Sqrt, scale=1.0 / n, bias=eps)
            P.recip(rstd[:, h, :], rtmp[:, :])

        def norm_to_hT(gcol):
            for h in range(NH):
                psn = ps[6 + h]
                rms_stats(xT, h, psn)
                rstd_from(psn, h, D, RMS_EPS)
                for k in range(8):
                    P.stt(hT[:, h, k, 2:514], xT[:, h, k, :], vcol(gcol + k), rstd[:, h, :], MUL, MUL)

        def residual_update(hgcol):
            for h in range(NH):
                rstd_from(ps[6 + h], h, D, RMS_EPS)
            for h in range(NH):
                for k in range(8):
                    P.tt(f32b[:, h, k, :], f32b[:, h, k, :], rstd[:, h, :], MUL)
                    P.stt(xT[:, h, k, :], f32b[:, h, k, :], vcol(hgcol + k), xT[:, h, k, :], MUL, ADD)

        def out_proj_phase(nk, rhs_of):
            for dch in range(8):
                w = ws_get()
                for h in range(NH):
                    pso = ps[4 + (dch * NH + h) % 2]
                    for k in range(nk):
                        P.mm(pso[:, :], w[:, k * 128:(k + 1) * 128], rhs_of(h, k), start=(k == 0), stop=(k == nk - 1))
                    P.act(f32b[:, h, dch, :], pso[:, :], AF.Copy)
            for h in range(NH):
                rms_stats(f32b, h, ps[6 + h])

        def ffn(gcol_in, hgcol_out):
            norm_to_hT(gcol_in)
            chk("ffn_norm")
            for u in range(NFU):
                if u == 1:
                    chk("ffnA0")
                w = ws_get()
                for h in range(NH):
                    i = (u * NH + h) % 2
                    psg, psu = ps[2 * i], ps[2 * i + 1]
                    for k in range(8):
                        P.mm(psg[:, :], w[:, k * 128:(k + 1) * 128], hT[:, h, k, 2:514], start=(k == 0), stop=(k == 7))
                    for k in range(8):
                        P.mm(psu[:, :], w[:, 1024 + k * 128:1024 + (k + 1) * 128], hT[:, h, k, 2:514], start=(k == 0), stop=(k == 7))
                    P.act(sil[i][:, :], psg[:, :], AF.Silu)
                    P.tt(hid[:, h, u, :], psu[:, :], sil[i][:, :], MUL)
            chk("ffnA")
            out_proj_phase(NFU, lambda h, k: hid[:, h, k, :])
            chk("ffnB")
            residual_update(hgcol_out)
            chk("ffn")

        NCk = 4
        TW = NCk * CH
        NE = 2 * NCk

        def half_bufs(hh):
            b0 = 16 * K + hh * 30 * K
            Bf = {}
            Bf["eP"] = scr(b0, 1 * K, F32)
            Bf["G32"] = scr(b0 + 1 * K, 1 * K, F32)
            Bf["bonv"] = scr(b0 + 2 * K, 1 * K, F32)
            Bf["bk"] = scr(b0 + 3 * K, 1 * K, BF16, "p (c two t) -> p c two t", two=2, t=64)
            Bf["ar"] = scr(b0 + 4 * K, 1 * K, BF16, "p (c two t) -> p c two t", two=2, t=64)
            Bf["vT"] = scr(b0 + 5 * K, 1 * K, BF16, "p (c x) -> p c x", x=128, parts=64)
            Bf["bT"] = scr(b0 + 6 * K, 1 * K, BF16, "p (c x) -> p c x", x=128, parts=64)
            Bf["kT"] = scr(b0 + 7 * K, 1 * K, BF16, "p (c x) -> p c x", x=128, parts=64)
            Bf["AM"] = scr(b0 + 8 * K, 4 * K, BF16, "p (hd c q t) -> p hd c q t", hd=2, q=4, t=64, parts=64)
            p0 = b0 + 12 * K
            Bf["Pst"] = [scr(p0 + i_ * 1152, 1032, F32) for i_ in range(3)]
            q0 = p0 + 3456
            names = ["r32", "k32", "sw", "a32", "Lp", "eN", "ePm", "kkn", "kmod", "t1", "t2"]
            for n_, nm in enumerate(names):
                Bf[nm] = scr(q0 + n_ * K, 1 * K, F32)
            q1 = q0 + len(names) * K
            Bf["vb"] = scr(q1, 512, BF16)
            Bf["sqk"] = scr(q1 + 512, 512, BF16)
            Bf["rbb"] = scr(q1 + 1024, 512, BF16)
            assert q1 + 1536 <= b0 + 30 * K
            Bf["PM"] = [scr(p0 + i_ * 2 * K, 2 * K, BF16, "p (e two t) -> p e two t", two=2, t=64, parts=64) for i_ in range(2)]
            Bf["PkT"] = [scr(p0 + 4 * K + i_ * K, 1 * K, BF16, "p (e t) -> p e t", t=64, parts=64) for i_ in range(2)]
            Bf["PV32"] = scr(p0 + 6 * K, 2 * K, F32, "p (e t) -> p e t", t=64, parts=64)
            Bf["Pb"] = scr(p0 + 8 * K, 256, BF16, "p (hd t) -> p hd t", hd=2, parts=64)
            Bf["Ub"] = scr(p0 + 8 * K + 256, 256, BF16, "p (hd t) -> p hd t", hd=2, parts=64)
            Bf["Tt"] = scr(p0 + 8 * K + 512, 256, F32)
            Bf["Y1"] = scr(p0 + 9 * K, 1 * K, F32)
            Bf["Y32"] = scr(p0 + 10 * K, 1 * K, F32)
            Bf["gt1"] = scr(p0 + 11 * K, 1 * K, F32)
            Bf["gt2"] = scr(p0 + 12 * K, 1 * K, F32)
            Bf["gt3"] = scr(p0 + 13 * K, 1 * K, F32)
            Bf["ybf"] = scr(p0 + 14 * K, 512, BF16)
            Bf["ysq"] = scr(p0 + 14 * K + 512, 512, BF16)
            Bf["ps"] = [ps[4 * hh + i_] for i_ in range(4)]
            return Bf

        HB = [half_bufs(0), half_bufs(1)]

        def wkv_gen(j, h, c4, sub, w):
            Bf = HB[h]
            eP, G32, bonv, bk, ar, vT, bT, kT, AM = (Bf[n_] for n_ in ("eP", "G32", "bonv", "bk", "ar", "vT", "bT", "kT", "AM"))
            Pst, r32, k32, sw, a32, Lp, eN, ePm, kkn, kmod, t1, t2, vb, sqk, rbb = (Bf[n_] for n_ in (
                "Pst", "r32", "k32", "sw", "a32", "Lp", "eN", "ePm", "kkn", "kmod", "t1", "t2", "vb", "sqk", "rbb"))
            PM, PkT, PV32, Pb, Ub, Tt, Y1, Y32, gt1, gt2, gt3, ybf, ysq = (Bf[n_] for n_ in (
                "PM", "PkT", "PV32", "Pb", "Ub", "Tt", "Y1", "Y32", "gt1", "gt2", "gt3", "ybf", "ysq"))
            b0_, b1_, b2_, b3_ = Bf["ps"]
            t0 = sub * TW
            W_ = slice(0, TW)
            Zs32 = Z32[:, h, c4, :]
            Zsb = Zb[:, h, c4, :]
            dst = [r32, k32, vb]
            for i in range(3):
                pp = (b0_, b1_)[i % 2]
                for k in range(8):
                    P.mm(pp[:, W_], w[:, i * 1024 + k * 128:i * 1024 + (k + 1) * 128], hT[:, h, k, 2 + t0:2 + t0 + TW], start=(k == 0), stop=(k == 7))
                st = Pst[i]
                P.copy(st[:, 0:1], pcar[:, h, c4 * 3 + i:c4 * 3 + i + 1], eng="dve")
                P.act(st[:, 1:TW + 1], pp[:, W_], AF.Copy)
                P.copy(pcar[:, h, c4 * 3 + i:c4 * 3 + i + 1], st[:, TW:TW + 1], eng="dve")
                P.ts(t1, st[:, 0:TW], vcol(V_MU + i * 4 + c4), MUL)
                P.stt(dst[i], st[:, 1:TW + 1], vcol(V_OMU + i * 4 + c4), t1, MUL, ADD)
                yield
            cs = slice(c4 * 128, (c4 + 1) * 128)
            P.mm(b0_[:, W_], lb[0:64, cs], lora1[0:64, h, 0, t0:t0 + TW])
            P.act(sw, b0_[:, W_], AF.Sigmoid, bias=vcol(V_W0 + c4))
            P.mm(b1_[:, W_], lb[64:128, cs], lora1[64:128, h, 0, t0:t0 + TW])
            P.act(a32, b1_[:, W_], AF.Sigmoid, bias=vcol(V_A0 + c4))
            P.mm(b2_[:, W_], lb[:, 512 + c4 * 128:512 + (c4 + 1) * 128], lora1[:, h, 1, t0:t0 + TW])
            P.act(G32, b2_[:, W_], AF.Copy)
            yield
            P.scan(Lp, resetm[:, W_], sw, 0.0, MUL, ADD)
            P.tt(t2, Lp, sw, SUB)
            P.act(eP, Lp, AF.Exp, scale=-C0)
            P.act(eN, Lp, AF.Exp, scale=C0)
            P.act(ePm, t2, AF.Exp, scale=-C0)
            P.act(sqk, k32, AF.Square, scale=vcol(V_KK + c4))
            P.mm(b1_[:, W_], bonesb[:, :], sqk)
            yield
            P.act(t1, b1_[:, W_], AF.Sqrt)
            P.ts(t1, t1, 1e-12, MAX)
            P.recip(t1, t1)
            P.stt(kkn, k32, vcol(V_KK + c4), t1, MUL, MUL)
            P.ts(t2, a32, -1.0, ADD, vcol(V_KA + c4), MUL)
            P.stt(kmod, t2, 1.0, k32, ADD, MUL)
            yield
            v3 = lambda a: a.rearrange("p (c t) -> p c t", t=64)
            P.tt(bk[:, :, 1, :], v3(kmod), v3(eN), MUL)
            P.tt(t2, kkn, a32, MUL)
            P.tt(bk[:, :, 0, :], v3(t2), v3(eN), MUL)
            P.stt(ar[:, :, 0, :], v3(kkn), -1.0, v3(ePm), MUL, MUL)
            P.tt(ar[:, :, 1, :], v3(r32), v3(eP), MUL)
            P.stt(rbb, r32, vcol(V_RK + c4), kmod, MUL, MUL)
            P.mm(b0_[:, W_], bonesb[:, :], rbb)
            P.tt(bonv, b0_[:, W_], vb, MUL)
            yield
            for (src_of, dstT, pst) in ((lambda c: vb[:, c * 64:(c + 1) * 64], vT, b2_),
                                        (lambda c: bk[:, c, 0, :], bT, b3_),
                                        (lambda c: bk[:, c, 1, :], kT, b1_)):
                pv = pst[0:64, 0:256].bitcast(BF16).rearrange("p (c x) -> p c x", x=128)
                for c in range(NCk):
                    P.tr(pv[:, c, :], src_of(c), identb[:, :], signal=(c == NCk - 1))
                P.copy(dstT[:, :, :], pv[:, :, :], eng=evac_eng())
            yield
            for cp in range(NCk // 2):
                for hd in range(2):
                    pb = hd * 64
                    bank = (b2_, b3_)[hd]
                    pa = bank[0:64, :].rearrange("p (cc x) -> p cc x", cc=2)
                    for cc in range(2):
                        c = cp * 2 + cc
                        rhs = ar[pb:pb + 64, c, :, :].rearrange("p two t -> p (two t)")
                        P.mm(pa[:, cc, 0:128], bk[pb:pb + 64, c, 0, :], rhs, signal=False)
                        P.mm(pa[:, cc, 128:256], bk[pb:pb + 64, c, 1, :], rhs, signal=(cc == 1))
                    P.tt(AM[:, hd, cp * 2:cp * 2 + 2, :, :].rearrange("p c q t -> p (c q t)"), bank[0:64, :], maskA[:, :], MUL)
                yield
            pnt = [b0_[0:64, 0:256].rearrange("p (e t) -> p e t", t=64), b1_[0:64, 0:256].rearrange("p (e t) -> p e t", t=64)]
            for c in range(NCk):
                for hd in range(2):
                    pb = hd * 64
                    P.mm(pnt[hd][:, c, :], ar[pb:pb + 64, c, 0, :], bk[pb:pb + 64, c, 0, :], signal=(c == NCk - 1))
            for hd in range(2):
                P.tt(PkT[0][:, hd * NCk:(hd + 1) * NCk, :].rearrange("p e t -> p (e t)"), (b0_, b1_)[hd][0:64, 0:256], maskNT[:, 0:256], MUL)
            Nview = AM[:, :, :, 0, :].rearrange("p hd c t -> p (hd c) t")
            P.copy(PM[0][:, :, 0, :], Nview, eng="act")
            P.tt(PM[0][:, :, 1, :], Nview, identM[:, 0:NE * 64].rearrange("p (e t) -> p e t", t=64), ADD)
            ppv = b2_[0:64, :].rearrange("p (e t) -> p e t", t=64)
            for hd in range(2):
                for c in range(NCk):
                    P.mm(ppv[:, hd * NCk + c, :], AM[:, hd, c, 2, :], vT[:, c, hd * 64:(hd + 1) * 64], signal=(c == NCk - 1))
            P.copy(PV32[:, :, :], ppv, eng=evac_eng())
            yield
            cur = 0
            for lvl in range(6):
                nxt = 1 - cur
                for sbi in range(2):
                    es_ = range(sbi * NCk, (sbi + 1) * NCk)
                    p1 = (b2_, b3_)[sbi][0:64, :].rearrange("p (e x) -> p e x", x=128)
                    p2 = (b0_, b1_)[sbi][0:64, 0:256].rearrange("p (e t) -> p e t", t=64)
                    sl = slice(sbi * NCk, (sbi + 1) * NCk)
                    if lvl == 0:
                        for i, e in enumerate(es_):
                            P.mm(p1[:, i, 0:64], PkT[cur][:, e, :], PM[cur][:, e, 0, :], signal=(i == NCk - 1))
                        for i, e in enumerate(es_):
                            P.mm(p2[:, i, :], PM[cur][:, e, 0, :], PkT[cur][:, e, :], signal=(i == NCk - 1))
                        P.copy(PM[nxt][:, sl, 0, :], p1[:, :, 0:64], eng="act")
                        P.copy(PM[nxt][:, sl, 1, :], PM[cur][:, sl, 1, :], eng="dve")
                        P.copy(PkT[nxt][:, sl, :], p2, eng="dve")
                    elif lvl < 5:
                        for i, e in enumerate(es_):
                            P.mm(p1[:, i, :], PkT[cur][:, e, :], PM[cur][:, e, :, :].rearrange("p two t -> p (two t)"), signal=(i == NCk - 1))
                        for i, e in enumerate(es_):
                            P.mm(p2[:, i, :], PM[cur][:, e, 0, :], PkT[cur][:, e, :], signal=(i == NCk - 1))
                        P.copy(PM[nxt][:, sl, 0, :], p1[:, :, 0:64], eng="act")
                        P.tt(PM[nxt][:, sl, 1, :], p1[:, :, 64:128], PM[cur][:, sl, 1, :], ADD)
                        P.copy(PkT[nxt][:, sl, :], p2, eng="act")
                    else:
                        for i, e in enumerate(es_):
                            P.mm(p2[:, i, :], PkT[cur][:, e, :], PM[cur][:, e, 1, :], signal=(i == NCk - 1))
                        P.tt(PM[nxt][:, sl, 1, :], p2, PM[cur][:, sl, 1, :], ADD)
                    yield
                cur = nxt
            Mf = PM[cur]
            psc = [b2_[0:64, 0:64], b3_[0:64, 0:64]]
            psc2 = b0_[0:64, 0:128].rearrange("p (hd t) -> p hd t", hd=2)
            psz = b1_
            psya = [b2_, b3_]
            psyb = b1_
            for c in range(NCk):
                yc = slice(256 + c * 64, 256 + (c + 1) * 64)
                for hd in range(2):
                    pb = hd * 64
                    P.mm(psc[hd], ar[pb:pb + 64, c, 0, :], Zsb[pb:pb + 64, :], signal=(hd == 1))
                for hd in range(2):
                    pb = hd * 64
                    P.mm(psya[hd][pb:pb + 64, yc], Zsb[pb:pb + 64, :], ar[pb:pb + 64, c, 1, :], signal=(hd == 1))
                for hd in range(2):
                    P.tt(Pb[:, hd, :], psc[hd], PV32[:, hd * NCk + c, :], ADD)
                yield
                for hd in range(2):
                    P.mm(psc2[:, hd, :], Mf[:, hd * NCk + c, 1, :], Pb[:, hd, :], signal=(hd == 1))
                P.copy(Ub[:, :, :], psc2, eng="act")
                yield
                for hd in range(2):
                    pb = hd * 64
                    P.mm(psz[pb:pb + 64, 0:64], bT[:, c, pb:pb + 64], Ub[:, hd, :], start=True, stop=False, signal=False)
                    P.mm(psz[pb:pb + 64, 0:64], kT[:, c, pb:pb + 64], vT[:, c, pb:pb + 64], start=False, stop=True, signal=(hd == 1))
                for hd in range(2):
                    pb = hd * 64
                    P.mm(psyb[pb:pb + 64, yc], Ub[:, hd, :], AM[:, hd, c, 1, :], start=True, stop=False, signal=False)
                    P.mm(psyb[pb:pb + 64, yc], vT[:, c, pb:pb + 64], AM[:, hd, c, 3, :], start=False, stop=True, signal=(hd == 1))
                wc = eP[:, c * 64 + 63:c * 64 + 64]
                P.tt(Tt, psz[:, 0:64], Zs32, ADD)
                P.act(Zsb, Tt, AF.Copy, scale=wc)
                P.ts(Zs32, Tt, wc, MUL)
                yield
            for hd in range(2):
                pb = hd * 64
                P.act(Y1[pb:pb + 64, :], psya[hd][pb:pb + 64, 256:512], AF.Copy)
            P.tt(Y32, psyb[:, 256:512], Y1, ADD)
            P.act(ybf, Y32, AF.Copy)
            P.act(ysq, Y32, AF.Square)
            yield
            P.mm(b0_[:, W_], bonesb[:, :], ybf)
            P.mm(b2_[:, W_], bonesb[:, :], ysq)
            P.act(gt1, b0_[:, W_], AF.Copy, scale=1.0 / 64)
            P.tt(gt2, gt1, gt1, MUL)
            P.stt(gt2, b2_[:, W_], 1.0 / 64, gt2, MUL, SUB)
            P.ts(gt2, gt2, 0.0, MAX, GN_EPS, ADD)
            P.act(gt2, gt2, AF.Sqrt)
            P.recip(gt2, gt2)
            yield
            P.tt(gt3, Y32, gt1, SUB)
            P.tt(gt3, gt3, gt2, MUL)
            P.ts(gt3, gt3, vcol(V_LW + c4), MUL, vcol(V_LB + c4), ADD)
            P.tt(gt3, gt3, bonv, ADD)
            P.tt(ybuf[:, h, c4, t0:t0 + TW], gt3, G32, MUL)
            yield

        def run_lockstep(gens):
            live = list(gens)
            while live:
                nxt_live = []
                for g_ in live:
                    try:
                        next(g_)
                        nxt_live.append(g_)
                    except StopIteration:
                        pass
                live = nxt_live

        def mixer(j):
            norm_to_hT(V_G + 16)
            for h in range(NH):
                P.copy(hT[:, h, :, 1:2], hprev[:, h, :].unsqueeze(2), eng="dve")
                P.copy(hprev[:, h, :].unsqueeze(2), hT[:, h, :, 513:514], eng="dve")
            for h in range(NH):
                for part in range(2):
                    pp = ps[part]
                    cs0 = part * 128
                    n = 0
                    for k in range(8):
                        P.mm(pp[:, :], la_cur[:, k * 256 + cs0:k * 256 + cs0 + 128], hT[:, h, k, 2:514], start=(n == 0), stop=False)
                        n += 1
                        P.mm(pp[:, :], la_prev[:, k * 256 + cs0:k * 256 + cs0 + 128], hT[:, h, k, 1:513], start=False, stop=(k == 7))
                    if part == 0:
                        P.act(lora1[0:64, h, 0, :], pp[0:64, :], AF.Tanh)
                        P.act(lora1[64:128, h, 0, :], pp[64:128, :], AF.Copy)
                    else:
                        P.act(lora1[:, h, 1, :], pp[:, :], AF.Sigmoid)
            chk("lora_a")
            for c4 in range(4):
                w = ws_get()
                for sub in range(TT // TW):
                    run_lockstep([wkv_gen(j, h, c4, sub, w) for h in range(NH)])
            chk("wkv")
            for g, wd in enumerate((2, 4, 8, 16)):
                w = ws_get()
                nlev = g + 1
                for h in range(NH):
                    PB, PS1, PS2, pmix = PBs[h], PS1s[h], PS2s[h], pmixs[h]
                    pp = ps[h % 2]
                    for k in range(8):
                        P.mm(pp[:, :], w[:, k * 128:(k + 1) * 128], hT[:, h, k, 2:514], start=(k == 0), stop=(k == 7))
                    P.copy(PB[:, 0:16], poolcar[:, h, g, :], eng="dve")
                    P.act(PB[:, 16:528], pp[:, :], AF.Copy)
                    P.copy(poolcar[:, h, g, :], PB[:, 512:528], eng="dve")
                    src, lo = PB, 0
                    bufs = [PS1, PS2]
                    for lv in range(nlev):
                        sh = 1 << lv
                        dstb = bufs[lv % 2]
                        nlo = lo + sh
                        P.tt(dstb[:, nlo:528], src[:, nlo:528], src[:, nlo - sh:528 - sh], ADD)
                        src, lo = dstb, nlo
                    P.stt(pmix, src[:, 16:528], 1.0 / wd, PB[:, 16:528], MUL, SUB)
                    if j == 0:
                        P.tt(t1[:, 0:16], src[:, 16:32], invc0[:, g * 16:(g + 1) * 16], MUL)
                        P.tt(pmix[:, 0:16], t1[:, 0:16], PB[:, 16:32], SUB)
                    pq = ps[2 + h % 2]
                    P.mm(pq[:, :], poolw[:, g * 128:(g + 1) * 128], pmix)
                    P.act(ypool[:, h, g, :], pq[:, :], AF.Copy, scale=vcol(V_PS + g))
            chk("pool")
            for dch in range(8):
                w = ws_get()
                for h in range(NH):
                    b0 = (h % 2) * 4
                    pg0, pg1, pbr, pbp = ps[b0], ps[b0 + 1], ps[b0 + 2], ps[b0 + 3]
                    for k in range(8):
                        P.mm(pg0[:, :], w[:, k * 128:(k + 1) * 128], hT[:, h, k, 2:514], start=(k == 0), stop=(k == 7))
                    for k in range(8):
                        P.mm(pg1[:, :], w[:, 1024 + k * 128:1024 + (k + 1) * 128], hT[:, h, k, 2:514], start=(k == 0), stop=(k == 7))
                    for k in range(4):
                        P.mm(pbr[:, :], w[:, 2048 + k * 128:2048 + (k + 1) * 128], ybuf[:, h, k, :], start=(k == 0), stop=(k == 3))
                    for k in range(4):
                        P.mm(pbp[:, :], w[:, 2560 + k * 128:2560 + (k + 1) * 128], ypool[:, h, k, :], start=(k == 0), stop=(k == 3))
                    P.act(ms0, pg0[:, :], AF.Sigmoid, bias=vcol(V_GB + dch))
                    P.act(ms1, pg1[:, :], AF.Sigmoid, bias=vcol(V_GB + 8 + dch))
                    P.tt(mm0, pbr[:, :], ms0, MUL)
                    P.tt(mm1, pbp[:, :], ms1, MUL)
                    P.tt(mrg[:, h, dch, :], mm0, mm1, ADD)
            chk("merge")
            out_proj_phase(8, lambda h, k: mrg[:, h, k, :])
            residual_update(V_G + 24)
            chk("mixer")

        def main_loop():
          for j in range(NT):
            for h in range(NH):
                for tb in range(4):
                    si = rot["xio"] % 3
                    rot["xio"] += 1
                    xs = xio[si]
                    P.dma("sp", CH_X[si], xs[:, :], dr["xin"][h, j * TT + tb * 128:j * TT + (tb + 1) * 128, :])
                    for half in range(2):
                        pst = ps[(tb * 2 + half) % 4]
                        for kk in range(4):
                            k = half * 4 + kk
                            P.tr(pst[:, kk * 128:(kk + 1) * 128], xs[:, k * 128:(k + 1) * 128], ident[:, :], signal=(kk == 3))
                        P.copy(xT[:, h, half * 4:half * 4 + 4, tb * 128:(tb + 1) * 128],
                               pst[:, :].rearrange("p (k t) -> p k t", t=128), eng=evac_eng())
            chk("load")
            ffn(V_G + 0, V_HG1)
            if j == 0:
                dbg_dump("x1", xT[:, :, :, :])
            mixer(j)
            if j == 0:
                dbg_dump("ybuf", ybuf)
                dbg_dump("x2", xT[:, :, :, :])
            ffn(V_G + 32, V_HG5)
            for h in range(NH):
                for tb in range(4):
                    si = rot["xio"] % 3
                    rot["xio"] += 1
                    xs = xio[si]
                    for half in range(2):
                        pst = ps[(tb * 2 + half) % 4]
                        for kk in range(4):
                            k = half * 4 + kk
                            P.tr(pst[:, kk * 128:(kk + 1) * 128], xT[:, h, k, tb * 128:(tb + 1) * 128], ident[:, :], signal=(kk == 3))
                        P.copy(xs[:, half * 512:(half + 1) * 512], pst[:, :], eng=evac_eng())
                    P.dma("sp", CH_X[si], dr["out"][h, j * TT + tb * 128:j * TT + (tb + 1) * 128, :], xs[:, :])
        try:
            main_loop()
            assert ws["next"] == len(units), (ws["next"], len(units))
        except StopBuild:
            print("[kernel] build stopped after", stop_after, flush=True)
        P.finish("sp")
        P.emit(block, sems, dsems)
        print(f"[kernel] S={S} ops={P.nops} per-engine={ {e: len(P.ops[e]) for e in ENGS} }", flush=True)
    return nc


_CACHE = {}


def run(inputs, S, ncores, dbg=None, stop_after=None):
    x = np.asarray(inputs["x"], np.float32)
    inp = {k: np.asarray(v, np.float32) for k, v in inputs.items() if k != "x"}
    shared = host_weights(inp)
    shared.update(host_consts())
    key = (S, tuple(sorted(dbg.items())) if dbg else None)
    nc = build(S, dbg, stop_after)
    in_maps = []
    for c in range(ncores):
        m = dict(shared)
        m["xin"] = np.ascontiguousarray(x[c * NH:(c + 1) * NH, :S])
        in_maps.append(m)
    res = run_bass_kernel_spmd(nc, in_maps, core_ids=list(range(ncores)))
    out = np.concatenate([r["out"] for r in res.results], 0)
    return out, res


def kernel(**inputs):
    out, _ = run(inputs, 2048, NCORES)
    return out.astype(np.float32)
```
